# Optimizing a Trainium2 kernel written in Bass

```python
import math
import jax, jax.numpy as jnp
from jax import lax
import numpy as np

D_MODEL = 2048
BATCH = 2
SEQ = 8192
DEPTH = 4

N_MEM = 256
N_MIXERS = 3
MIX_WIDTH = 1536
XA_HEADS = 4
XA_HEAD_DIM = 128
XA_WIDTH = XA_HEADS * XA_HEAD_DIM
CAT_WIDTH = MIX_WIDTH + XA_WIDTH
HEAD_DIM = 64
N_Q_HEADS = MIX_WIDTH // HEAD_DIM
N_KV_HEADS = 3
Q_PER_KV = N_Q_HEADS // N_KV_HEADS
WINDOW = 128
ROPE_DIM = HEAD_DIM // 4
ROPE_THETA = 500000.0
SSD_HEAD_DIM = 64
SSD_HEADS = MIX_WIDTH // SSD_HEAD_DIM
SSD_GROUPS = 4
SSD_HEADS_PER_GROUP = SSD_HEADS // SSD_GROUPS
SSD_STATE = 128
SSD_CONV = 4
SSD_CHUNK = 128
SSD_CONV_CH = MIX_WIDTH + 2 * SSD_GROUPS * SSD_STATE
CONF_WIDTH = 31
N_EXPERTS = 32
TOP_K = 4
D_FF = 1024
SWIGLU_LIMIT = 7.0
SWIGLU_ALPHA = 1.702
MOE_BLOCK = 512
DN_ALPHA = (2 * DEPTH) ** 0.25
DN_BETA = (8 * DEPTH) ** -0.25
LN_EPS = 1e-5

ATTN_IN = MIX_WIDTH + 2 * N_KV_HEADS * HEAD_DIM + XA_WIDTH
SSD_IN = MIX_WIDTH + SSD_CONV_CH + SSD_HEADS + XA_WIDTH
CONF_IN = 2 * MIX_WIDTH + XA_WIDTH
N_ATTN = (DEPTH + 2) // 3
N_SSD = (DEPTH + 1) // 3
N_CONF = DEPTH // 3

kernel_name = "hybrid_swa_ssd_conformer_moe_deepnorm"


def layer_norm(x, g, b):
    xf = x.astype(jnp.float32)
    mu = jnp.mean(xf, -1, keepdims=True)
    var = jnp.mean(jnp.square(xf - mu), -1, keepdims=True)
    return ((xf - mu) * lax.rsqrt(var + LN_EPS)).astype(x.dtype) * g + b


def rms_norm(x, g):
    xf = x.astype(jnp.float32)
    return (xf * lax.rsqrt(jnp.mean(xf * xf, -1, keepdims=True) + LN_EPS)).astype(x.dtype) * g


def partial_rope(t, positions):
    half = ROPE_DIM // 2
    inv_freq = ROPE_THETA ** (-jnp.arange(half, dtype=jnp.float32) / half)
    ang = positions.astype(jnp.float32)[..., None] * inv_freq
    ang = ang[:, :, None, :]
    cos, sin = jnp.cos(ang).astype(t.dtype), jnp.sin(ang).astype(t.dtype)
    t1, t2, rest = t[..., :half], t[..., half:ROPE_DIM], t[..., ROPE_DIM:]
    return jnp.concatenate([t1 * cos - t2 * sin, t2 * cos + t1 * sin, rest], -1)


def causal_dwconv(x, w, b):
    K, C = w.shape
    y = lax.conv_general_dilated(x, w[:, None, :].astype(x.dtype), window_strides=(1,),
                                 padding=[(K - 1, 0)], dimension_numbers=('NWC', 'WIO', 'NWC'),
                                 feature_group_count=C)
    return y + b


def sliding_window_attention(q, k, v, sinks):
    Bsz, L = q.shape[:2]
    nb = L // WINDOW
    qb = q.reshape(Bsz, nb, WINDOW, N_KV_HEADS, Q_PER_KV, HEAD_DIM)

    def band(t):
        tb = t.reshape(Bsz, nb, WINDOW, N_KV_HEADS, HEAD_DIM)
        prev = jnp.pad(tb[:, :-1], ((0, 0), (1, 0), (0, 0), (0, 0), (0, 0)))
        return jnp.concatenate([prev, tb], axis=2)

    kb, vb = band(k), band(v)
    s = jnp.einsum('bnqgrd,bnsgd->bngrqs', qb, kb).astype(jnp.float32) * (HEAD_DIM ** -0.5)
    qi = jnp.arange(WINDOW)[:, None]
    sj = jnp.arange(2 * WINDOW)[None, :]
    rel = qi + WINDOW - sj
    band_mask = (rel >= 0) & (rel < WINDOW)
    mask = band_mask[None] & ((jnp.arange(nb)[:, None, None] > 0) | (sj[None] >= WINDOW))
    s = jnp.where(mask[None, :, None, None], s, -jnp.inf)
    sink = jnp.broadcast_to(sinks.astype(jnp.float32).reshape(1, 1, N_KV_HEADS, Q_PER_KV, 1, 1),
                            s.shape[:-1] + (1,))
    p = jax.nn.softmax(jnp.concatenate([s, sink], -1), axis=-1)[..., :-1]
    o = jnp.einsum('bngrqs,bnsgd->bnqgrd', p.astype(vb.dtype), vb)
    return o.reshape(Bsz, L, MIX_WIDTH)


def swa_mixer(u, positions, sinks):
    Bsz, L = u.shape[:2]
    q = u[..., :MIX_WIDTH].reshape(Bsz, L, N_Q_HEADS, HEAD_DIM)
    kv = u[..., MIX_WIDTH:].reshape(Bsz, L, 2, N_KV_HEADS, HEAD_DIM)
    k, v = kv[:, :, 0], kv[:, :, 1]
    q = partial_rope(q, positions).reshape(Bsz, L, N_KV_HEADS, Q_PER_KV, HEAD_DIM)
    k = partial_rope(k, positions)
    return sliding_window_attention(q, k, v, sinks)


def ssd_chunked_scan(x, dt, A, Bm, Cm):
    Bsz, L = x.shape[:2]
    nc, Q, G, R = L // SSD_CHUNK, SSD_CHUNK, SSD_GROUPS, SSD_HEADS_PER_GROUP
    xf = x.astype(jnp.float32).reshape(Bsz, nc, Q, G, R, SSD_HEAD_DIM)
    dtc = dt.reshape(Bsz, nc, Q, G, R)
    Bc = Bm.astype(jnp.float32).reshape(Bsz, nc, Q, G, SSD_STATE)
    Cc = Cm.astype(jnp.float32).reshape(Bsz, nc, Q, G, SSD_STATE)
    a_cs = jnp.cumsum(dtc * A.reshape(G, R), axis=2)
    xdt = xf * dtc[..., None]
    seg = a_cs[:, :, :, None] - a_cs[:, :, None, :]
    causal = jnp.tril(jnp.ones((Q, Q), bool))[None, None, :, :, None, None]
    decay = jnp.exp(jnp.where(causal, seg, -jnp.inf))
    cb = jnp.einsum('bclgn,bcsgn->bclsg', Cc, Bc)
    y_diag = jnp.einsum('bclsg,bclsgr,bcsgrp->bclgrp', cb, decay, xdt)
    decay_to_end = jnp.exp(a_cs[:, :, -1:] - a_cs)
    states = jnp.einsum('bclgn,bclgr,bclgrp->bcgrpn', Bc, decay_to_end, xdt)
    chunk_decay = jnp.exp(a_cs[:, :, -1])

    def step(h, inp):
        s_c, d_c = inp
        return h * d_c[..., None, None] + s_c, h

    h0 = jnp.zeros((Bsz, G, R, SSD_HEAD_DIM, SSD_STATE), jnp.float32)
    _, prev = lax.scan(step, h0, (jnp.moveaxis(states, 1, 0), jnp.moveaxis(chunk_decay, 1, 0)))
    prev = jnp.moveaxis(prev, 0, 1)
    y_off = jnp.einsum('bclgn,bcgrpn,bclgr->bclgrp', Cc, prev, jnp.exp(a_cs))
    return (y_diag + y_off).reshape(Bsz, L, SSD_HEADS, SSD_HEAD_DIM)


def ssd_mixer(u, conv_w, conv_b, dt_bias, a_log, d_skip, norm_g):
    Bsz, L = u.shape[:2]
    gn = SSD_GROUPS * SSD_STATE
    z = u[..., :MIX_WIDTH]
    xbc = jax.nn.silu(causal_dwconv(u[..., MIX_WIDTH:MIX_WIDTH + SSD_CONV_CH], conv_w, conv_b))
    dt_raw = u[..., MIX_WIDTH + SSD_CONV_CH:]
    xs = xbc[..., :MIX_WIDTH].reshape(Bsz, L, SSD_HEADS, SSD_HEAD_DIM)
    Bm = xbc[..., MIX_WIDTH:MIX_WIDTH + gn].reshape(Bsz, L, SSD_GROUPS, SSD_STATE)
    Cm = xbc[..., MIX_WIDTH + gn:].reshape(Bsz, L, SSD_GROUPS, SSD_STATE)
    dt = jax.nn.softplus(dt_raw.astype(jnp.float32) + dt_bias.astype(jnp.float32))
    A = -jnp.exp(a_log.astype(jnp.float32))
    y = ssd_chunked_scan(xs, dt, A, Bm, Cm)
    y = y + d_skip.astype(jnp.float32)[:, None] * xs.astype(jnp.float32)
    y = y.reshape(Bsz, L, MIX_WIDTH).astype(u.dtype)
    return rms_norm(y * jax.nn.silu(z), norm_g)


def conformer_conv(u, dw_w, dw_b, ln_g, ln_b):
    h = u[..., :MIX_WIDTH] * jax.nn.sigmoid(u[..., MIX_WIDTH:])
    h = causal_dwconv(h, dw_w, dw_b)
    h = layer_norm(h, ln_g, ln_b)
    return jax.nn.silu(h)


def memory_attention(q, mem, w_kv):
    Bsz, L = q.shape[:2]
    qh = q.reshape(Bsz, L, XA_HEADS, XA_HEAD_DIM)
    kv = (mem @ w_kv).reshape(Bsz, mem.shape[1], 2, XA_HEADS, XA_HEAD_DIM)
    mk, mv = kv[:, :, 0], kv[:, :, 1]
    s = jnp.einsum('blhd,bmhd->bhlm', qh, mk).astype(jnp.float32) * (XA_HEAD_DIM ** -0.5)
    p = jax.nn.softmax(s, axis=-1).astype(mv.dtype)
    return jnp.einsum('bhlm,bmhd->blhd', p, mv).reshape(Bsz, L, XA_WIDTH)


def moe_ffn(x, router_w, router_b, w1, b1, w2, b2):
    Bsz, L, D = x.shape
    T = Bsz * L
    xt = x.reshape(T, D)
    logits = (xt @ router_w).astype(jnp.float32) + router_b.astype(jnp.float32)
    top_val, top_idx = lax.top_k(logits, TOP_K)
    gates = jax.nn.softmax(top_val, axis=-1)
    flat_e = top_idx.reshape(-1)
    flat_t = jnp.arange(T * TOP_K, dtype=jnp.int32) // TOP_K
    flat_g = gates.reshape(-1)
    order = jnp.argsort(flat_e)
    se = flat_e[order]
    counts = jnp.bincount(flat_e, length=N_EXPERTS)
    start = jnp.cumsum(counts) - counts
    pcounts = (counts + MOE_BLOCK - 1) // MOE_BLOCK * MOE_BLOCK
    pend = jnp.cumsum(pcounts)
    pstart = pend - pcounts
    dest = pstart[se] + (jnp.arange(T * TOP_K) - start[se])
    n_blocks = -(-(T * TOP_K) // MOE_BLOCK) + N_EXPERTS
    S = n_blocks * MOE_BLOCK
    buf_tok = jnp.full((S,), T, jnp.int32).at[dest].set(flat_t[order])
    buf_g = jnp.zeros((S,), x.dtype).at[dest].set(flat_g[order].astype(x.dtype))
    block_e = jnp.minimum(jnp.searchsorted(pend, jnp.arange(n_blocks) * MOE_BLOCK, side='right'),
                          N_EXPERTS - 1)
    x_pad = jnp.concatenate([xt, jnp.zeros((1, D), xt.dtype)], axis=0)

    def run_block(args):
        tok, g, e = args
        h = x_pad[tok] @ w1[e] + b1[e]
        glu = jnp.minimum(h[:, :D_FF], SWIGLU_LIMIT)
        lin = jnp.clip(h[:, D_FF:], -SWIGLU_LIMIT, SWIGLU_LIMIT)
        act = glu * jax.nn.sigmoid(SWIGLU_ALPHA * glu) * (lin + 1)
        return (act @ w2[e] + b2[e]) * g[:, None]

    out = lax.map(run_block, (buf_tok.reshape(n_blocks, MOE_BLOCK),
                              buf_g.reshape(n_blocks, MOE_BLOCK), block_e))
    y = jax.ops.segment_sum(out.reshape(S, D), buf_tok, num_segments=T + 1)[:T]
    return y.reshape(Bsz, L, D)


def setup_inputs(seed: int = 0) -> dict:
    key = jax.random.key(seed)
    ks = iter(jax.random.split(key, 64))

    def nrm(shape, scale):
        return jax.random.normal(next(ks), shape, jnp.float32) * scale

    D = D_MODEL
    x = nrm((BATCH, SEQ, D), 1.0)
    mem = nrm((BATCH, N_MEM, D), 1.0)
    offs = jax.random.randint(next(ks), (BATCH, 1), 0, 4096, jnp.int32)
    positions = offs + jnp.arange(SEQ, dtype=jnp.int32)[None, :]
    dt0 = jnp.exp(jax.random.uniform(next(ks), (N_SSD, SSD_HEADS), jnp.float32)
                  * (math.log(0.1) - math.log(0.001)) + math.log(0.001))
    ssd_dt_bias = dt0 + jnp.log(-jnp.expm1(-dt0))
    ssd_a_log = jnp.log(jax.random.uniform(next(ks), (N_SSD, SSD_HEADS), jnp.float32, 1.0, 16.0))
    return {
        "x": x, "mem": mem, "positions": positions,
        "attn_w_in": nrm((N_ATTN, D, ATTN_IN), D ** -0.5),
        "attn_b_in": nrm((N_ATTN, ATTN_IN), 0.02),
        "attn_sinks": nrm((N_ATTN, N_Q_HEADS), 0.5),
        "ssd_w_in": nrm((N_SSD, D, SSD_IN), D ** -0.5),
        "ssd_b_in": nrm((N_SSD, SSD_IN), 0.02),
        "ssd_conv_w": nrm((N_SSD, SSD_CONV, SSD_CONV_CH), SSD_CONV ** -0.5),
        "ssd_conv_b": nrm((N_SSD, SSD_CONV_CH), 0.02),
        "ssd_dt_bias": ssd_dt_bias,
        "ssd_a_log": ssd_a_log,
        "ssd_d_skip": 1.0 + nrm((N_SSD, SSD_HEADS), 0.1),
        "ssd_norm_g": 1.0 + nrm((N_SSD, MIX_WIDTH), 0.02),
        "conf_w_in": nrm((N_CONF, D, CONF_IN), D ** -0.5),
        "conf_b_in": nrm((N_CONF, CONF_IN), 0.02),
        "conf_dw_w": nrm((N_CONF, CONF_WIDTH, MIX_WIDTH), CONF_WIDTH ** -0.5),
        "conf_dw_b": nrm((N_CONF, MIX_WIDTH), 0.02),
        "conf_ln_g": 1.0 + nrm((N_CONF, MIX_WIDTH), 0.02),
        "conf_ln_b": nrm((N_CONF, MIX_WIDTH), 0.02),
        "mem_w_kv": nrm((DEPTH, D, 2 * XA_WIDTH), D ** -0.5),
        "w_out": nrm((DEPTH, CAT_WIDTH, D), CAT_WIDTH ** -0.5 * DN_BETA),
        "b_out": nrm((DEPTH, D), 0.02),
        "ln1_g": 1.0 + nrm((DEPTH, D), 0.02),
        "ln1_b": nrm((DEPTH, D), 0.02),
        "router_w": nrm((DEPTH, D, N_EXPERTS), D ** -0.5),
        "router_b": nrm((DEPTH, N_EXPERTS), 0.01),
        "moe_w1": nrm((DEPTH, N_EXPERTS, D, 2 * D_FF), D ** -0.5),
        "moe_b1": nrm((DEPTH, N_EXPERTS, 2 * D_FF), 0.02),
        "moe_w2": nrm((DEPTH, N_EXPERTS, D_FF, D), D_FF ** -0.5 * DN_BETA),
        "moe_b2": nrm((DEPTH, N_EXPERTS, D), 0.02),
        "ln2_g": 1.0 + nrm((DEPTH, D), 0.02),
        "ln2_b": nrm((DEPTH, D), 0.02),
    }


def reference(x, mem, positions,
              attn_w_in, attn_b_in, attn_sinks,
              ssd_w_in, ssd_b_in, ssd_conv_w, ssd_conv_b, ssd_dt_bias, ssd_a_log, ssd_d_skip, ssd_norm_g,
              conf_w_in, conf_b_in, conf_dw_w, conf_dw_b, conf_ln_g, conf_ln_b,
              mem_w_kv, w_out, b_out, ln1_g, ln1_b,
              router_w, router_b, moe_w1, moe_b1, moe_w2, moe_b2, ln2_g, ln2_b):
    h = x
    for i in range(DEPTH):
        kind, j = i % N_MIXERS, i // N_MIXERS
        if kind == 0:
            u = h @ attn_w_in[j] + attn_b_in[j]
            mix = swa_mixer(u[..., :-XA_WIDTH], positions, attn_sinks[j])
        elif kind == 1:
            u = h @ ssd_w_in[j] + ssd_b_in[j]
            mix = ssd_mixer(u[..., :-XA_WIDTH], ssd_conv_w[j], ssd_conv_b[j], ssd_dt_bias[j],
                            ssd_a_log[j], ssd_d_skip[j], ssd_norm_g[j])
        else:
            u = h @ conf_w_in[j] + conf_b_in[j]
            mix = conformer_conv(u[..., :-XA_WIDTH], conf_dw_w[j], conf_dw_b[j], conf_ln_g[j], conf_ln_b[j])
        xa = memory_attention(u[..., -XA_WIDTH:], mem, mem_w_kv[i])
        sub = jnp.concatenate([mix.astype(h.dtype), xa.astype(h.dtype)], axis=-1) @ w_out[i] + b_out[i]
        h = layer_norm(DN_ALPHA * h + sub, ln1_g[i], ln1_b[i])
        ffn = moe_ffn(h, router_w[i], router_b[i], moe_w1[i], moe_b1[i], moe_w2[i], moe_b2[i])
        h = layer_norm(DN_ALPHA * h + ffn, ln2_g[i], ln2_b[i])
    return h
```

```python
import numpy as np
from contextlib import ExitStack
import concourse.bass as bass
import concourse.mybir as mybir
from concourse.bass_utils import run_bass_kernel_spmd

F32 = mybir.dt.float32
BF16 = mybir.dt.bfloat16
I32 = mybir.dt.int32
AF = mybir.ActivationFunctionType
ALU = mybir.AluOpType
AX = mybir.AxisListType

NDS = 8
SELF_SYNC = True


class _Ev:
    __slots__ = ("sem", "key", "val", "clock")

    def __init__(self, sem, key, val, clock):
        self.sem, self.key, self.val, self.clock = sem, key, val, clock


class _Eng:
    def __init__(self, name, eng, sem):
        self.name, self.eng, self.sem = name, eng, sem
        self.key = name
        self.count = 0
        self.clock = {}
        self.dsems = []
        self.dcnt = []
        self.devs = []
        self.dn = 0


class Ctx:
    def __init__(self, nc):
        self.nc = nc
        self.es = ExitStack()
        self.E = {}
        for name, e in (("pe", nc.tensor), ("dve", nc.vector), ("act", nc.scalar),
                        ("pool", nc.gpsimd), ("sp", nc.sync)):
            sem = self.es.enter_context(nc.semaphore("s_" + name))
            self.E[name] = _Eng(name, e, sem)
        for q in ("sp", "pool", "act"):
            Q = self.E[q]
            for i in range(NDS):
                Q.dsems.append(self.es.enter_context(nc.semaphore("d_%s%d" % (q, i))))
                Q.dcnt.append(0)
                Q.devs.append(None)
        self.dep = {}
        self.nwaits = 0
        self.nops = 0

    def sb(self, name, shape, dtype, es=None):
        return (es or self.es).enter_context(self.nc.sbuf_tensor(name, list(shape), dtype))

    def ps(self, name, shape, dtype, es=None):
        return (es or self.es).enter_context(self.nc.psum_tensor(name, list(shape), dtype))

    def _wait(self, E, ev):
        if ev is None:
            return
        if E.clock.get(ev.key, 0) >= ev.val:
            return
        E.eng.wait_ge(ev.sem, ev.val)
        self.nwaits += 1
        ck = E.clock
        for k, v in ev.clock.items():
            if ck.get(k, 0) < v:
                ck[k] = v
        if ck.get(ev.key, 0) < ev.val:
            ck[ev.key] = ev.val

    def _deps(self, E, r, w):
        dep = self.dep
        for k in r:
            d = dep.get(k)
            if d is not None and d[0] is not None:
                self._wait(E, d[0])
        for k in w:
            d = dep.get(k)
            if d is not None:
                if d[0] is not None:
                    self._wait(E, d[0])
                for ev in d[1]:
                    self._wait(E, ev)

    def _record(self, ev, r, w, prune_key=None):
        dep = self.dep
        for k in r:
            d = dep.get(k)
            if d is None:
                d = dep[k] = [None, []]
            if prune_key is not None:
                d[1] = [x for x in d[1] if x.key != prune_key]
            d[1].append(ev)
        for k in w:
            dep[k] = [ev, []]

    def op(self, en, fn, r=(), w=()):
        E = self.E[en]
        self._deps(E, r, w)
        inst = fn(E.eng)
        E.count += 1
        inst.then_inc(E.sem, 1)
        if en == "pe" or not SELF_SYNC:
            E.clock[E.key] = E.count
        ck = dict(E.clock)
        ck[E.key] = E.count
        ev = _Ev(E.sem, E.key, E.count, ck)
        self._record(ev, r, w, prune_key=E.key)
        self.nops += 1
        return ev

    def dma(self, q, fn, r=(), w=()):
        Q = self.E[q]
        slot = Q.dn % NDS
        Q.dn += 1
        self._wait(Q, Q.devs[slot])
        self._deps(Q, r, w)
        inst = fn(Q.eng)
        Q.dcnt[slot] += 16
        inst.then_inc(Q.dsems[slot], 16)
        key = "d_%s%d" % (q, slot)
        ck = dict(Q.clock)
        ck[key] = Q.dcnt[slot]
        ev = _Ev(Q.dsems[slot], key, Q.dcnt[slot], ck)
        Q.devs[slot] = ev
        self._record(ev, r, w)
        self.nops += 1
        return ev

    def barrier(self, engines=("pe", "dve", "act", "pool", "sp")):
        evs = []
        for n, E in self.E.items():
            if E.count > 0:
                ck = dict(E.clock)
                ck[E.key] = E.count
                evs.append(_Ev(E.sem, E.key, E.count, ck))
            for ev in E.devs:
                if ev is not None:
                    evs.append(ev)
        for n in engines:
            E = self.E[n]
            for ev in evs:
                if ev.key == E.key and (n == "pe" or not SELF_SYNC):
                    continue
                self._wait(E, ev)

    def finish(self):
        self.barrier(engines=("sp",))


DN_ALPHA = 8 ** 0.25
LN_EPS = 1e-5


def make_consts(cx, es=None):
    nc = cx.nc
    c = {}
    c["idf"] = cx.sb("c_idf", [128, 128], F32, es)
    c["idb"] = cx.sb("c_idb", [128, 128], BF16, es)
    c["onesb"] = cx.sb("c_onesb", [128, 128], BF16, es)
    c["onesf"] = cx.sb("c_onesf", [128, 128], F32, es)
    c["trib"] = cx.sb("c_trib", [128, 128], BF16, es)
    c["trif"] = cx.sb("c_trif", [128, 128], F32, es)
    cx.op("pool", lambda e: e.memset(c["onesf"][:], 1.0), w=["c_onesf"])
    cx.op("pool", lambda e: e.memset(c["onesb"][:], 1.0), w=["c_onesb"])
    cx.op("pool", lambda e: e.affine_select(out=c["idf"][:], in_=c["onesf"][:], pattern=[[-1, 128]],
                                            compare_op=ALU.is_equal, fill=0.0, base=0, channel_multiplier=1),
          r=["c_onesf"], w=["c_idf"])
    cx.op("pool", lambda e: e.tensor_copy(out=c["idb"][:], in_=c["idf"][:]), r=["c_idf"], w=["c_idb"])
    cx.op("pool", lambda e: e.affine_select(out=c["trib"][:], in_=c["onesb"][:], pattern=[[1, 128]],
                                            compare_op=ALU.is_gt, fill=0.0, base=0, channel_multiplier=-1),
          r=["c_onesb"], w=["c_trib"])
    cx.op("pool", lambda e: e.affine_select(out=c["trif"][:], in_=c["onesf"][:], pattern=[[1, 128]],
                                            compare_op=ALU.is_ge, fill=0.0, base=0, channel_multiplier=-1),
          r=["c_onesf"], w=["c_trif"])
    return c


def layer_norm_tile(cx, acc, out, G, Bt, tmp_stats, D, tagr, tagw, eng="dve", rk=(), wk=()):
    nch = D // 512
    st, mv, rstd = tmp_stats
    for j in range(nch):
        cx.op("dve", lambda e, j=j: e.bn_stats(out=st[:, j * 6:(j + 1) * 6], in_=acc[:, j * 512:(j + 1) * 512]),
              r=[tagr], w=[("st", tagw, j)])
    cx.op("dve", lambda e: e.bn_aggr(out=mv[:, 0:2], in_=st[:, 0:nch * 6]),
          r=[("st", tagw, j) for j in range(nch)], w=[("mv", tagw)])
    cx.op("dve", lambda e: e.tensor_scalar(out=rstd[:, 0:1], in0=mv[:, 1:2], scalar1=LN_EPS, scalar2=None,
                                           op0=ALU.add), r=[("mv", tagw)], w=[("rstd", tagw)])
    cx.op("act", lambda e: e.sqrt(out=rstd[:, 0:1], in_=rstd[:, 0:1]), r=[("rstd", tagw)], w=[("rstd", tagw)])
    cx.op("dve", lambda e: e.reciprocal(out=rstd[:, 0:1], in_=rstd[:, 0:1]), r=[("rstd", tagw)], w=[("rstd", tagw)])
    cx.op("dve", lambda e: e.tensor_scalar(out=out, in0=acc, scalar1=mv[:, 0:1], scalar2=rstd[:, 0:1],
                                           op0=ALU.subtract, op1=ALU.mult),
          r=[tagr, ("mv", tagw), ("rstd", tagw)], w=[tagw])
    cx.op("dve", lambda e: e.tensor_tensor(out=out, in0=out, in1=G, op=ALU.mult), r=[tagw] + list(rk), w=[tagw])
    cx.op("dve", lambda e: e.tensor_tensor(out=out, in0=out, in1=Bt, op=ALU.add), r=[tagw] + list(rk), w=[tagw])


def build_moe(T=2048, D=2048, E=32, FF=1024, C=384, alpha=DN_ALPHA):
    nc = bass.Bass("TRN2", target_bir_lowering=False)
    NT = T // 128
    KD = D // 128
    KF = FF // 128
    NS = E * C
    CS = C // 128
    h1 = nc.dram_tensor("h1", [T, D], F32, kind="ExternalInput").ap()
    rw = nc.dram_tensor("router_w", [D, E], F32, kind="ExternalInput").ap()
    rb = nc.dram_tensor("router_b", [1, E], F32, kind="ExternalInput").ap()
    w1 = nc.dram_tensor("w1", [E, D, 2 * FF], F32, kind="ExternalInput").ap()
    b1 = nc.dram_tensor("b1", [E, 2 * FF], F32, kind="ExternalInput").ap()
    w2 = nc.dram_tensor("w2", [E, FF, D], F32, kind="ExternalInput").ap()
    b2 = nc.dram_tensor("b2", [E, D], F32, kind="ExternalInput").ap()
    lg = nc.dram_tensor("ln_g", [1, D], F32, kind="ExternalInput").ap()
    lb = nc.dram_tensor("ln_b", [1, D], F32, kind="ExternalInput").ap()
    out = nc.dram_tensor("out", [T, D], F32, kind="ExternalOutput").ap()
    Xg = nc.dram_tensor("Xg", [NS + 128, D], BF16, kind="Internal").ap()
    Yg = nc.dram_tensor("Yg", [NS + 128, D], F32, kind="Internal").ap()

    cx = Ctx(nc)
    cst = make_consts(cx)
    SL = cx.sb("SL", [128, NT, 4], I32)
    GK = cx.sb("GK", [128, NT, 4], F32)
    B1T = cx.sb("B1T", [128, 2 * KF, E], F32)

    with ExitStack() as es:
        Wr = cx.sb("Wr", [128, KD, E], F32, es)
        rbt = cx.sb("rbt", [1, E], F32, es)
        b1s = cx.sb("b1s", [E, 2 * FF], F32, es)
        zt = cx.sb("zt", [128, 4, D], BF16, es)
        CNT = cx.sb("CNT", [128, E], F32, es)
        EOFF = cx.sb("EOFF", [128, E], F32, es)
        xt = [cx.sb("xt%d" % i, [128, D], F32, es) for i in range(2)]
        xb = [cx.sb("xb%d" % i, [128, D], BF16, es) for i in range(2)]
        xT = [cx.sb("xT%d" % i, [128, KD, 128], F32, es) for i in range(2)]
        LGt = cx.sb("LGt", [128, E], F32, es)
        MKb = cx.sb("MKb", [128, E], BF16, es)
        MKf = cx.sb("MKf", [128, E], F32, es)
        GT = cx.sb("GT", [128, E], F32, es)
        EX = cx.sb("EX", [128, E], F32, es)
        V = cx.sb("V", [128, E], F32, es)
        POS = cx.sb("POS", [128, E], F32, es)
        TM = cx.sb("TM", [128, E], F32, es)
        mx8 = cx.sb("mx8", [128, 8], F32, es)
        vx8 = cx.sb("vx8", [128, 8], F32, es)
        sm = cx.sb("sm", [128, 8], F32, es)
        ps_t = [cx.ps("ps_t%d" % i, [128, 512], F32, es) for i in range(4)]
        ps_l = cx.ps("ps_l", [128, E], F32, es)
        ps_p = cx.ps("ps_p", [128, E], F32, es)
        ps_c = cx.ps("ps_c", [128, E], F32, es)

        cx.dma("sp", lambda e: e.dma_start(out=Wr[:], in_=rw.rearrange("(j p) e -> p j e", p=128)), w=["Wr"])
        cx.dma("sp", lambda e: e.dma_start(out=rbt[:], in_=rb), w=["rbt"])
        cx.dma("sp", lambda e: e.dma_start(out=b1s[:], in_=b1), w=["b1s"])
        for c in range(2 * KF):
            pst = ps_t[c % 4]
            cx.op("pe", lambda e, c=c, pst=pst: e.transpose(pst[:, 0:E], b1s[:, c * 128:(c + 1) * 128], cst["idf"][0:E, 0:E]),
                  r=["b1s", "c_idf"], w=[("ps_t", c % 4)])
            cx.op("dve", lambda e, c=c, pst=pst: e.tensor_copy(out=B1T[:, c, :], in_=pst[:, 0:E]),
                  r=[("ps_t", c % 4)], w=[("B1T", c)])
        cx.op("pool", lambda e: e.memset(zt[:], 0.0), w=["zt"])
        Xg4 = Xg.rearrange("(n p) d -> p n d", p=128)
        nrow = NS // 128 + 1
        for j in range(0, nrow, 4):
            cx.dma("sp", lambda e, j=j: e.dma_start(out=Xg4[:, j:min(j + 4, nrow), :], in_=zt[:, 0:min(4, nrow - j), :]),
                   r=["zt"], w=[("Xg0", j)])
        cx.op("dve", lambda e: e.memset(CNT[:], 0.0), w=["CNT"])
        zf = cx.sb("zf", [128, D], F32, es)
        cx.op("pool", lambda e: e.memset(zf[:], 0.0), w=["zf"])
        cx.dma("sp", lambda e: e.dma_start(out=Yg[NS:NS + 128, :], in_=zf[:]), r=["zf"], w=["Yg0"])
        cx.op("pool", lambda e: e.iota(EOFF[:], pattern=[[C, E]], base=1, channel_multiplier=0,
                                       allow_small_or_imprecise_dtypes=True), w=["EOFF"])
        xg0_keys = [("Xg0", j) for j in range(0, nrow, 4)]

        for i in range(NT):
            b = i % 2
            cx.dma("sp", lambda e, i=i, b=b: e.dma_start(out=xt[b][:], in_=h1[i * 128:(i + 1) * 128, :]),
                   w=[("xt", b)])
            cx.op("act", lambda e, b=b: e.copy(out=xb[b][:], in_=xt[b][:]), r=[("xt", b)], w=[("xb", b)])
            for g in range(KD // 4):
                for jj in range(4):
                    j = g * 4 + jj
                    cx.op("pe", lambda e, j=j, jj=jj, g=g, b=b: e.transpose(
                        ps_t[g][:, jj * 128:(jj + 1) * 128], xt[b][:, j * 128:(j + 1) * 128], cst["idf"][:]),
                        r=[("xt", b), "c_idf"], w=[("ps_t", g)])
                cx.op("dve", lambda e, g=g, b=b: e.tensor_copy(
                    out=xT[b][:, g * 4:(g + 1) * 4, :], in_=ps_t[g][:].rearrange("p (a m) -> p a m", a=4)),
                    r=[("ps_t", g)], w=[("xT", b, g)])
            for j in range(KD):
                cx.op("pe", lambda e, j=j, b=b: e.matmul(ps_l[:], lhsT=xT[b][:, j, :], rhs=Wr[:, j, :],
                                                       start=(j == 0), stop=False),
                      r=[("xT", b, j // 4), "Wr"], w=["ps_l"])
            cx.op("pe", lambda e: e.matmul(ps_l[:], lhsT=cst["onesf"][0:1, :], rhs=rbt[0:1, :], start=False, stop=True),
                  r=["c_onesf", "rbt"], w=["ps_l"])
            cx.op("dve", lambda e: e.tensor_copy(out=LGt[:], in_=ps_l[:]), r=["ps_l"], w=["LGt"])
            cx.op("dve", lambda e: e.max(out=mx8[:], in_=LGt[:]), r=["LGt"], w=["mx8"])
            cx.op("dve", lambda e: e.tensor_scalar(out=MKf[:], in0=LGt[:], scalar1=mx8[:, 3:4], scalar2=None,
                                                   op0=ALU.is_ge), r=["LGt", "mx8"], w=["MKf"])
            cx.op("dve", lambda e: e.tensor_scalar(out=sm[:, 0:1], in0=mx8[:, 0:1], scalar1=-1.0, scalar2=None,
                                                   op0=ALU.mult), r=["mx8"], w=["sm0"])
            cx.op("act", lambda e: e.activation(out=EX[:], in_=LGt[:], func=AF.Exp, bias=sm[:, 0:1], scale=1.0),
                  r=["LGt", "sm0"], w=["EX"])
            cx.op("dve", lambda e: e.tensor_tensor(out=EX[:], in0=EX[:], in1=MKf[:], op=ALU.mult),
                  r=["EX", "MKf"], w=["EX"])
            cx.op("dve", lambda e: e.reduce_sum(out=sm[:, 1:2], in_=EX[:], axis=AX.X), r=["EX"], w=["sm1"])
            cx.op("dve", lambda e: e.reciprocal(out=sm[:, 2:3], in_=sm[:, 1:2]), r=["sm1"], w=["sm2"])
            cx.op("dve", lambda e: e.tensor_scalar(out=GT[:], in0=EX[:], scalar1=sm[:, 2:3], scalar2=None,
                                                   op0=ALU.mult), r=["EX", "sm2"], w=["GT"])
            cx.op("pool", lambda e: e.tensor_copy(out=MKb[:], in_=MKf[:]), r=["MKf"], w=["MKb"])
            cx.op("pe", lambda e: e.matmul(ps_p[:], lhsT=cst["trib"][:], rhs=MKb[:], start=True, stop=True),
                  r=["c_trib", "MKb"], w=["ps_p"])
            cx.op("pe", lambda e: e.matmul(ps_c[:], lhsT=cst["onesb"][:], rhs=MKb[:], start=True, stop=True),
                  r=["c_onesb", "MKb"], w=["ps_c"])
            cx.op("dve", lambda e: e.tensor_tensor(out=POS[:], in0=ps_p[:], in1=CNT[:], op=ALU.add),
                  r=["ps_p", "CNT"], w=["POS"])
            cx.op("dve", lambda e: e.tensor_tensor(out=CNT[:], in0=ps_c[:], in1=CNT[:], op=ALU.add),
                  r=["ps_c", "CNT"], w=["CNT"])
            cx.op("dve", lambda e: e.scalar_tensor_tensor(out=TM[:], in0=POS[:], scalar=float(C) - 0.5, in1=MKf[:],
                                                          op0=ALU.is_lt, op1=ALU.mult),
                  r=["POS", "MKf"], w=["TM"])
            cx.op("dve", lambda e: e.tensor_tensor(out=V[:], in0=POS[:], in1=EOFF[:], op=ALU.add),
                  r=["POS", "EOFF"], w=["V"])
            cx.op("dve", lambda e: e.tensor_tensor(out=V[:], in0=V[:], in1=TM[:], op=ALU.mult),
                  r=["V", "TM"], w=["V"])
            cx.op("dve", lambda e: e.max(out=vx8[:], in_=V[:]), r=["V"], w=["vx8"])
            for k in range(4):
                cx.op("dve", lambda e, k=k: e.scalar_tensor_tensor(out=TM[:], in0=V[:], scalar=vx8[:, k:k + 1], in1=GT[:],
                                                                   op0=ALU.is_equal, op1=ALU.mult),
                      r=["V", "vx8", "GT", "TM"], w=["TM"])
                cx.op("dve", lambda e, k=k, i=i: e.reduce_sum(out=GK[:, i, k:k + 1], in_=TM[:], axis=AX.X),
                      r=["TM"], w=[("GK", i)])
            cx.op("dve", lambda e: e.tensor_scalar(out=sm[:, 4:8], in0=vx8[:, 0:4], scalar1=0.5, scalar2=float(NS + 1),
                                                   op0=ALU.is_lt, op1=ALU.mult), r=["vx8"], w=["sm4"])
            cx.op("dve", lambda e: e.tensor_tensor(out=sm[:, 4:8], in0=sm[:, 4:8], in1=vx8[:, 0:4], op=ALU.add),
                  r=["sm4", "vx8"], w=["sm4"])
            cx.op("dve", lambda e, i=i: e.tensor_scalar(out=SL[:, i, :], in0=sm[:, 4:8], scalar1=-1.0, scalar2=None,
                                                        op0=ALU.add), r=["sm4"], w=[("SL", i)])
            for k in range(4):
                cx.dma("pool", lambda e, k=k, i=i, b=b: e.indirect_dma_start(
                    out=Xg[:, :], out_offset=bass.IndirectOffsetOnAxis(ap=SL[:, i, k:k + 1], axis=0),
                    in_=xb[b][:, :], in_offset=None),
                    r=[("SL", i), ("xb", b)] + xg0_keys, w=[])
        cx.barrier()

    with ExitStack() as es:
        W1P = [cx.sb("W1P%d" % i, [128, KD, 2, 256], BF16, es) for i in range(4)]
        W2D = [cx.sb("W2D%d" % i, [128, KF, 512], BF16, es) for i in range(4)]
        B2F = [cx.sb("B2F%d" % i, [1, D], F32, es) for i in range(2)]
        XE = [cx.sb("XE%d" % i, [128, CS, D], BF16, es) for i in range(2)]
        XT = [cx.sb("XT%d" % i, [128, KD, C], BF16, es) for i in range(2)]
        AT = [cx.sb("AT%d" % i, [128, KF, C], BF16, es) for i in range(2)]
        YS = [cx.sb("YS%d" % i, [128, CS, 512], F32, es) for i in range(2)]
        B2B = [cx.sb("B2B%d" % i, [1, D], BF16, es) for i in range(2)]
        g1 = [cx.sb("g1_%d" % i, [128, C], F32, es) for i in range(2)]
        sg = [cx.sb("sg_%d" % i, [128, C], F32, es) for i in range(2)]
        l1 = [cx.sb("l1_%d" % i, [128, C], F32, es) for i in range(2)]
        ps_g = [cx.ps("ps_g%d" % i, [128, 512], F32, es) for i in range(2)]
        ps_h = [cx.ps("ps_h%d" % i, [128, 512], F32, es) for i in range(2)]
        ps_y = [cx.ps("ps_y%d" % i, [128, 512], F32, es) for i in range(2)]
        ps_x = [cx.ps("ps_x%d" % i, [128, 1024], BF16, es) for i in range(2)]
        w1v = w1.rearrange("e (j p) (g f) -> e p j g f", p=128, g=2)
        w2v = w2.rearrange("e (c p) d -> e p c d", p=128)
        Xgv = Xg[0:NS, :].rearrange("(e s p) d -> e p s d", p=128, s=CS)
        Ygv = Yg[0:NS, :].rearrange("(e s p) d -> e p s d", p=128, s=CS)
        NW1, NW2 = len(W1P), len(W2D)
        LA = 3
        seq = []
        for ex in range(E):
            for pbk in range(4):
                seq.append(("w1", ex, pbk))
            for db in range(D // 512):
                seq.append(("w2", ex, db))
        issued = [0]
        cnt = {"w1": 0, "w2": 0}
        slot_of = {}

        def prefetch(upto):
            while issued[0] < min(upto + 1, len(seq)):
                kind, ex, idx = seq[issued[0]]
                issued[0] += 1
                if kind == "w1":
                    wb = cnt["w1"] % NW1
                    cnt["w1"] += 1
                    slot_of[(kind, ex, idx)] = wb
                    for g in range(2):
                        cx.dma("pool", lambda e, ex=ex, idx=idx, wb=wb, g=g: e.dma_start(
                            out=W1P[wb][:, :, g, :], in_=w1v[ex][:, :, g, idx * 256:(idx + 1) * 256]),
                            w=[("W1P", wb, g)])
                else:
                    wb = cnt["w2"] % NW2
                    cnt["w2"] += 1
                    slot_of[(kind, ex, idx)] = wb
                    cx.dma("pool", lambda e, ex=ex, idx=idx, wb=wb: e.dma_start(
                        out=W2D[wb][:], in_=w2v[ex][:, :, idx * 512:(idx + 1) * 512]), w=[("W2D", wb)])

        def cp(en, out, in_, r, w):
            if en == "act":
                cx.op("act", lambda e: e.copy(out=out, in_=in_), r=r, w=w)
            else:
                cx.op(en, lambda e: e.tensor_copy(out=out, in_=in_), r=r, w=w)

        nchunk = 0
        ny = 0
        nx = 0
        t = 0
        cx.dma("sp", lambda e: e.dma_start(out=XE[0][:], in_=Xgv[0]), w=[("XE", 0)])
        for ex in range(E):
            eb = ex % 2
            if ex + 1 < E:
                cx.dma("sp", lambda e, ex=ex: e.dma_start(out=XE[(ex + 1) % 2][:], in_=Xgv[ex + 1]),
                       w=[("XE", (ex + 1) % 2)])
            cx.dma("sp", lambda e, ex=ex, eb=eb: e.dma_start(out=B2F[eb][:], in_=b2[ex:ex + 1, :]), w=[("B2F", eb)])
            cx.op("act", lambda e, eb=eb: e.copy(out=B2B[eb][:], in_=B2F[eb][:]), r=[("B2F", eb)], w=[("B2B", eb)])
            prefetch(t + LA)
            for s in range(CS):
                for hh in range(KD // 8):
                    pb = nx % 2
                    nx += 1
                    for jj in range(8):
                        j = hh * 8 + jj
                        cx.op("pe", lambda e, s=s, j=j, jj=jj, pb=pb, eb=eb: e.transpose(
                            ps_x[pb][:, jj * 128:(jj + 1) * 128], XE[eb][:, s, j * 128:(j + 1) * 128], cst["idb"][:]),
                            r=[("XE", eb), "c_idb"], w=[("ps_x", pb)])
                    cp("dve" if (nx % 2) else "act", XT[eb][:, hh * 8:(hh + 1) * 8, s * 128:(s + 1) * 128],
                       ps_x[pb][:].rearrange("p (a m) -> p a m", a=8), [("ps_x", pb)], [("XT", eb, s, hh)])
            xt_keys = [("XT", eb, s, hh) for s in range(CS) for hh in range(KD // 8)]
            for pbk in range(4):
                prefetch(t + LA)
                wb = slot_of[("w1", ex, pbk)]
                t += 1
                for cc in range(2):
                    c = pbk * 2 + cc
                    q = nchunk % 2
                    nchunk += 1
                    for j in range(KD):
                        cx.op("pe", lambda e, j=j, cc=cc, wb=wb, q=q, eb=eb: e.matmul(
                            ps_g[q][:, 0:C], lhsT=W1P[wb][:, j, 0, cc * 128:(cc + 1) * 128], rhs=XT[eb][:, j, :],
                            start=(j == 0), stop=(j == KD - 1)),
                            r=[("W1P", wb, 0)] + xt_keys, w=[("ps_g", q)])
                    for j in range(KD):
                        cx.op("pe", lambda e, j=j, cc=cc, wb=wb, q=q, eb=eb: e.matmul(
                            ps_h[q][:, 0:C], lhsT=W1P[wb][:, j, 1, cc * 128:(cc + 1) * 128], rhs=XT[eb][:, j, :],
                            start=(j == 0), stop=(j == KD - 1)),
                            r=[("W1P", wb, 1)] + xt_keys, w=[("ps_h", q)])
                    cx.op("dve", lambda e, c=c, q=q, ex=ex: e.tensor_scalar(
                        out=g1[q][:], in0=ps_g[q][:, 0:C], scalar1=B1T[:, c, ex:ex + 1], scalar2=7.0,
                        op0=ALU.add, op1=ALU.min), r=[("ps_g", q), ("B1T", c)], w=[("g1", q)])
                    cx.op("act", lambda e, q=q: e.activation(out=sg[q][:], in_=g1[q][:], func=AF.Sigmoid, scale=1.702),
                          r=[("g1", q)], w=[("sg", q)])
                    cx.op("dve", lambda e, c=c, q=q, ex=ex: e.tensor_scalar(
                        out=l1[q][:], in0=ps_h[q][:, 0:C], scalar1=B1T[:, KF + c, ex:ex + 1], scalar2=7.0,
                        op0=ALU.add, op1=ALU.min), r=[("ps_h", q), ("B1T", KF + c)], w=[("l1", q)])
                    cx.op("dve", lambda e, q=q: e.tensor_scalar(
                        out=l1[q][:], in0=l1[q][:], scalar1=-7.0, scalar2=1.0, op0=ALU.max, op1=ALU.add),
                        r=[("l1", q)], w=[("l1", q)])
                    cx.op("dve", lambda e, q=q: e.tensor_tensor(out=g1[q][:], in0=g1[q][:], in1=sg[q][:], op=ALU.mult),
                          r=[("g1", q), ("sg", q)], w=[("g1", q)])
                    cx.op("dve", lambda e, q=q, c=c, eb=eb: e.tensor_tensor(
                        out=AT[eb][:, c, :], in0=g1[q][:], in1=l1[q][:], op=ALU.mult),
                        r=[("g1", q), ("l1", q)], w=[("AT", eb, c)])
            at_keys = [("AT", eb, c) for c in range(KF)]
            for db in range(D // 512):
                prefetch(t + LA)
                wb = slot_of[("w2", ex, db)]
                t += 1
                yb = db % 2
                for s in range(CS):
                    q = ny % 2
                    ny += 1
                    for c in range(KF):
                        cx.op("pe", lambda e, c=c, s=s, q=q, wb=wb, eb=eb: e.matmul(
                            ps_y[q][:], lhsT=AT[eb][:, c, s * 128:(s + 1) * 128], rhs=W2D[wb][:, c, :],
                            start=(c == 0), stop=False),
                            r=[("W2D", wb)] + at_keys, w=[("ps_y", q)])
                    cx.op("pe", lambda e, q=q, db=db, eb=eb: e.matmul(
                        ps_y[q][:], lhsT=cst["onesb"][0:1, :], rhs=B2B[eb][0:1, db * 512:(db + 1) * 512],
                        start=False, stop=True), r=[("B2B", eb), "c_onesb"], w=[("ps_y", q)])
                    cx.op("act", lambda e, q=q, s=s, yb=yb: e.copy(out=YS[yb][:, s, :], in_=ps_y[q][:]),
                          r=[("ps_y", q)], w=[("YS", yb, s)])
                cx.dma("sp", lambda e, ex=ex, db=db, yb=yb: e.dma_start(
                    out=Ygv[ex][:, :, db * 512:(db + 1) * 512], in_=YS[yb][:]),
                    r=[("YS", yb, s) for s in range(CS)], w=[])
        cx.barrier()

    with ExitStack() as es:
        G2 = cx.sb("G2", [128, D], F32, es)
        Bt2 = cx.sb("Bt2", [128, D], F32, es)
        YK = [cx.sb("YK%d" % i, [128, 4, D], F32, es) for i in range(2)]
        xt = [cx.sb("cxt%d" % i, [128, D], F32, es) for i in range(2)]
        acc = [cx.sb("acc%d" % i, [128, D], F32, es) for i in range(2)]
        st = cx.sb("lnst", [128, 6 * (D // 512)], F32, es)
        mv = cx.sb("lnmv", [128, 2], F32, es)
        rstd = cx.sb("lnrs", [128, 1], F32, es)
        cx.dma("sp", lambda e: e.dma_start(out=G2[:], in_=lg.partition_broadcast(128)), w=["G2"])
        cx.dma("sp", lambda e: e.dma_start(out=Bt2[:], in_=lb.partition_broadcast(128)), w=["Bt2"])
        for b in range(2):
            cx.op("pool", lambda e, b=b: e.memset(YK[b][:], 0.0), w=[("YK", b, k) for k in range(4)])
        for i in range(NT):
            b = i % 2
            cx.dma("sp", lambda e, i=i, b=b: e.dma_start(out=xt[b][:], in_=h1[i * 128:(i + 1) * 128, :]), w=[("cxt", b)])
            for k in range(4):
                cx.dma("pool", lambda e, k=k, i=i, b=b: e.indirect_dma_start(
                    out=YK[b][:, k, :], out_offset=None, in_=Yg[:, :],
                    in_offset=bass.IndirectOffsetOnAxis(ap=SL[:, i, k:k + 1], axis=0)), r=[("SL", i)], w=[("YK", b, k)])
            cx.op("act", lambda e, b=b: e.activation(out=acc[b][:], in_=xt[b][:], func=AF.Copy, scale=float(alpha)),
                  r=[("cxt", b)], w=[("acc", b)])
            for k in range(4):
                cx.op("dve", lambda e, k=k, i=i, b=b: e.scalar_tensor_tensor(
                    out=acc[b][:], in0=YK[b][:, k, :], scalar=GK[:, i, k:k + 1], in1=acc[b][:],
                    op0=ALU.mult, op1=ALU.add), r=[("YK", b, k), ("GK", i), ("acc", b)], w=[("acc", b)])
            layer_norm_tile(cx, acc[b][:], xt[b][:], G2[:], Bt2[:], (st, mv, rstd), D,
                            ("acc", b), ("cxt", b), rk=["G2", "Bt2"])
            cx.dma("sp", lambda e, i=i, b=b: e.dma_start(out=out[i * 128:(i + 1) * 128, :], in_=xt[b][:]),
                   r=[("cxt", b)], w=[])
    cx.finish()
    print("moe program: ops", cx.nops, "waits", cx.nwaits)
    return nc

import math

NEG = -30000.0
T_LOC = 2048
HALO = 128


def cpy(cx, en, out, in_, r, w):
    if en == "act":
        return cx.op("act", lambda e: e.copy(out=out, in_=in_), r=r, w=w)
    return cx.op(en, lambda e: e.tensor_copy(out=out, in_=in_), r=r, w=w)


class Mix:
    def __init__(self, kind, n_in, lite=False):
        self.kind = kind
        self.lite = lite
        self.n_in = n_in
        D = 2048
        self.D = D
        self.KD = 16
        self.NTT = (T_LOC + HALO) // 128
        nc = self.nc = bass.Bass("TRN2", target_bir_lowering=False)
        dt = nc.dram_tensor
        self.hx = dt("hx", [T_LOC + HALO, D], F32, kind="ExternalInput").ap()
        self.flag = dt("flag", [128, 1], F32, kind="ExternalInput").ap()
        if not lite:
            self.mem = dt("mem", [256, D], F32, kind="ExternalInput").ap()
        self.w_in = dt("w_in", [D, n_in], F32, kind="ExternalInput").ap()
        self.b_in = dt("b_in", [1, n_in], F32, kind="ExternalInput").ap()
        if not lite:
            self.w_kv = dt("w_kv", [D, 1024], F32, kind="ExternalInput").ap()
            self.w_out = dt("w_out", [D, D], F32, kind="ExternalInput").ap()
            self.b_out = dt("b_out", [1, D], F32, kind="ExternalInput").ap()
            self.ln_g = dt("ln_g", [1, D], F32, kind="ExternalInput").ap()
            self.ln_b = dt("ln_b", [1, D], F32, kind="ExternalInput").ap()
            self.out = dt("out", [T_LOC, D], F32, kind="ExternalOutput").ap()
        self.U = dt("U", [T_LOC + HALO, n_in], F32, kind="Internal").ap()
        self.cx = Ctx(nc)
        self.cst = make_consts(self.cx)
        cx = self.cx
        self.flg = cx.sb("flg", [128, 1], F32)
        cx.dma("sp", lambda e: e.dma_start(out=self.flg[:], in_=self.flag), w=["flg"])
        self.mkT = cx.sb("mkT", [128, 4, 256], BF16)
        self.mv = cx.sb("mv", [128, 2, 512], BF16)

    def phase_inproj(self, tm_ranges=None, fm_sink=None):
        cx, cst, D, KD, NTT = self.cx, self.cst, self.D, self.KD, self.NTT
        n_in = self.n_in
        with ExitStack() as es:
            hT = cx.sb("hT", [128, KD, NTT * 128], BF16, es)
            memT = cx.sb("memT", [128, KD, 256], BF16, es)
            Wb = [cx.sb("Wb%d" % i, [128, KD, 512], BF16, es) for i in range(2)]
            bbc = cx.sb("bbc", [128, n_in], F32, es)
            ev = [cx.sb("iev%d" % i, [128, 512], F32, es) for i in range(3)]
            ps_o = [cx.ps("ips_o%d" % i, [128, 512], F32, es) for i in range(3)]
            es0 = ExitStack()
            xt = [cx.sb("ixt%d" % i, [128, D], F32, es0) for i in range(2)]
            xb = [cx.sb("ixb%d" % i, [128, D], BF16, es0) for i in range(2)]
            ps_x = [cx.ps("ips_x%d" % i, [128, 1024], BF16, es0) for i in range(2)]
            cx.dma("sp", lambda e: e.dma_start(out=bbc[:], in_=self.b_in.partition_broadcast(128)), w=["bbc"])
            nx = 0
            srcs = [(self.hx[t * 128:(t + 1) * 128, :], hT, t) for t in range(NTT)]
            if not self.lite:
                srcs += [(self.mem[t * 128:(t + 1) * 128, :], memT, t) for t in range(2)]
            for n, (src, dstT, t) in enumerate(srcs):
                b = n % 2
                cx.dma("sp", lambda e, src=src, b=b: e.dma_start(out=xt[b][:], in_=src), w=[("ixt", b)])
                cpy(cx, "act" if n % 2 else "dve", xb[b][:], xt[b][:], [("ixt", b)], [("ixb", b)])
                for hh in range(2):
                    pb = nx % 2
                    nx += 1
                    for jj in range(8):
                        j = hh * 8 + jj
                        cx.op("pe", lambda e, j=j, jj=jj, pb=pb, b=b: e.transpose(
                            ps_x[pb][:, jj * 128:(jj + 1) * 128], xb[b][:, j * 128:(j + 1) * 128], cst["idb"][:]),
                            r=[("ixb", b), "c_idb"], w=[("ips_x", pb)])
                    cpy(cx, "dve" if nx % 2 else "act", dstT[:, hh * 8:(hh + 1) * 8, t * 128:(t + 1) * 128],
                        ps_x[pb][:].rearrange("p (a m) -> p a m", a=8), [("ips_x", pb)],
                        [("T", id(dstT), t, hh)])
            cx.barrier()
            es0.close()
            hT_keys = [("T", id(hT), t, hh) for t in range(NTT) for hh in range(2)]
            memT_keys = [("T", id(memT), t, hh) for t in range(2) for hh in range(2)]
            self.hT_keys = hT_keys
            no = 0
            if not self.lite:
                wkv = self.w_kv.rearrange("(j p) n -> p j n", p=128)
            for blk in range(0 if self.lite else 2):
                wb = blk % 2
                cx.dma("pool", lambda e, blk=blk, wb=wb: e.dma_start(out=Wb[wb][:], in_=wkv[:, :, blk * 512:(blk + 1) * 512]),
                       w=[("Wb", wb)])
                if blk == 0:
                    for h in range(4):
                        q = no % 3
                        no += 1
                        for j in range(KD):
                            cx.op("pe", lambda e, j=j, h=h, q=q, wb=wb: e.matmul(
                                ps_o[q][:, 0:256], lhsT=Wb[wb][:, j, h * 128:(h + 1) * 128], rhs=memT[:, j, :],
                                start=(j == 0), stop=(j == KD - 1)), r=[("Wb", wb)] + memT_keys, w=[("ips_o", q)])
                        cpy(cx, "dve", self.mkT[:, h, :], ps_o[q][:, 0:256], [("ips_o", q)], [("mkT", h)])
                else:
                    for mc in range(2):
                        q = no % 3
                        no += 1
                        for j in range(KD):
                            cx.op("pe", lambda e, j=j, mc=mc, q=q, wb=wb: e.matmul(
                                ps_o[q][:], lhsT=memT[:, j, mc * 128:(mc + 1) * 128], rhs=Wb[wb][:, j, :],
                                start=(j == 0), stop=(j == KD - 1)), r=[("Wb", wb)] + memT_keys, w=[("ips_o", q)])
                        cpy(cx, "dve", self.mv[:, mc, :], ps_o[q][:], [("ips_o", q)], [("mv", mc)])
            win = self.w_in.rearrange("(j p) n -> p j n", p=128)
            if tm_ranges is None:
                tm_ranges = [(0, n_in)]
            blks = []
            for (a0, a1) in tm_ranges:
                c = a0
                while c < a1:
                    blks.append((c, min(512, a1 - c)))
                    c += 512
            nw = 0
            nev = 0
            for (c0, cw) in blks:
                wb = nw % 2
                nw += 1
                cx.dma("pool", lambda e, c0=c0, cw=cw, wb=wb: e.dma_start(out=Wb[wb][:, :, 0:cw], in_=win[:, :, c0:c0 + cw]),
                       w=[("Wb", wb)])
                for t in range(NTT):
                    q = no % 3
                    no += 1
                    for j in range(KD):
                        cx.op("pe", lambda e, j=j, t=t, q=q, wb=wb, cw=cw: e.matmul(
                            ps_o[q][:, 0:cw], lhsT=hT[:, j, t * 128:(t + 1) * 128], rhs=Wb[wb][:, j, 0:cw],
                            start=(j == 0), stop=(j == KD - 1)),
                            r=[("Wb", wb), ("T", id(hT), t, 0), ("T", id(hT), t, 1)], w=[("ips_o", q)])
                    eb = nev % 3
                    nev += 1
                    cx.op("dve", lambda e, q=q, eb=eb, c0=c0, cw=cw: e.tensor_tensor(
                        out=ev[eb][:, 0:cw], in0=ps_o[q][:, 0:cw], in1=bbc[:, c0:c0 + cw], op=ALU.add),
                        r=[("ips_o", q), "bbc"], w=[("iev", eb)])
                    cx.dma("sp", lambda e, t=t, eb=eb, c0=c0, cw=cw: e.dma_start(
                        out=self.U[t * 128:(t + 1) * 128, c0:c0 + cw], in_=ev[eb][:, 0:cw]),
                        r=[("iev", eb)], w=[])
            if fm_sink is not None:
                fm_sink(es, hT, win, Wb, ps_o, ev)
            cx.barrier()

    def attn_setup(self, es):
        cx = self.cx
        a = {}
        a["Sm"] = [cx.sb("aSm%d" % i, [128, 4, 256], F32, es) for i in range(2)]
        a["P"] = [cx.sb("aP%d" % i, [128, 4, 256], BF16, es) for i in range(2)]
        a["PT"] = [cx.sb("aPT%d" % i, [128, 8, 128], BF16, es) for i in range(2)]
        a["st"] = [cx.sb("ast%d" % i, [128, 4, 4], F32, es) for i in range(2)]
        a["ps_s"] = [cx.ps("aps_s%d" % i, [128, 4, 256], F32, es) for i in range(1)]
        a["ps_pt"] = cx.ps("aps_pt", [128, 8, 128], BF16, es)
        a["ps_o"] = cx.ps("aps_o", [128, 512], F32, es)
        a["n"] = 0
        self.att = a
        return a

    def attn_group(self, s_fn, s_keys, scale, mask, mask_keys, sink_ap, sink_keys, v_fn, v_keys, hd, out_ap, out_key):
        cx, cst, a = self.cx, self.cst, self.att
        b = a["n"] % 2
        a["n"] += 1
        ps = a["ps_s"][0]
        Sm, P, PT, st = a["Sm"][b], a["P"][b], a["PT"][b], a["st"][b]
        kS, kSm, kP, kPT, kst = ("aps_s", 0), ("aSm", b), ("aP", b), ("aPT", b), ("ast", b)
        for g in range(4):
            s_fn(ps, g, list(s_keys), [kS])
        if mask is not None:
            cx.op("dve", lambda e: e.tensor_tensor(out=Sm[:], in0=ps[:], in1=mask, op=ALU.add),
                  r=[kS] + list(mask_keys), w=[kSm])
        else:
            cpy(cx, "dve", Sm[:], ps[:], [kS], [kSm])
        cx.op("dve", lambda e: e.tensor_reduce(out=st[:, 0, :], in_=Sm[:], axis=AX.X, op=ALU.max), r=[kSm], w=[(kst, 0)])
        cx.op("dve", lambda e: e.tensor_scalar(out=st[:, 1, :], in0=st[:, 0, :], scalar1=-float(scale), scalar2=None,
                                               op0=ALU.mult), r=[(kst, 0)], w=[(kst, 1)])
        for g in range(4):
            cx.op("act", lambda e, g=g: e.activation(out=P[:, g, :], in_=Sm[:, g, :], func=AF.Exp,
                                                    bias=st[:, 1, g:g + 1], scale=float(scale)),
                  r=[kSm, (kst, 1)], w=[(kP, g)])
        pk = [(kP, g) for g in range(4)]
        cx.op("dve", lambda e: e.tensor_reduce(out=st[:, 2, :], in_=P[:], axis=AX.X, op=ALU.add), r=pk, w=[(kst, 2)])
        if sink_ap is not None:
            cx.op("dve", lambda e: e.tensor_tensor(out=st[:, 3, :], in0=st[:, 1, :], in1=sink_ap, op=ALU.add),
                  r=[(kst, 1)] + list(sink_keys), w=[(kst, 3)])
            cx.op("act", lambda e: e.activation(out=st[:, 3, :], in_=st[:, 3, :], func=AF.Exp), r=[(kst, 3)], w=[(kst, 3)])
            cx.op("dve", lambda e: e.tensor_tensor(out=st[:, 2, :], in0=st[:, 2, :], in1=st[:, 3, :], op=ALU.add),
                  r=[(kst, 2), (kst, 3)], w=[(kst, 2)])
        cx.op("dve", lambda e: e.reciprocal(out=st[:, 3, :], in_=st[:, 2, :]), r=[(kst, 2)], w=[(kst, 3)])
        for g in range(4):
            for kc in range(2):
                cx.op("pe", lambda e, g=g, kc=kc: e.transpose(a["ps_pt"][:, g * 2 + kc, :], P[:, g, kc * 128:(kc + 1) * 128],
                                                              cst["idb"][:]),
                      r=[(kP, g), "c_idb"], w=["aps_pt"])
        cpy(cx, "act", PT[:], a["ps_pt"][:], ["aps_pt"], [kPT])
        for g in range(4):
            for kc in range(2):
                cx.op("pe", lambda e, g=g, kc=kc: e.matmul(a["ps_o"][:, g * hd:(g + 1) * hd], lhsT=PT[:, g * 2 + kc, :],
                                                           rhs=v_fn(g, kc), start=(kc == 0), stop=(kc == 1)),
                      r=[kPT] + list(v_keys), w=["aps_o"])
        cx.op("dve", lambda e: e.tensor_tensor(
            out=out_ap.rearrange("p (g d) -> p g d", g=4),
            in0=a["ps_o"][:, 0:4 * hd].rearrange("p (g d) -> p g d", g=4),
            in1=st[:, 3, :].unsqueeze(2).to_broadcast([128, 4, hd]), op=ALU.mult),
            r=["aps_o", (kst, 3)], w=[out_key])

    def xattn_setup(self, es):
        cx = self.cx
        self.qmb = cx.sb("qmb", [128, 512], BF16, es)
        self.qmT = cx.sb("qmT", [128, 4, 128], BF16, es)
        self.ps_q = cx.ps("ps_q", [128, 4, 128], BF16, es)

    def xattn_tile(self, qm_ap, qm_keys, cat_ap, cat_key):
        cx, cst = self.cx, self.cst
        cpy(cx, "act", self.qmb[:], qm_ap, list(qm_keys), ["qmb"])
        for h in range(4):
            cx.op("pe", lambda e, h=h: e.transpose(self.ps_q[:, h, :], self.qmb[:, h * 128:(h + 1) * 128], cst["idb"][:]),
                  r=["qmb", "c_idb"], w=["ps_q"])
        cpy(cx, "dve", self.qmT[:], self.ps_q[:], ["ps_q"], ["qmT"])

        def s_fn(ps, g, rk, wk):
            cx.op("pe", lambda e: e.matmul(ps[:, g, :], lhsT=self.qmT[:, g, :], rhs=self.mkT[:, g, :], start=True, stop=True),
                  r=["qmT", ("mkT", g)] + rk, w=wk)

        self.attn_group(s_fn, [], 128 ** -0.5, None, [], None, [],
                        lambda g, kc: self.mv[:, kc, g * 128:(g + 1) * 128], [("mv", 0), ("mv", 1)], 128,
                        cat_ap, cat_key)

    def outproj_setup(self, es, nbuf=2):
        cx, D, KD = self.cx, self.D, self.KD
        o = {}
        o["Wo"] = cx.sb("Wo", [128, KD, D], BF16, es)
        wo = self.w_out.rearrange("(j p) n -> p j n", p=128)
        for q4 in range(4):
            cx.dma("pool", lambda e, q4=q4: e.dma_start(out=o["Wo"][:, q4 * 4:(q4 + 1) * 4, :], in_=wo[:, q4 * 4:(q4 + 1) * 4, :]),
                   w=[("Wo", q4)])
        o["G"] = cx.sb("oG", [128, D], F32, es)
        o["B"] = cx.sb("oB", [128, D], F32, es)
        o["bo"] = cx.sb("obo", [128, D], F32, es)
        cx.dma("sp", lambda e: e.dma_start(out=o["G"][:], in_=self.ln_g.partition_broadcast(128)), w=["oG"])
        cx.dma("sp", lambda e: e.dma_start(out=o["B"][:], in_=self.ln_b.partition_broadcast(128)), w=["oB"])
        cx.dma("sp", lambda e: e.dma_start(out=o["bo"][:], in_=self.b_out.partition_broadcast(128)), w=["obo"])
        o["catT"] = [cx.sb("catT%d" % i, [128, KD, 128], BF16, es) for i in range(nbuf)]
        o["nbuf"] = nbuf
        o["ht"] = [cx.sb("oht%d" % i, [128, D], F32, es) for i in range(nbuf)]
        o["acc"] = [cx.sb("oacc%d" % i, [128, D], F32, es) for i in range(nbuf)]
        o["st"] = cx.sb("ost", [128, 6 * (D // 512)], F32, es)
        o["mv"] = cx.sb("omv", [128, 2], F32, es)
        o["rs"] = cx.sb("ors", [128, 1], F32, es)
        o["ps_ct"] = cx.ps("ps_ct", [128, 8, 128], BF16, es)
        o["ps_op"] = [cx.ps("ps_op%d" % i, [128, 512], F32, es) for i in range(2)]
        o["n"] = 0
        o["nq"] = 0
        self.o = o

    def outproj_tile(self, t_own, cat_ap, cat_keys):
        cx, cst, o, D, KD = self.cx, self.cst, self.o, self.D, self.KD
        b = o["n"] % o["nbuf"]
        o["n"] += 1
        catT, ht, acc = o["catT"][b], o["ht"][b], o["acc"][b]
        cx.dma("sp", lambda e: e.dma_start(out=ht[:], in_=self.hx[HALO + t_own * 128: HALO + (t_own + 1) * 128, :]),
               w=[("oht", b)])
        for hh in range(2):
            for jj in range(8):
                j = hh * 8 + jj
                cx.op("pe", lambda e, j=j, jj=jj: e.transpose(o["ps_ct"][:, jj, :], cat_ap[:, j * 128:(j + 1) * 128], cst["idb"][:]),
                      r=list(cat_keys) + ["c_idb"], w=["ps_ct"])
            cpy(cx, "act" if hh else "dve", catT[:, hh * 8:(hh + 1) * 8, :], o["ps_ct"][:], ["ps_ct"], [("catT", b, hh)])
        cx.op("dve", lambda e: e.scalar_tensor_tensor(out=acc[:], in0=ht[:], scalar=float(DN_ALPHA), in1=o["bo"][:],
                                                      op0=ALU.mult, op1=ALU.add), r=[("oht", b), "obo"], w=[("oacc", b)])
        for db in range(4):
            q = o["nq"] % 2
            o["nq"] += 1
            for j in range(KD):
                cx.op("pe", lambda e, j=j, db=db, q=q: e.matmul(
                    o["ps_op"][q][:], lhsT=catT[:, j, :], rhs=o["Wo"][:, j, db * 512:(db + 1) * 512],
                    start=(j == 0), stop=(j == KD - 1)),
                    r=[("catT", b, 0), ("catT", b, 1), ("Wo", j // 4)], w=[("ps_op", q)])
            cx.op("dve", lambda e, db=db, q=q: e.tensor_tensor(
                out=acc[:, db * 512:(db + 1) * 512], in0=o["ps_op"][q][:], in1=acc[:, db * 512:(db + 1) * 512], op=ALU.add),
                r=[("ps_op", q), ("oacc", b)], w=[("oacc", b)])
        layer_norm_tile(cx, acc[:], ht[:], o["G"][:], o["B"][:], (o["st"], o["mv"], o["rs"]), D,
                        ("oacc", b), ("oht", b), rk=["oG", "oB"])
        cx.dma("sp", lambda e: e.dma_start(out=self.out[t_own * 128:(t_own + 1) * 128, :], in_=ht[:]),
               r=[("oht", b)], w=[])

import os
STAGE = int(os.environ.get('STAGE', '9'))

ATTN_IN = 2432


def build_swa():
    m = Mix("swa", ATTN_IN)
    nc, cx, cst = m.nc, m.cx, m.cst
    NTT = m.NTT
    pos = nc.dram_tensor("pos", [NTT, 128], I32, kind="ExternalInput").ap()
    sinks = nc.dram_tensor("sinks", [1, 24], F32, kind="ExternalInput").ap()
    m.phase_inproj()
    with ExitStack() as es:
        m.attn_setup(es)
        m.xattn_setup(es)
        m.outproj_setup(es)
        o = m.o
        COS = cx.sb("COS", [128, NTT, 8], F32, es)
        SIN = cx.sb("SIN", [128, NTT, 8], F32, es)
        SINKB = cx.sb("SINKB", [128, 24], F32, es)
        MASK = cx.sb("MASK", [128, 4, 256], F32, es)
        MASK0 = cx.sb("MASK0", [128, 4, 256], F32, es)
        cx.dma("sp", lambda e: e.dma_start(out=SINKB[:], in_=sinks.partition_broadcast(128)), w=["SINKB"])
        with ExitStack() as es2:
            pi_ = cx.sb("pi_", [NTT, 128], I32, es2)
            pf = cx.sb("pf", [NTT, 128], F32, es2)
            POSF = cx.sb("POSF", [128, NTT], F32, es2)
            INVF = cx.sb("INVF", [128, 8], F32, es2)
            ANG = cx.sb("ANG", [128, NTT, 8], F32, es2)
            AR = cx.sb("AR", [128, NTT, 8], F32, es2)
            pst = m.att["ps_o"][:, 0:NTT]
            cx.dma("sp", lambda e: e.dma_start(out=pi_[:], in_=pos), w=["pi_"])
            cx.op("dve", lambda e: e.tensor_copy(out=pf[:], in_=pi_[:]), r=["pi_"], w=["pf"])
            cx.op("pe", lambda e: e.transpose(pst, pf[:], cst["idf"][0:NTT, 0:NTT]), r=["pf", "c_idf"], w=["aps_o"])
            cx.op("dve", lambda e: e.tensor_copy(out=POSF[:], in_=pst), r=["aps_o"], w=["POSF"])
            for j in range(8):
                cx.op("pool", lambda e, j=j: e.memset(INVF[:, j:j + 1], float(500000.0 ** (-j / 8.0))), w=[("INVF", j)])
            cx.op("dve", lambda e: e.tensor_tensor(out=ANG[:], in0=POSF[:].unsqueeze(2).to_broadcast([128, NTT, 8]),
                                                   in1=INVF[:].unsqueeze(1).to_broadcast([128, NTT, 8]), op=ALU.mult),
                  r=["POSF"] + [("INVF", j) for j in range(8)], w=["ANG"])
            NI = cx.sb("NI", [128, NTT, 8], I32, es2)
            NF = cx.sb("NF", [128, NTT, 8], F32, es2)
            TW = cx.sb("TW", [128, NTT, 8], F32, es2)
            C1 = 6.28125
            C2 = 2 * math.pi - C1
            cx.op("dve", lambda e: e.tensor_scalar(out=NI[:], in0=ANG[:], scalar1=1.0 / (2 * math.pi), scalar2=None,
                                                   op0=ALU.mult), r=["ANG"], w=["NI"])
            cx.op("dve", lambda e: e.tensor_copy(out=NF[:], in_=NI[:]), r=["NI"], w=["NF"])
            cx.op("dve", lambda e: e.scalar_tensor_tensor(out=AR[:], in0=NF[:], scalar=-C1, in1=ANG[:],
                                                          op0=ALU.mult, op1=ALU.add), r=["NF", "ANG"], w=["AR"])
            cx.op("dve", lambda e: e.scalar_tensor_tensor(out=AR[:], in0=NF[:], scalar=-C2, in1=AR[:],
                                                          op0=ALU.mult, op1=ALU.add), r=["NF", "AR"], w=["AR"])

            def wrap(dst_key):
                cx.op("dve", lambda e: e.tensor_scalar(out=TW[:], in0=AR[:], scalar1=math.pi, scalar2=2 * math.pi,
                                                       op0=ALU.is_gt, op1=ALU.mult), r=["AR"], w=["TW"])
                cx.op("dve", lambda e: e.tensor_tensor(out=AR[:], in0=AR[:], in1=TW[:], op=ALU.subtract),
                      r=["AR", "TW"], w=["AR"])
                cx.op("dve", lambda e: e.tensor_scalar(out=TW[:], in0=AR[:], scalar1=-math.pi, scalar2=2 * math.pi,
                                                       op0=ALU.is_lt, op1=ALU.mult), r=["AR"], w=["TW"])
                cx.op("dve", lambda e: e.tensor_tensor(out=AR[:], in0=AR[:], in1=TW[:], op=ALU.add),
                      r=["AR", "TW"], w=["AR"])
            wrap(None)
            cx.op("act", lambda e: e.activation(out=SIN[:], in_=AR[:], func=AF.Sin), r=["AR"], w=["SIN"])
            cx.op("dve", lambda e: e.tensor_scalar(out=AR[:], in0=AR[:], scalar1=math.pi / 2, scalar2=None,
                                                   op0=ALU.add), r=["AR"], w=["AR"])
            wrap(None)
            cx.op("act", lambda e: e.activation(out=COS[:], in_=AR[:], func=AF.Sin), r=["AR"], w=["COS"])
            cx.barrier()
        cx.op("pool", lambda e: e.memset(MASK[:], 0.0), w=["MASK"])
        for g in range(4):
            cx.op("pool", lambda e, g=g: e.affine_select(out=MASK[:, g, :], in_=MASK[:, g, :], pattern=[[1, 256]],
                                                        compare_op=ALU.is_ge, fill=NEG, base=-1, channel_multiplier=-1),
                  r=["MASK"], w=["MASK"])
            cx.op("pool", lambda e, g=g: e.affine_select(out=MASK[:, g, :], in_=MASK[:, g, :], pattern=[[-1, 256]],
                                                        compare_op=ALU.is_ge, fill=NEG, base=128, channel_multiplier=1),
                  r=["MASK"], w=["MASK"])
        fm1 = cx.sb("fm1", [128, 1], F32, es)
        cx.op("dve", lambda e: e.tensor_scalar(out=fm1[:], in0=m.flg[:], scalar1=-1.0, scalar2=-NEG, op0=ALU.add, op1=ALU.mult),
              r=["flg"], w=["fm1"])
        cx.op("dve", lambda e: e.tensor_copy(out=MASK0[:], in_=MASK[:]), r=["MASK"], w=["MASK0"])
        cx.op("dve", lambda e: e.tensor_scalar(out=MASK0[:, :, 0:128], in0=MASK0[:, :, 0:128], scalar1=fm1[:, 0:1], scalar2=None,
                                               op0=ALU.add), r=["MASK0", "fm1"], w=["MASK0"])
        Ut = [cx.sb("Ut%d" % i, [128, ATTN_IN], F32, es) for i in range(2)]
        QR = cx.sb("QR", [128, 1536], BF16, es)
        K2 = cx.sb("K2", [128, 3, 64], BF16, es)
        qT = cx.sb("qT", [64, 24, 128], BF16, es)
        kT2 = [cx.sb("kT2_%d" % i, [64, 3, 128], BF16, es) for i in range(3)]
        Vr = [cx.sb("Vr%d" % i, [128, 192], BF16, es) for i in range(3)]
        cat = [cx.sb("cat%d" % i, [128, 2048], BF16, es) for i in range(2)]
        RA = [cx.sb("RA%d" % i, [128, 24, 8], F32, es) for i in range(4)]

        def rope(src3, nh, dst1, dst2, cos_b, sin_b, rk, wk):
            t1 = src3[:, :, 0:8]
            t2 = src3[:, :, 8:16]
            A, B_, C_, D_ = [RA[i][:, 0:nh, :] for i in range(4)]
            cx.op("dve", lambda e: e.tensor_tensor(out=A, in0=t1, in1=cos_b, op=ALU.mult), r=rk + ["COS"], w=[("RA", 0)])
            cx.op("dve", lambda e: e.tensor_tensor(out=B_, in0=t2, in1=sin_b, op=ALU.mult), r=rk + ["SIN"], w=[("RA", 1)])
            cx.op("dve", lambda e: e.tensor_tensor(out=C_, in0=t2, in1=cos_b, op=ALU.mult), r=rk + ["COS"], w=[("RA", 2)])
            cx.op("dve", lambda e: e.tensor_tensor(out=D_, in0=t1, in1=sin_b, op=ALU.mult), r=rk + ["SIN"], w=[("RA", 3)])
            cx.op("dve", lambda e: e.tensor_tensor(out=dst1, in0=A, in1=B_, op=ALU.subtract),
                  r=[("RA", 0), ("RA", 1)] + wk, w=wk)
            cx.op("dve", lambda e: e.tensor_tensor(out=dst2, in0=C_, in1=D_, op=ALU.add),
                  r=[("RA", 2), ("RA", 3)] + wk, w=wk)

        for t in range(NTT):
            ub = t % 2
            U_ = Ut[ub]
            cx.dma("sp", lambda e, t=t, U_=U_: e.dma_start(out=U_[:], in_=m.U[t * 128:(t + 1) * 128, :]), w=[("Ut", ub)])
            ku = [("Ut", ub)]
            kv3 = U_[:, 1536:1728].rearrange("p (h d) -> p h d", h=3)
            cpy(cx, "act", K2[:], kv3, ku, ["K2"])
            rope(kv3, 3, K2[:, :, 0:8], K2[:, :, 8:16],
                 COS[:, t, :].unsqueeze(1).to_broadcast([128, 3, 8]), SIN[:, t, :].unsqueeze(1).to_broadcast([128, 3, 8]),
                 ku, ["K2"])
            for g in range(3):
                cx.op("pe", lambda e, g=g: e.transpose(o["ps_ct"][0:64, g, :], K2[:, g, :], cst["idb"][:]),
                      r=["K2", "c_idb"], w=["ps_ct"])
            cpy(cx, "dve", kT2[t % 3][:], o["ps_ct"][0:64, 0:3, :], ["ps_ct"], [("kT2", t % 3)])
            cpy(cx, "act", Vr[t % 3][:], U_[:, 1728:1920], ku, [("Vr", t % 3)])
            if t == 0 or STAGE < 3:
                continue
            q3 = U_[:, 0:1536].rearrange("p (h d) -> p h d", h=24)
            QR3 = QR[:].rearrange("p (h d) -> p h d", h=24)
            cpy(cx, "act", QR[:], U_[:, 0:1536], ku, ["QR"])
            rope(q3, 24, QR3[:, :, 0:8], QR3[:, :, 8:16],
                 COS[:, t, :].unsqueeze(1).to_broadcast([128, 24, 8]), SIN[:, t, :].unsqueeze(1).to_broadcast([128, 24, 8]),
                 ku, ["QR"])
            for c0 in (0, 8, 16):
                for jj in range(8):
                    cx.op("pe", lambda e, c0=c0, jj=jj: e.transpose(o["ps_ct"][0:64, jj, :], QR[:, (c0 + jj) * 64:(c0 + jj + 1) * 64],
                                                                   cst["idb"][:]), r=["QR", "c_idb"], w=["ps_ct"])
                cpy(cx, "dve" if c0 != 8 else "act", qT[:, c0:c0 + 8, :], o["ps_ct"][0:64, :, :], ["ps_ct"], [("qT", c0)])
            cb = t % 2
            cat_t = cat[cb]
            if STAGE < 4:
                continue
            mask_t = MASK0 if t == 1 else MASK
            for g in range(3):
                for r0 in (0, 4):
                    h0 = g * 8 + r0

                    def s_fn(ps, gi, rk, wk, h0=h0, g=g, t=t):
                        h = h0 + gi
                        qk = [("qT", 0), ("qT", 8), ("qT", 16)]
                        cx.op("pe", lambda e: e.matmul(ps[:, gi, 0:128], lhsT=qT[:, h, :], rhs=kT2[(t - 1) % 3][:, g, :],
                                                       start=True, stop=True),
                              r=qk + [("kT2", (t - 1) % 3)] + rk, w=wk)
                        cx.op("pe", lambda e: e.matmul(ps[:, gi, 128:256], lhsT=qT[:, h, :], rhs=kT2[t % 3][:, g, :],
                                                       start=True, stop=True),
                              r=qk + [("kT2", t % 3)] + rk, w=wk)

                    m.attn_group(s_fn, [], 64 ** -0.5, mask_t[:], ["MASK", "MASK0"], SINKB[:, h0:h0 + 4], ["SINKB"],
                                 lambda gi, kc, g=g, t=t: Vr[(t - 1 + kc) % 3][:, g * 64:(g + 1) * 64],
                                 [("Vr", (t - 1) % 3), ("Vr", t % 3)], 64,
                                 cat_t[:, h0 * 64:(h0 + 4) * 64], ("cat", cb, h0 // 4))
            if STAGE < 5:
                continue
            m.xattn_tile(U_[:, 1920:2432], ku, cat_t[:, 1536:2048], ("cat", cb, 6))
            if STAGE < 6:
                continue
            m.outproj_tile(t - 1, cat_t, [("cat", cb, i) for i in range(7)])
    cx.finish()
    print("swa program: ops", cx.nops, "waits", cx.nwaits)
    return nc


CONF_IN = 3584


def build_conf():
    m = Mix("conf", CONF_IN)
    nc, cx, cst = m.nc, m.cx, m.cst
    NTT = m.NTT
    TT = NTT * 128
    dw_w = nc.dram_tensor("dw_w", [31, 1536], F32, kind="ExternalInput").ap()
    dw_b = nc.dram_tensor("dw_b", [1, 1536], F32, kind="ExternalInput").ap()
    cg = nc.dram_tensor("cln_g", [1, 1536], F32, kind="ExternalInput").ap()
    cb_ = nc.dram_tensor("cln_b", [1, 1536], F32, kind="ExternalInput").ap()
    Ymix = nc.dram_tensor("Ymix", [T_LOC, 1536], F32, kind="Internal").ap()

    def fm_sink(es, hT, win, Wb, ps_o, ev):
        KD = m.KD
        bsrc = cx.sb("bsrc", [28, 128], F32, es)
        BT = cx.sb("BT", [128, 28], F32, es)
        dws = cx.sb("dws", [31, 1536], F32, es)
        DWT = cx.sb("DWT", [128, 12, 32], F32, es)
        dbs = cx.sb("dbs", [12, 128], F32, es)
        DWB = cx.sb("DWB", [128, 12], F32, es)
        HC = [cx.sb("HC%d" % i, [128, TT], F32, es) for i in range(2)]
        Y = [cx.sb("Y%d" % i, [128, T_LOC], F32, es) for i in range(4)]
        sg = [cx.sb("csg%d" % i, [128, 512], F32, es) for i in range(2)]
        ps_tr = [cx.ps("ps_tr%d" % i, [128, 512], F32, es) for i in range(2)]
        cx.dma("sp", lambda e: e.dma_start(out=bsrc[:], in_=m.b_in.rearrange("o (c p) -> (o c) p", p=128)), w=["bsrc"])
        cx.dma("sp", lambda e: e.dma_start(out=dws[:], in_=dw_w), w=["dws"])
        cx.dma("sp", lambda e: e.dma_start(out=dbs[:], in_=dw_b.rearrange("o (c p) -> (o c) p", p=128)), w=["dbs"])
        cx.op("pe", lambda e: e.transpose(ps_tr[0][:, 0:28], bsrc[:], cst["idf"][0:28, 0:28]), r=["bsrc", "c_idf"], w=[("ps_tr", 0)])
        cpy(cx, "dve", BT[:], ps_tr[0][:, 0:28], [("ps_tr", 0)], ["BT"])
        cx.op("pe", lambda e: e.transpose(ps_tr[0][:, 0:12], dbs[:], cst["idf"][0:12, 0:12]), r=["dbs", "c_idf"], w=[("ps_tr", 0)])
        cpy(cx, "dve", DWB[:], ps_tr[0][:, 0:12], [("ps_tr", 0)], ["DWB"])
        for c in range(12):
            cx.op("pe", lambda e, c=c: e.transpose(ps_tr[1][:, c * 32:c * 32 + 31], dws[:, c * 128:(c + 1) * 128],
                                                   cst["idf"][0:31, 0:31]), r=["dws", "c_idf"], w=[("ps_tr", 1)])
        cpy(cx, "dve", DWT[:, :, 0:31], ps_tr[1][:, 0:384].rearrange("p (c k) -> p c k", k=32)[:, :, 0:31], [("ps_tr", 1)], ["DWT"])
        no = 0
        ntr = 0
        nst = 0
        for cb in range(3):
            cx.dma("pool", lambda e, cb=cb: e.dma_start(out=Wb[0][:], in_=win[:, :, cb * 512:(cb + 1) * 512]), w=[("Wb", 0)])
            cx.dma("pool", lambda e, cb=cb: e.dma_start(out=Wb[1][:], in_=win[:, :, 1536 + cb * 512:1536 + (cb + 1) * 512]),
                   w=[("Wb", 1)])
            for cc in range(4):
                c = cb * 4 + cc
                hb = c % 2
                H = HC[hb]
                for tb in range((TT + 511) // 512):
                    t0 = tb * 512
                    tw = min(512, TT - t0)
                    hk = []
                    for tt in range(t0 // 128, (t0 + tw) // 128):
                        hk += [("T", id(hT), tt, 0), ("T", id(hT), tt, 1)]
                    qa = no % 3
                    qb = (no + 1) % 3
                    no += 2
                    for j in range(KD):
                        cx.op("pe", lambda e, j=j, cc=cc, qa=qa, t0=t0, tw=tw: e.matmul(
                            ps_o[qa][:, 0:tw], lhsT=Wb[0][:, j, cc * 128:(cc + 1) * 128], rhs=hT[:, j, t0:t0 + tw],
                            start=(j == 0), stop=(j == KD - 1)), r=[("Wb", 0)] + hk, w=[("ips_o", qa)])
                    for j in range(KD):
                        cx.op("pe", lambda e, j=j, cc=cc, qb=qb, t0=t0, tw=tw: e.matmul(
                            ps_o[qb][:, 0:tw], lhsT=Wb[1][:, j, cc * 128:(cc + 1) * 128], rhs=hT[:, j, t0:t0 + tw],
                            start=(j == 0), stop=(j == KD - 1)), r=[("Wb", 1)] + hk, w=[("ips_o", qb)])
                    sb_ = no % 2
                    cx.op("act", lambda e, qb=qb, tw=tw, c=c, sb_=sb_: e.activation(
                        out=sg[sb_][:, 0:tw], in_=ps_o[qb][:, 0:tw], func=AF.Sigmoid, bias=BT[:, 12 + c:13 + c], scale=1.0),
                        r=[("ips_o", qb), "BT"], w=[("csg", sb_)])
                    cx.op("dve", lambda e, qa=qa, t0=t0, tw=tw, c=c, sb_=sb_, H=H: e.scalar_tensor_tensor(
                        out=H[:, t0:t0 + tw], in0=ps_o[qa][:, 0:tw], scalar=BT[:, c:c + 1], in1=sg[sb_][:, 0:tw],
                        op0=ALU.add, op1=ALU.mult), r=[("ips_o", qa), ("csg", sb_), "BT"], w=[("HC", hb)])
                cx.op("dve", lambda e, H=H: e.tensor_scalar(out=H[:, 0:128], in0=H[:, 0:128], scalar1=m.flg[:, 0:1], scalar2=None,
                                                            op0=ALU.mult), r=[("HC", hb), "flg"], w=[("HC", hb)])
                Yc = Y[cc]
                cx.op("dve", lambda e, H=H, Yc=Yc, c=c: e.tensor_scalar(
                    out=Yc[:], in0=H[:, 98:98 + T_LOC], scalar1=DWT[:, c, 0:1], scalar2=DWB[:, c:c + 1],
                    op0=ALU.mult, op1=ALU.add), r=[("HC", hb), "DWT", "DWB"], w=[("Y", cc)])
                for k in range(1, 31):
                    cx.op("dve", lambda e, H=H, Yc=Yc, c=c, k=k: e.scalar_tensor_tensor(
                        out=Yc[:], in0=H[:, 98 + k:98 + k + T_LOC], scalar=DWT[:, c, k:k + 1], in1=Yc[:],
                        op0=ALU.mult, op1=ALU.add), r=[("HC", hb), "DWT", ("Y", cc)], w=[("Y", cc)])
            for t in range(T_LOC // 128):
                pb = ntr % 2
                ntr += 1
                for cc in range(4):
                    cx.op("pe", lambda e, cc=cc, t=t, pb=pb: e.transpose(
                        ps_tr[pb][:, cc * 128:(cc + 1) * 128], Y[cc][:, t * 128:(t + 1) * 128], cst["idf"][:]),
                        r=[("Y", cc), "c_idf"], w=[("ps_tr", pb)])
                eb = nst % 3
                nst += 1
                cpy(cx, "act", ev[eb][:], ps_tr[pb][:], [("ps_tr", pb)], [("iev", eb)])
                cx.dma("sp", lambda e, t=t, cb=cb, eb=eb: e.dma_start(
                    out=Ymix[t * 128:(t + 1) * 128, cb * 512:(cb + 1) * 512], in_=ev[eb][:]), r=[("iev", eb)], w=[])

    m.phase_inproj(tm_ranges=[(3072, 3584)], fm_sink=fm_sink)
    with ExitStack() as es:
        m.attn_setup(es)
        m.xattn_setup(es)
        m.outproj_setup(es)
        CG = cx.sb("CG", [128, 1536], F32, es)
        CB = cx.sb("CB", [128, 1536], F32, es)
        cx.dma("sp", lambda e: e.dma_start(out=CG[:], in_=cg.partition_broadcast(128)), w=["CG"])
        cx.dma("sp", lambda e: e.dma_start(out=CB[:], in_=cb_.partition_broadcast(128)), w=["CB"])
        yt = [cx.sb("yt%d" % i, [128, 1536], F32, es) for i in range(2)]
        yn = [cx.sb("yn%d" % i, [128, 1536], F32, es) for i in range(2)]
        qm = [cx.sb("qm%d" % i, [128, 512], F32, es) for i in range(2)]
        cat = [cx.sb("cat%d" % i, [128, 2048], BF16, es) for i in range(2)]
        st = cx.sb("cst_", [128, 18], F32, es)
        mv = cx.sb("cmv_", [128, 2], F32, es)
        rs = cx.sb("crs_", [128, 1], F32, es)
        for t in range(T_LOC // 128):
            b = t % 2
            cx.dma("sp", lambda e, t=t, b=b: e.dma_start(out=yt[b][:], in_=Ymix[t * 128:(t + 1) * 128, :]), w=[("yt", b)])
            cx.dma("sp", lambda e, t=t, b=b: e.dma_start(out=qm[b][:], in_=m.U[HALO + t * 128:HALO + (t + 1) * 128, 3072:3584]),
                   w=[("qm", b)])
            layer_norm_tile(cx, yt[b][:], yn[b][:], CG[:], CB[:], (st, mv, rs), 1536, ("yt", b), ("yn", b), rk=["CG", "CB"])
            cx.op("act", lambda e, b=b: e.activation(out=cat[b][:, 0:1536], in_=yn[b][:], func=AF.Silu), r=[("yn", b)],
                  w=[("cat", b, 0)])
            m.xattn_tile(qm[b][:], [("qm", b)], cat[b][:, 1536:2048], ("cat", b, 1))
            m.outproj_tile(t, cat[b], [("cat", b, 0), ("cat", b, 1)])
    cx.finish()
    print("conf program: ops", cx.nops, "waits", cx.nwaits)
    return nc


SSD_IN = 4632


def build_ssd(mode="B"):
    A_ONLY = (mode == "A")
    m = Mix("ssd", SSD_IN, lite=A_ONLY)
    nc, cx, cst = m.nc, m.cx, m.cst
    NTT = m.NTT
    TT = NTT * 128
    NCH = T_LOC // 128
    dt_ = nc.dram_tensor
    conv_w = dt_("conv_w", [4, 2560], F32, kind="ExternalInput").ap()
    conv_b = dt_("conv_b", [1, 2560], F32, kind="ExternalInput").ap()
    dt_bias = dt_("dt_bias", [1, 24], F32, kind="ExternalInput").ap()
    a_log = dt_("a_log", [1, 24], F32, kind="ExternalInput").ap()
    if A_ONLY:
        S_end = dt_("S_end", [128, 1536], F32, kind="ExternalOutput").ap()
        a_tot = dt_("a_tot", [128, 24], F32, kind="ExternalOutput").ap()
    else:
        d_skip = dt_("d_skip", [1, 24], F32, kind="ExternalInput").ap()
        norm_g = dt_("norm_g", [1, 1536], F32, kind="ExternalInput").ap()
        Sp = dt_("Sp", [3, 128, 1536], F32, kind="ExternalInput").ap()
        Ap = dt_("Ap", [3, 128, 24], F32, kind="ExternalInput").ap()
    XTOK = dt_("XTOK", [T_LOC, 1536], F32, kind="Internal").ap()
    BTOK = dt_("BTOK", [T_LOC, 512], F32, kind="Internal").ap()
    BCT = dt_("BCT", [1024, T_LOC], BF16, kind="Internal").ap()

    def fm_sink(es, hT, win, Wb, ps_o, ev):
        KD = m.KD
        bsrc = cx.sb("bsrc", [36, 128], F32, es)
        BT = cx.sb("BT", [128, 36], F32, es)
        cws = cx.sb("cws", [4, 1280], F32, es)
        CWT = cx.sb("CWT", [128, 20, 4], F32, es)
        cbs = cx.sb("cbs", [20, 128], F32, es)
        CBT_ = cx.sb("CBT_", [128, 20], F32, es)
        PRE = [cx.sb("PRE%d" % i, [128, TT], F32, es) for i in range(1)]
        XC = [cx.sb("XC%d" % i, [128, T_LOC], F32, es) for i in range(1)]
        XS = [cx.sb("XS%d" % i, [128, T_LOC], F32, es) for i in range(4)]
        XSb = [cx.sb("XSb%d" % i, [128, T_LOC], BF16, es) for i in range(1)]
        ps_tr = [cx.ps("ps_tr%d" % i, [128, 512], F32, es) for i in range(2)]
        cx.dma("sp", lambda e: e.dma_start(out=bsrc[:], in_=m.b_in[:, 0:4608].rearrange("o (c p) -> (o c) p", p=128)), w=["bsrc"])
        cx.dma("sp", lambda e: e.dma_start(out=cbs[:], in_=conv_b.rearrange("o (c p) -> (o c) p", p=128)), w=["cbs"])
        cx.op("pe", lambda e: e.transpose(ps_tr[0][:, 0:36], bsrc[:], cst["idf"][0:36, 0:36]), r=["bsrc", "c_idf"], w=[("ps_tr", 0)])
        cpy(cx, "dve", BT[:], ps_tr[0][:, 0:36], [("ps_tr", 0)], ["BT"])
        cx.op("pe", lambda e: e.transpose(ps_tr[0][:, 0:20], cbs[:], cst["idf"][0:20, 0:20]), r=["cbs", "c_idf"], w=[("ps_tr", 0)])
        cpy(cx, "dve", CBT_[:], ps_tr[0][:, 0:20], [("ps_tr", 0)], ["CBT_"])
        for half in range(2):
            cx.dma("sp", lambda e, half=half: e.dma_start(out=cws[:], in_=conv_w[:, half * 1280:(half + 1) * 1280]), w=["cws"])
            for c in range(10):
                cx.op("pe", lambda e, c=c, half=half: e.transpose(ps_tr[1][:, (half * 10 + c) * 4:(half * 10 + c) * 4 + 4],
                                                                 cws[:, c * 128:(c + 1) * 128], cst["idf"][0:4, 0:4]),
                      r=["cws", "c_idf"], w=[("ps_tr", 1)])
        cpy(cx, "dve", CWT[:], ps_tr[1][:, 0:80].rearrange("p (c k) -> p c k", k=4), [("ps_tr", 1)], ["CWT"])
        no = 0
        ntr = 0
        nst = 0
        nxb = 0
        for cblk in range(5):
            wb = cblk % 2
            cx.dma("pool", lambda e, cblk=cblk, wb=wb: e.dma_start(
                out=Wb[wb][:], in_=win[:, :, 1536 + cblk * 512:1536 + (cblk + 1) * 512]), w=[("Wb", wb)])
            for cc in range(4):
                c = cblk * 4 + cc
                hb = 0
                P_ = PRE[hb]
                for tb in range((TT + 511) // 512):
                    t0 = tb * 512
                    tw = min(512, TT - t0)
                    hk = []
                    for tt in range(t0 // 128, (t0 + tw) // 128):
                        hk += [("T", id(hT), tt, 0), ("T", id(hT), tt, 1)]
                    qa = no % 3
                    no += 1
                    for j in range(KD):
                        cx.op("pe", lambda e, j=j, cc=cc, qa=qa, t0=t0, tw=tw, wb=wb: e.matmul(
                            ps_o[qa][:, 0:tw], lhsT=Wb[wb][:, j, cc * 128:(cc + 1) * 128], rhs=hT[:, j, t0:t0 + tw],
                            start=(j == 0), stop=(j == KD - 1)), r=[("Wb", wb)] + hk, w=[("ips_o", qa)])
                    cx.op("act", lambda e, qa=qa, t0=t0, tw=tw, c=c, P_=P_: e.activation(
                        out=P_[:, t0:t0 + tw], in_=ps_o[qa][:, 0:tw], func=AF.Identity, bias=BT[:, 12 + c:13 + c], scale=1.0),
                        r=[("ips_o", qa), "BT"], w=[("PRE", hb)])
                cx.op("dve", lambda e, P_=P_: e.tensor_scalar(out=P_[:, 0:128], in0=P_[:, 0:128], scalar1=m.flg[:, 0:1], scalar2=None,
                                                              op0=ALU.mult), r=[("PRE", hb), "flg"], w=[("PRE", hb)])
                X_ = XC[0]
                cx.op("dve", lambda e, P_=P_, X_=X_, c=c: e.tensor_scalar(
                    out=X_[:], in0=P_[:, 125:125 + T_LOC], scalar1=CWT[:, c, 0:1], scalar2=CBT_[:, c:c + 1],
                    op0=ALU.mult, op1=ALU.add), r=[("PRE", hb), "CWT", "CBT_"], w=[("XC", 0)])
                for k in range(1, 4):
                    cx.op("dve", lambda e, P_=P_, X_=X_, c=c, k=k: e.scalar_tensor_tensor(
                        out=X_[:], in0=P_[:, 125 + k:125 + k + T_LOC], scalar=CWT[:, c, k:k + 1], in1=X_[:],
                        op0=ALU.mult, op1=ALU.add), r=[("PRE", hb), "CWT", ("XC", 0)], w=[("XC", 0)])
                if cblk <= 3:
                    cx.op("act", lambda e, X_=X_, cc=cc: e.activation(out=XS[cc][:], in_=X_[:], func=AF.Silu),
                          r=[("XC", 0)], w=[("XS", cc)])
                if cblk >= 3:
                    xb_ = 0
                    nxb += 1
                    cx.op("act", lambda e, X_=X_, xb_=xb_: e.activation(out=XSb[xb_][:], in_=X_[:], func=AF.Silu),
                          r=[("XC", 0)], w=[("XSb", xb_)])
                    cx.dma("sp", lambda e, c=c, xb_=xb_: e.dma_start(out=BCT[(c - 12) * 128:(c - 11) * 128, :], in_=XSb[xb_][:]),
                           r=[("XSb", xb_)], w=[])
            if cblk <= 3:
                for t in range(NCH):
                    pb = ntr % 2
                    ntr += 1
                    for cc in range(4):
                        cx.op("pe", lambda e, cc=cc, t=t, pb=pb: e.transpose(
                            ps_tr[pb][:, cc * 128:(cc + 1) * 128], XS[cc][:, t * 128:(t + 1) * 128], cst["idf"][:]),
                            r=[("XS", cc), "c_idf"], w=[("ps_tr", pb)])
                    eb = nst % 3
                    nst += 1
                    cpy(cx, "dve", ev[eb][:], ps_tr[pb][:], [("ps_tr", pb)], [("iev", eb)])
                    if cblk < 3:
                        dst = XTOK[t * 128:(t + 1) * 128, cblk * 512:(cblk + 1) * 512]
                    else:
                        dst = BTOK[t * 128:(t + 1) * 128, :]
                    cx.dma("sp", lambda e, dst=dst, eb=eb: e.dma_start(out=dst, in_=ev[eb][:]), r=[("iev", eb)], w=[])

    m.phase_inproj(tm_ranges=([(4096, 4632)] if A_ONLY else [(0, 1536), (4096, 4632)]), fm_sink=fm_sink)

    with ExitStack() as es:
        m.attn_setup(es)
        m.xattn_setup(es)
        if not A_ONLY:
            m.outproj_setup(es, nbuf=1)
            o = m.o
            ps_ct, ps_op1, k_op1 = o["ps_ct"], o["ps_op"][1], ("ps_op", 1)
        else:
            ps_ct = cx.ps("ps_ct", [128, 8, 128], BF16, es)
            ps_op1 = cx.ps("ps_op1", [128, 512], F32, es)
            k_op1 = ("ps_op", 1)
        a = m.att
        ps_s = a["ps_s"][0]
        P_SNEW = ps_s[:, 0:2, :].rearrange("p a b -> p (a b)")[:, 0:384]
        P_YOFF = ps_s[:, 2:4, :].rearrange("p a b -> p (a b)")[:, 0:384]
        kS = ("aps_s", 0)
        P_CBT = a["ps_pt"][:].rearrange("p a b -> p (a b)").bitcast(F32)[:, 0:128]
        kCBT = "aps_pt"
        P_YDG = a["ps_o"][:, 0:384]
        kYDG = "aps_o"
        segq = m.ps_q[:].rearrange("p a b -> p (a b)").bitcast(F32)
        P_SEG = [segq[:, 0:128], ps_op1[:, 0:128]]
        kSEG = ["ps_q", k_op1]
        smallp = ps_ct[:].rearrange("p a b -> p (a b)").bitcast(F32)
        P_ACS = smallp[:, 0:24]
        P_ATOT = smallp[:, 32:56]
        kSM = "ps_ct"
        DTB = cx.sb("DTB", [128, 24], F32, es)
        ANEG = cx.sb("ANEG", [128, 24], F32, es)
        LT = cx.sb("LT", [128, 128], F32, es)
        NEGM = cx.sb("NEGM", [128, 128], F32, es)
        cx.dma("sp", lambda e: e.dma_start(out=DTB[:], in_=dt_bias.partition_broadcast(128)), w=["DTB"])
        cx.dma("sp", lambda e: e.dma_start(out=ANEG[:], in_=a_log.partition_broadcast(128)), w=["ANEG"])
        cx.op("act", lambda e: e.activation(out=ANEG[:], in_=ANEG[:], func=AF.Exp), r=["ANEG"], w=["ANEG"])
        cx.op("dve", lambda e: e.tensor_scalar(out=ANEG[:], in0=ANEG[:], scalar1=-1.0, scalar2=None, op0=ALU.mult),
              r=["ANEG"], w=["ANEG"])
        cx.op("pool", lambda e: e.affine_select(out=LT[:], in_=cst["onesf"][:], pattern=[[-1, 128]], compare_op=ALU.is_gt,
                                                fill=0.0, base=0, channel_multiplier=1), r=["c_onesf"], w=["LT"])
        cx.op("pool", lambda e: e.tensor_scalar(out=NEGM[:], in0=LT[:], scalar1=NEG, scalar2=None, op0=ALU.mult),
              r=["LT"], w=["NEGM"])
        S = cx.sb("S", [128, 1536], F32, es)
        S3 = S[:].rearrange("p (h d) -> p h d", h=24)
        SM = cx.sb("SM", [128, 8, 24], F32, es)
        xc = [cx.sb("xc%d" % i, [128, 1536], F32, es) for i in range(1)] * 2
        bc = [cx.sb("bc%d" % i, [128, 512], F32, es) for i in range(2)]
        dtr = [cx.sb("dtr%d" % i, [128, 536], F32, es) for i in range(2)]
        bcb = cx.sb("bcb", [128, 512], BF16, es)
        xdt = cx.sb("xdt", [128, 1536], F32, es)
        xwb = cx.sb("xwb", [128, 1536], BF16, es)
        if A_ONLY:
            ATS = cx.sb("ATS", [128, 24], F32, es)
            cx.op("dve", lambda e: e.memset(S[:], 0.0), w=["S"])
            cx.op("dve", lambda e: e.memset(ATS[:], 0.0), w=["ATS"])
        else:
            Sb = cx.sb("Sb", [128, 1536], BF16, es)
            DSK = cx.sb("DSK", [128, 24], F32, es)
            GN = cx.sb("GN", [128, 1536], F32, es)
            cx.dma("sp", lambda e: e.dma_start(out=DSK[:], in_=d_skip.partition_broadcast(128)), w=["DSK"])
            cx.dma("sp", lambda e: e.dma_start(out=GN[:], in_=norm_g.partition_broadcast(128)), w=["GN"])
            zc = [cx.sb("zc%d" % i, [128, 1536], F32, es) for i in range(1)] * 2
            BTc = [cx.sb("BTc%d" % i, [128, 4, 128], BF16, es) for i in range(2)]
            CTc = [cx.sb("CTc%d" % i, [128, 4, 128], BF16, es) for i in range(2)]
            xdb = cx.sb("xdb", [128, 1536], BF16, es)
            M1 = [cx.sb("M1_%d" % i, [128, 128], F32, es) for i in range(2)]
            DEC = [cx.sb("DEC%d" % i, [128, 128], F32, es) for i in range(2)]
            Gt = [cx.sb("Gt%d" % i, [128, 128], BF16, es) for i in range(2)]
            Yt = cx.sb("Yt", [128, 1536], F32, es)
            tmp = cx.sb("ytmp", [128, 1536], F32, es)
            cat = [cx.sb("cat%d" % i, [128, 2048], BF16, es) for i in range(1)] * 2
            ssq = cx.sb("ssq", [128, 2], F32, es)
            cx.dma("sp", lambda e: e.dma_start(out=S[:], in_=Sp[0]), w=["S"])
            for i in (1, 2):
                cx.dma("sp", lambda e, i=i: e.dma_start(out=tmp[:], in_=Sp[i]), w=["ytmp"])
                cx.dma("sp", lambda e, i=i: e.dma_start(out=SM[:, 0, :], in_=Ap[i]), w=[("SM", 0)])
                cx.op("act", lambda e: e.activation(out=SM[:, 0, :], in_=SM[:, 0, :], func=AF.Exp), r=[("SM", 0)], w=[("SM", 0)])
                cx.op("dve", lambda e: e.tensor_tensor(out=S3, in0=S3, in1=SM[:, 0, :].unsqueeze(2).to_broadcast([128, 24, 64]),
                                                       op=ALU.mult), r=["S", ("SM", 0)], w=["S"])
                cx.op("dve", lambda e: e.tensor_tensor(out=S[:], in0=S[:], in1=tmp[:], op=ALU.add), r=["S", "ytmp"], w=["S"])
            cpy(cx, "act", Sb[:], S[:], ["S"], ["Sb"])
        nhead = 0
        for c in range(NCH):
            b = c % 2
            t = c + 1
            cx.dma("sp", lambda e, c=c, b=b: e.dma_start(out=xc[b][:], in_=XTOK[c * 128:(c + 1) * 128, :]), w=[("xc", 0)])
            cx.dma("sp", lambda e, c=c, b=b: e.dma_start(out=bc[b][:], in_=BTOK[c * 128:(c + 1) * 128, :]), w=[("bc", b)])
            cx.dma("sp", lambda e, t=t, b=b: e.dma_start(out=dtr[b][:], in_=m.U[t * 128:(t + 1) * 128, 4096:4632]), w=[("dtr", b)])
            if not A_ONLY:
                cx.dma("sp", lambda e, t=t, b=b: e.dma_start(out=zc[b][:], in_=m.U[t * 128:(t + 1) * 128, 0:1536]), w=[("zc", 0)])
                cx.dma("sp", lambda e, c=c, b=b: e.dma_start(
                    out=BTc[b][:], in_=BCT[0:512, c * 128:(c + 1) * 128].rearrange("(g n) l -> n g l", n=128)), w=[("BTc", b)])
                cx.dma("sp", lambda e, c=c, b=b: e.dma_start(
                    out=CTc[b][:], in_=BCT[512:1024, c * 128:(c + 1) * 128].rearrange("(g n) l -> n g l", n=128)), w=[("CTc", b)])
            sm = lambda i: SM[:, i, :]
            k = lambda i: ("SM", i)
            cx.op("dve", lambda e: e.tensor_tensor(out=sm(0), in0=dtr[b][:, 0:24], in1=DTB[:], op=ALU.add),
                  r=[("dtr", b), "DTB"], w=[k(0)])
            cx.op("act", lambda e: e.activation(out=sm(0), in_=sm(0), func=AF.Exp), r=[k(0)], w=[k(0)])
            cx.op("dve", lambda e: e.tensor_scalar(out=sm(0), in0=sm(0), scalar1=1.0, scalar2=None, op0=ALU.add), r=[k(0)], w=[k(0)])
            cx.op("act", lambda e: e.activation(out=sm(1), in_=sm(0), func=AF.Ln), r=[k(0)], w=[k(1)])
            cx.op("dve", lambda e: e.tensor_tensor(out=sm(2), in0=sm(1), in1=ANEG[:], op=ALU.mult), r=[k(1), "ANEG"], w=[k(2)])
            cx.op("pe", lambda e: e.matmul(P_ACS, lhsT=cst["trif"][:], rhs=sm(2), start=True, stop=True),
                  r=["c_trif", k(2)], w=[kSM])
            cx.op("pe", lambda e: e.matmul(P_ATOT, lhsT=cst["onesf"][:], rhs=sm(2), start=True, stop=True),
                  r=["c_onesf", k(2)], w=[kSM])
            cpy(cx, "dve", sm(3), P_ACS, [kSM], [k(3)])
            cpy(cx, "dve", sm(4), P_ATOT, [kSM], [k(4)])
            cx.op("dve", lambda e: e.tensor_tensor(out=sm(5), in0=sm(4), in1=sm(3), op=ALU.subtract), r=[k(3), k(4)], w=[k(5)])
            cx.op("act", lambda e: e.activation(out=sm(5), in_=sm(5), func=AF.Exp), r=[k(5)], w=[k(5)])
            cx.op("act", lambda e: e.activation(out=sm(6), in_=sm(4), func=AF.Exp), r=[k(4)], w=[k(6)])
            if A_ONLY:
                cx.op("dve", lambda e: e.tensor_tensor(out=ATS[:], in0=ATS[:], in1=sm(4), op=ALU.add), r=["ATS", k(4)], w=["ATS"])
            else:
                cx.op("act", lambda e: e.activation(out=sm(7), in_=sm(3), func=AF.Exp), r=[k(3)], w=[k(7)])
            xc3 = xc[b][:].rearrange("p (h d) -> p h d", h=24)
            xdt3 = xdt[:].rearrange("p (h d) -> p h d", h=24)
            cx.op("dve", lambda e: e.tensor_tensor(out=xdt3, in0=xc3, in1=sm(1).unsqueeze(2).to_broadcast([128, 24, 64]),
                                                   op=ALU.mult), r=[("xc", 0), k(1)], w=["xdt"])
            cx.op("dve", lambda e: e.tensor_tensor(out=xwb[:].rearrange("p (h d) -> p h d", h=24), in0=xdt3,
                                                   in1=sm(5).unsqueeze(2).to_broadcast([128, 24, 64]), op=ALU.mult),
                  r=["xdt", k(5)], w=["xwb"])
            cpy(cx, "act", bcb[:], bc[b][:], [("bc", b)], ["bcb"])
            if not A_ONLY:
                cpy(cx, "act", xdb[:], xdt[:], ["xdt"], ["xdb"])
            for g in range(4):
                gs = slice(g * 384, (g + 1) * 384)
                cx.op("pe", lambda e, g=g, gs=gs: e.matmul(P_SNEW, lhsT=bcb[:, g * 128:(g + 1) * 128], rhs=xwb[:, gs],
                                                           start=True, stop=True), r=["bcb", "xwb"], w=[kS])
                if not A_ONLY:
                    cx.op("pe", lambda e, g=g, gs=gs: e.matmul(P_YOFF, lhsT=CTc[b][:, g, :], rhs=Sb[:, gs], start=True, stop=True),
                          r=[("CTc", b), "Sb"], w=[kS])
                    cx.op("pe", lambda e, g=g: e.matmul(P_CBT, lhsT=BTc[b][:, g, :], rhs=CTc[b][:, g, :], start=True, stop=True),
                          r=[("BTc", b), ("CTc", b)], w=[kCBT])
                    for r_ in range(6):
                        h = g * 6 + r_
                        hb = nhead % 2
                        nhead += 1
                        cx.op("pool", lambda e, h=h, hb=hb: e.tensor_scalar(out=M1[hb][:], in0=LT[:], scalar1=SM[:, 2, h:h + 1],
                                                                           scalar2=None, op0=ALU.mult),
                              r=["LT", k(2)], w=[("M1", hb)])
                        cx.op("pe", lambda e, hb=hb: e.matmul(P_SEG[hb], lhsT=M1[hb][:], rhs=cst["trif"][:], start=True, stop=False),
                              r=[("M1", hb), "c_trif"], w=[kSEG[hb]])
                        cx.op("pe", lambda e, hb=hb: e.matmul(P_SEG[hb], lhsT=cst["idf"][:], rhs=NEGM[:], start=False, stop=True),
                              r=["c_idf", "NEGM"], w=[kSEG[hb]])
                        cx.op("act", lambda e, hb=hb: e.activation(out=DEC[hb][:], in_=P_SEG[hb], func=AF.Exp),
                              r=[kSEG[hb]], w=[("DEC", hb)])
                        cx.op("dve", lambda e, hb=hb: e.tensor_tensor(out=Gt[hb][:], in0=DEC[hb][:], in1=P_CBT, op=ALU.mult),
                              r=[("DEC", hb), kCBT], w=[("Gt", hb)])
                        cx.op("pe", lambda e, hb=hb, h=h, r_=r_: e.matmul(
                            P_YDG[:, r_ * 64:(r_ + 1) * 64], lhsT=Gt[hb][:], rhs=xdb[:, h * 64:(h + 1) * 64], start=True, stop=True),
                            r=[("Gt", hb), "xdb"], w=[kYDG])
                    cx.op("dve", lambda e, g=g, gs=gs: e.tensor_tensor(
                        out=tmp[:, gs].rearrange("p (h d) -> p h d", h=6), in0=P_YOFF.rearrange("p (h d) -> p h d", h=6),
                        in1=SM[:, 7, g * 6:(g + 1) * 6].unsqueeze(2).to_broadcast([128, 6, 64]), op=ALU.mult),
                        r=[kS, k(7)], w=["ytmp"])
                    cx.op("dve", lambda e, gs=gs, g=g: e.tensor_tensor(out=Yt[:, gs], in0=tmp[:, gs], in1=P_YDG, op=ALU.add),
                          r=["ytmp", kYDG], w=["Yt"])
                S3g = S[:, gs].rearrange("p (h d) -> p h d", h=6)
                cx.op("dve", lambda e, g=g, S3g=S3g: e.tensor_tensor(
                    out=S3g, in0=S3g, in1=SM[:, 6, g * 6:(g + 1) * 6].unsqueeze(2).to_broadcast([128, 6, 64]), op=ALU.mult),
                    r=["S", k(6)], w=["S"])
                cx.op("dve", lambda e, gs=gs: e.tensor_tensor(out=S[:, gs], in0=S[:, gs], in1=P_SNEW, op=ALU.add),
                      r=["S", kS], w=["S"])
                if not A_ONLY:
                    cpy(cx, "act", Sb[:, gs], S[:, gs], ["S"], ["Sb"])
            if A_ONLY:
                continue
            ytk = ["Yt"]
            cx.op("dve", lambda e: e.tensor_tensor(out=tmp[:].rearrange("p (h d) -> p h d", h=24), in0=xc3,
                                                   in1=DSK[:].unsqueeze(2).to_broadcast([128, 24, 64]), op=ALU.mult),
                  r=[("xc", 0), "DSK", "ytmp"], w=["ytmp"])
            cx.op("dve", lambda e: e.tensor_tensor(out=Yt[:], in0=Yt[:], in1=tmp[:], op=ALU.add), r=["Yt", "ytmp"], w=["Yt"])
            cx.op("act", lambda e: e.activation(out=tmp[:], in_=zc[b][:], func=AF.Silu), r=[("zc", 0), "ytmp"], w=["ytmp"])
            cx.op("dve", lambda e: e.tensor_tensor(out=Yt[:], in0=Yt[:], in1=tmp[:], op=ALU.mult), r=["Yt", "ytmp"], w=["Yt"])
            cx.op("dve", lambda e: e.tensor_tensor(out=tmp[:], in0=Yt[:], in1=Yt[:], op=ALU.mult), r=["Yt", "ytmp"], w=["ytmp"])
            cx.op("dve", lambda e: e.reduce_sum(out=ssq[:, 0:1], in_=tmp[:], axis=AX.X), r=["ytmp"], w=["ssq"])
            cx.op("dve", lambda e: e.tensor_scalar(out=ssq[:, 0:1], in0=ssq[:, 0:1], scalar1=1.0 / 1536, scalar2=LN_EPS,
                                                   op0=ALU.mult, op1=ALU.add), r=["ssq"], w=["ssq"])
            cx.op("act", lambda e: e.sqrt(out=ssq[:, 0:1], in_=ssq[:, 0:1]), r=["ssq"], w=["ssq"])
            cx.op("dve", lambda e: e.reciprocal(out=ssq[:, 1:2], in_=ssq[:, 0:1]), r=["ssq"], w=["ssq"])
            cx.op("dve", lambda e: e.tensor_scalar(out=Yt[:], in0=Yt[:], scalar1=ssq[:, 1:2], scalar2=None, op0=ALU.mult),
                  r=["Yt", "ssq"], w=["Yt"])
            cx.op("dve", lambda e: e.tensor_tensor(out=cat[b][:, 0:1536], in0=Yt[:], in1=GN[:], op=ALU.mult),
                  r=["Yt", "GN"], w=[("cat", 0, 0)])
            m.xattn_tile(dtr[b][:, 24:536], [("dtr", b)], cat[b][:, 1536:2048], ("cat", 0, 1))
            m.outproj_tile(c, cat[b], [("cat", 0, 0), ("cat", 0, 1)])
        if A_ONLY:
            cx.dma("sp", lambda e: e.dma_start(out=S_end, in_=S[:]), r=["S"], w=[])
            cx.dma("sp", lambda e: e.dma_start(out=a_tot, in_=ATS[:]), r=["ATS"], w=[])
    cx.finish()
    print("ssd program", mode, ": ops", cx.nops, "waits", cx.nwaits)
    return nc

_PROGS = {}


def _prog(name):
    if name not in _PROGS:
        if name == "swa":
            _PROGS[name] = build_swa()
        elif name == "conf":
            _PROGS[name] = build_conf()
        elif name == "ssdA":
            _PROGS[name] = build_ssd("A")
        elif name == "ssdB":
            _PROGS[name] = build_ssd("B")
        elif name == "moe":
            _PROGS[name] = build_moe()
    return _PROGS[name]


NCORES = 8
SEGS = 4


def _run(name, in_maps):
    res = run_bass_kernel_spmd(_prog(name), in_maps, core_ids=list(range(NCORES)))
    return res.results


def _halo(h, positions=None):
    outs = []
    for c in range(NCORES):
        b, s = divmod(c, SEGS)
        s0 = s * T_LOC
        hx = np.zeros((T_LOC + HALO,) + h.shape[2:], h.dtype)
        hx[HALO:] = h[b, s0:s0 + T_LOC]
        if s > 0:
            hx[:HALO] = h[b, s0 - HALO:s0]
        outs.append(hx)
    return outs


def kernel(x, mem, positions,
           attn_w_in, attn_b_in, attn_sinks,
           ssd_w_in, ssd_b_in, ssd_conv_w, ssd_conv_b, ssd_dt_bias, ssd_a_log, ssd_d_skip, ssd_norm_g,
           conf_w_in, conf_b_in, conf_dw_w, conf_dw_b, conf_ln_g, conf_ln_b,
           mem_w_kv, w_out, b_out, ln1_g, ln1_b,
           router_w, router_b, moe_w1, moe_b1, moe_w2, moe_b2, ln2_g, ln2_b):
    f = lambda a: np.ascontiguousarray(np.asarray(a))
    h = f(x).astype(np.float32, copy=False)
    mem = f(mem)
    positions = f(positions).astype(np.int32, copy=False)
    flags = [np.full((128, 1), 1.0 if (c % SEGS) > 0 else 0.0, np.float32) for c in range(NCORES)]
    pos_h = _halo(positions[:, :, None])
    DEPTH = 4
    for i in range(DEPTH):
        kind, j = i % 3, i // 3
        hx = _halo(h)
        common = lambda c: dict(hx=hx[c], flag=flags[c], mem=f(mem[c // SEGS]), w_kv=f(mem_w_kv[i]), w_out=f(w_out[i]),
                                b_out=f(b_out[i])[None], ln_g=f(ln1_g[i])[None], ln_b=f(ln1_b[i])[None])
        if kind == 0:
            ins = [dict(common(c), w_in=f(attn_w_in[j]), b_in=f(attn_b_in[j])[None],
                        pos=np.ascontiguousarray(pos_h[c].reshape(17, 128)), sinks=f(attn_sinks[j])[None])
                   for c in range(NCORES)]
            r = _run("swa", ins)
        elif kind == 1:
            base = lambda c: dict(hx=hx[c], flag=flags[c], w_in=f(ssd_w_in[j]), b_in=f(ssd_b_in[j])[None],
                                  conv_w=f(ssd_conv_w[j]), conv_b=f(ssd_conv_b[j])[None],
                                  dt_bias=f(ssd_dt_bias[j])[None], a_log=f(ssd_a_log[j])[None])
            ra = _run("ssdA", [base(c) for c in range(NCORES)])
            ins = []
            for c in range(NCORES):
                b, s = divmod(c, SEGS)
                Sp = np.zeros((3, 128, 1536), np.float32)
                Ap = np.zeros((3, 128, 24), np.float32)
                for q in range(3):
                    sp = s - 3 + q
                    if sp >= 0:
                        Sp[q] = ra[b * SEGS + sp]["S_end"]
                        Ap[q] = ra[b * SEGS + sp]["a_tot"]
                d = dict(common(c), **base(c))
                d.update(Sp=Sp, Ap=Ap, d_skip=f(ssd_d_skip[j])[None], norm_g=f(ssd_norm_g[j])[None])
                ins.append(d)
            r = _run("ssdB", ins)
        else:
            ins = [dict(common(c), w_in=f(conf_w_in[j]), b_in=f(conf_b_in[j])[None], dw_w=f(conf_dw_w[j]),
                        dw_b=f(conf_dw_b[j])[None], cln_g=f(conf_ln_g[j])[None], cln_b=f(conf_ln_b[j])[None])
                   for c in range(NCORES)]
            r = _run("conf", ins)
        h1 = [r[c]["out"] for c in range(NCORES)]
        ins = [dict(h1=h1[c], router_w=f(router_w[i]), router_b=f(router_b[i])[None], w1=f(moe_w1[i]), b1=f(moe_b1[i]),
                    w2=f(moe_w2[i]), b2=f(moe_b2[i]), ln_g=f(ln2_g[i])[None], ln_b=f(ln2_b[i])[None]) for c in range(NCORES)]
        r = _run("moe", ins)
        h = np.stack([np.concatenate([r[b * SEGS + s]["out"] for s in range(SEGS)], 0) for b in range(2)], 0)
    return h.astype(np.float32, copy=False)
```

```python
import numpy as np
from contextlib import ExitStack
import concourse.bass as bass
import concourse.mybir as mybir
from concourse.bass_utils import run_bass_kernel_spmd

F32 = mybir.dt.float32
BF16 = mybir.dt.bfloat16
I32 = mybir.dt.int32
AF = mybir.ActivationFunctionType
ALU = mybir.AluOpType
AX = mybir.AxisListType

NDS = 8
SELF_SYNC = True


class _Ev:
    __slots__ = ("sem", "key", "val", "clock")

    def __init__(self, sem, key, val, clock):
        self.sem, self.key, self.val, self.clock = sem, key, val, clock


class _Eng:
    def __init__(self, name, eng, sem):
        self.name, self.eng, self.sem = name, eng, sem
        self.key = name
        self.count = 0
        self.clock = {}
        self.dsems = []
        self.dcnt = []
        self.devs = []
        self.dn = 0


class Ctx:
    def __init__(self, nc):
        self.nc = nc
        self.es = ExitStack()
        self.E = {}
        for name, e in (("pe", nc.tensor), ("dve", nc.vector), ("act", nc.scalar),
                        ("pool", nc.gpsimd), ("sp", nc.sync)):
            sem = self.es.enter_context(nc.semaphore("s_" + name))
            self.E[name] = _Eng(name, e, sem)
        for q in ("sp", "pool", "act"):
            Q = self.E[q]
            for i in range(NDS):
                Q.dsems.append(self.es.enter_context(nc.semaphore("d_%s%d" % (q, i))))
                Q.dcnt.append(0)
                Q.devs.append(None)
        self.dep = {}
        self.nwaits = 0
        self.nops = 0

    def sb(self, name, shape, dtype, es=None):
        self.uid = getattr(self, "uid", 0) + 1
        return (es or self.es).enter_context(self.nc.sbuf_tensor("%s_%d" % (name, self.uid), list(shape), dtype))

    def ps(self, name, shape, dtype, es=None):
        self.uid = getattr(self, "uid", 0) + 1
        return (es or self.es).enter_context(self.nc.psum_tensor("%s_%d" % (name, self.uid), list(shape), dtype))

    def _wait(self, E, ev):
        if ev is None:
            return
        if E.clock.get(ev.key, 0) >= ev.val:
            return
        E.eng.wait_ge(ev.sem, ev.val)
        self.nwaits += 1
        ck = E.clock
        for k, v in ev.clock.items():
            if ck.get(k, 0) < v:
                ck[k] = v
        if ck.get(ev.key, 0) < ev.val:
            ck[ev.key] = ev.val

    def _deps(self, E, r, w):
        dep = self.dep
        for k in r:
            d = dep.get(k)
            if d is not None and d[0] is not None:
                self._wait(E, d[0])
        for k in w:
            d = dep.get(k)
            if d is not None:
                if d[0] is not None:
                    self._wait(E, d[0])
                for ev in d[1]:
                    self._wait(E, ev)

    def _record(self, ev, r, w, prune_key=None):
        dep = self.dep
        for k in r:
            d = dep.get(k)
            if d is None:
                d = dep[k] = [None, []]
            if prune_key is not None:
                d[1] = [x for x in d[1] if x.key != prune_key]
            d[1].append(ev)
        for k in w:
            dep[k] = [ev, []]

    def op(self, en, fn, r=(), w=()):
        E = self.E[en]
        self._deps(E, r, w)
        inst = fn(E.eng)
        E.count += 1
        inst.then_inc(E.sem, 1)
        if en == "pe" or not SELF_SYNC:
            E.clock[E.key] = E.count
        ck = dict(E.clock)
        ck[E.key] = E.count
        ev = _Ev(E.sem, E.key, E.count, ck)
        self._record(ev, r, w, prune_key=E.key)
        self.nops += 1
        return ev

    def dma(self, q, fn, r=(), w=()):
        Q = self.E[q]
        slot = Q.dn % NDS
        Q.dn += 1
        self._wait(Q, Q.devs[slot])
        self._deps(Q, r, w)
        inst = fn(Q.eng)
        Q.dcnt[slot] += 16
        inst.then_inc(Q.dsems[slot], 16)
        key = "d_%s%d" % (q, slot)
        ck = dict(Q.clock)
        ck[key] = Q.dcnt[slot]
        ev = _Ev(Q.dsems[slot], key, Q.dcnt[slot], ck)
        Q.devs[slot] = ev
        self._record(ev, r, w)
        self.nops += 1
        return ev

    def coll(self, fn, r=(), w=()):
        Q = self.E["pool"]
        if not hasattr(self, "csem"):
            self.csem = self.es.enter_context(self.nc.semaphore("s_coll"))
            self.ccnt = 0
            self.cev = None
        self._wait(Q, self.cev)
        self._deps(Q, r, w)
        inst = fn(Q.eng)
        self.ccnt += 1
        inst.then_inc(self.csem, 1)
        ck = dict(Q.clock)
        ck["s_coll"] = self.ccnt
        ev = _Ev(self.csem, "s_coll", self.ccnt, ck)
        self.cev = ev
        self._record(ev, r, w)
        self.nops += 1
        return ev

    def barrier(self, engines=("pe", "dve", "act", "pool", "sp")):
        evs = []
        for n, E in self.E.items():
            if E.count > 0:
                ck = dict(E.clock)
                ck[E.key] = E.count
                evs.append(_Ev(E.sem, E.key, E.count, ck))
            for ev in E.devs:
                if ev is not None:
                    evs.append(ev)
        if getattr(self, "cev", None) is not None:
            evs.append(self.cev)
        for n in engines:
            E = self.E[n]
            for ev in evs:
                if ev.key == E.key and (n == "pe" or not SELF_SYNC):
                    continue
                self._wait(E, ev)

    def finish(self):
        self.barrier(engines=("sp",))


DN_ALPHA = 8 ** 0.25
LN_EPS = 1e-5


def make_consts(cx, es=None):
    nc = cx.nc
    c = {}
    c["idf"] = cx.sb("c_idf", [128, 128], F32, es)
    c["idb"] = cx.sb("c_idb", [128, 128], BF16, es)
    c["onesb"] = cx.sb("c_onesb", [128, 128], BF16, es)
    c["onesf"] = cx.sb("c_onesf", [128, 128], F32, es)
    c["trib"] = cx.sb("c_trib", [128, 128], BF16, es)
    c["trif"] = cx.sb("c_trif", [128, 128], F32, es)
    cx.op("pool", lambda e: e.memset(c["onesf"][:], 1.0), w=["c_onesf"])
    cx.op("pool", lambda e: e.memset(c["onesb"][:], 1.0), w=["c_onesb"])
    cx.op("pool", lambda e: e.affine_select(out=c["idf"][:], in_=c["onesf"][:], pattern=[[-1, 128]],
                                            compare_op=ALU.is_equal, fill=0.0, base=0, channel_multiplier=1),
          r=["c_onesf"], w=["c_idf"])
    cx.op("pool", lambda e: e.tensor_copy(out=c["idb"][:], in_=c["idf"][:]), r=["c_idf"], w=["c_idb"])
    cx.op("pool", lambda e: e.affine_select(out=c["trib"][:], in_=c["onesb"][:], pattern=[[1, 128]],
                                            compare_op=ALU.is_gt, fill=0.0, base=0, channel_multiplier=-1),
          r=["c_onesb"], w=["c_trib"])
    cx.op("pool", lambda e: e.affine_select(out=c["trif"][:], in_=c["onesf"][:], pattern=[[1, 128]],
                                            compare_op=ALU.is_ge, fill=0.0, base=0, channel_multiplier=-1),
          r=["c_onesf"], w=["c_trif"])
    return c


def layer_norm_tile(cx, acc, out, G, Bt, tmp_stats, D, tagr, tagw, eng="dve", rk=(), wk=()):
    nch = D // 512
    st, mv, rstd = tmp_stats
    for j in range(nch):
        cx.op("dve", lambda e, j=j: e.bn_stats(out=st[:, j * 6:(j + 1) * 6], in_=acc[:, j * 512:(j + 1) * 512]),
              r=[tagr], w=[("st", tagw, j)])
    cx.op("dve", lambda e: e.bn_aggr(out=mv[:, 0:2], in_=st[:, 0:nch * 6]),
          r=[("st", tagw, j) for j in range(nch)], w=[("mv", tagw)])
    cx.op("dve", lambda e: e.tensor_scalar(out=rstd[:, 0:1], in0=mv[:, 1:2], scalar1=LN_EPS, scalar2=None,
                                           op0=ALU.add), r=[("mv", tagw)], w=[("rstd", tagw)])
    cx.op("act", lambda e: e.sqrt(out=rstd[:, 0:1], in_=rstd[:, 0:1]), r=[("rstd", tagw)], w=[("rstd", tagw)])
    cx.op("dve", lambda e: e.reciprocal(out=rstd[:, 0:1], in_=rstd[:, 0:1]), r=[("rstd", tagw)], w=[("rstd", tagw)])
    cx.op("dve", lambda e: e.tensor_scalar(out=out, in0=acc, scalar1=mv[:, 0:1], scalar2=rstd[:, 0:1],
                                           op0=ALU.subtract, op1=ALU.mult),
          r=[tagr, ("mv", tagw), ("rstd", tagw)], w=[tagw])
    cx.op("dve", lambda e: e.tensor_tensor(out=out, in0=out, in1=G, op=ALU.mult), r=[tagw] + list(rk), w=[tagw])
    cx.op("dve", lambda e: e.tensor_tensor(out=out, in0=out, in1=Bt, op=ALU.add), r=[tagw] + list(rk), w=[tagw])


def build_moe(T=2048, D=2048, E=32, FF=1024, C=384, alpha=DN_ALPHA, ext=None):
    nc = ext["nc"] if ext else bass.Bass("TRN2", target_bir_lowering=False)
    NT = T // 128
    KD = D // 128
    KF = FF // 128
    NS = E * C
    CS = C // 128
    if ext is None:
        h1 = nc.dram_tensor("h1", [T, D], F32, kind="ExternalInput").ap()
        rw = nc.dram_tensor("router_w", [D, E], F32, kind="ExternalInput").ap()
        rb = nc.dram_tensor("router_b", [1, E], F32, kind="ExternalInput").ap()
        w1 = nc.dram_tensor("w1", [E, D, 2 * FF], F32, kind="ExternalInput").ap()
        b1 = nc.dram_tensor("b1", [E, 2 * FF], F32, kind="ExternalInput").ap()
        w2 = nc.dram_tensor("w2", [E, FF, D], F32, kind="ExternalInput").ap()
        b2 = nc.dram_tensor("b2", [E, D], F32, kind="ExternalInput").ap()
        lg = nc.dram_tensor("ln_g", [1, D], F32, kind="ExternalInput").ap()
        lb = nc.dram_tensor("ln_b", [1, D], F32, kind="ExternalInput").ap()
        out = nc.dram_tensor("out", [T, D], F32, kind="ExternalOutput").ap()
        Xg = nc.dram_tensor("Xg", [NS + 128, D], BF16, kind="Internal").ap()
        Yg = nc.dram_tensor("Yg", [NS + 128, D], F32, kind="Internal").ap()
        cx = Ctx(nc)
        cst = make_consts(cx)
        zero_fill = True
        tail_out = None
        pes = cx.es
    else:
        h1, rw, rb, w1, b1, w2, b2, lg, lb, out, Xg, Yg = [ext[k] for k in
            ("h1", "router_w", "router_b", "w1", "b1", "w2", "b2", "ln_g", "ln_b", "out", "Xg", "Yg")]
        cx, cst = ext["cx"], ext["cst"]
        zero_fill = ext["zero_fill"]
        tail_out = ext.get("tail_out")
        pes = ext["es"]
    SL = cx.sb("SL", [128, NT, 4], I32, pes)
    GK = cx.sb("GK", [128, NT, 4], F32, pes)
    B1T = cx.sb("B1T", [128, 2 * KF, E], F32, pes)

    with ExitStack() as es:
        Wr = cx.sb("Wr", [128, KD, E], F32, es)
        rbt = cx.sb("rbt", [1, E], F32, es)
        b1s = cx.sb("b1s", [E, 2 * FF], F32, es)
        zt = cx.sb("zt", [128, 4, D], BF16, es)
        CNT = cx.sb("CNT", [128, E], F32, es)
        EOFF = cx.sb("EOFF", [128, E], F32, es)
        xt = [cx.sb("xt%d" % i, [128, D], F32, es) for i in range(2)]
        xb = [cx.sb("xb%d" % i, [128, D], BF16, es) for i in range(2)]
        xT = [cx.sb("xT%d" % i, [128, KD, 128], F32, es) for i in range(2)]
        LGt = cx.sb("LGt", [128, E], F32, es)
        MKb = cx.sb("MKb", [128, E], BF16, es)
        MKf = cx.sb("MKf", [128, E], F32, es)
        GT = cx.sb("GT", [128, E], F32, es)
        EX = cx.sb("EX", [128, E], F32, es)
        V = cx.sb("V", [128, E], F32, es)
        POS = cx.sb("POS", [128, E], F32, es)
        TM = cx.sb("TM", [128, E], F32, es)
        mx8 = cx.sb("mx8", [128, 8], F32, es)
        vx8 = cx.sb("vx8", [128, 8], F32, es)
        sm = cx.sb("sm", [128, 8], F32, es)
        ps_t = [cx.ps("ps_t%d" % i, [128, 512], F32, es) for i in range(4)]
        ps_l = cx.ps("ps_l", [128, E], F32, es)
        ps_p = cx.ps("ps_p", [128, E], F32, es)
        ps_c = cx.ps("ps_c", [128, E], F32, es)

        cx.dma("act", lambda e: e.dma_start(out=Wr[:], in_=rw.rearrange("(j p) e -> p j e", p=128)), w=["Wr"])
        cx.dma("act", lambda e: e.dma_start(out=rbt[:], in_=rb), w=["rbt"])
        cx.dma("act", lambda e: e.dma_start(out=b1s[:], in_=b1), w=["b1s"])
        for c in range(2 * KF):
            pst = ps_t[c % 4]
            cx.op("pe", lambda e, c=c, pst=pst: e.transpose(pst[:, 0:E], b1s[:, c * 128:(c + 1) * 128], cst["idf"][0:E, 0:E]),
                  r=["b1s", "c_idf"], w=[("ps_t", c % 4)])
            cx.op("dve", lambda e, c=c, pst=pst: e.tensor_copy(out=B1T[:, c, :], in_=pst[:, 0:E]),
                  r=[("ps_t", c % 4)], w=[("B1T", c)])
        cx.op("pool", lambda e: e.memset(zt[:], 0.0), w=["zt"])
        Xg4 = Xg.rearrange("(n p) d -> p n d", p=128)
        nrow = NS // 128 + 1
        for j in (range(0, nrow, 4) if zero_fill else []):
            cx.dma("pool", lambda e, j=j: e.dma_start(out=Xg4[:, j:min(j + 4, nrow), :], in_=zt[:, 0:min(4, nrow - j), :]),
                   r=["zt"], w=[("Xg0", j)])
        cx.op("dve", lambda e: e.memset(CNT[:], 0.0), w=["CNT"])
        zf = cx.sb("zf", [128, D], F32, es)
        cx.op("pool", lambda e: e.memset(zf[:], 0.0), w=["zf"])
        if zero_fill:
            cx.dma("sp", lambda e: e.dma_start(out=Yg[NS:NS + 128, :], in_=zf[:]), r=["zf"], w=["Yg0"])
        cx.op("pool", lambda e: e.iota(EOFF[:], pattern=[[C, E]], base=1, channel_multiplier=0,
                                       allow_small_or_imprecise_dtypes=True), w=["EOFF"])
        xg0_keys = [("Xg0", j) for j in range(0, nrow, 4)]

        for i in range(NT):
            b = i % 2
            cx.dma("sp", lambda e, i=i, b=b: e.dma_start(out=xt[b][:], in_=h1[i * 128:(i + 1) * 128, :]),
                   w=[("xt", b)])
            cx.op("act", lambda e, b=b: e.copy(out=xb[b][:], in_=xt[b][:]), r=[("xt", b)], w=[("xb", b)])
            for g in range(KD // 4):
                for jj in range(4):
                    j = g * 4 + jj
                    cx.op("pe", lambda e, j=j, jj=jj, g=g, b=b: e.transpose(
                        ps_t[g][:, jj * 128:(jj + 1) * 128], xt[b][:, j * 128:(j + 1) * 128], cst["idf"][:]),
                        r=[("xt", b), "c_idf"], w=[("ps_t", g)])
                cx.op("dve", lambda e, g=g, b=b: e.tensor_copy(
                    out=xT[b][:, g * 4:(g + 1) * 4, :], in_=ps_t[g][:].rearrange("p (a m) -> p a m", a=4)),
                    r=[("ps_t", g)], w=[("xT", b, g)])
            for j in range(KD):
                cx.op("pe", lambda e, j=j, b=b: e.matmul(ps_l[:], lhsT=xT[b][:, j, :], rhs=Wr[:, j, :],
                                                       start=(j == 0), stop=False),
                      r=[("xT", b, j // 4), "Wr"], w=["ps_l"])
            cx.op("pe", lambda e: e.matmul(ps_l[:], lhsT=cst["onesf"][0:1, :], rhs=rbt[0:1, :], start=False, stop=True),
                  r=["c_onesf", "rbt"], w=["ps_l"])
            cx.op("dve", lambda e: e.tensor_copy(out=LGt[:], in_=ps_l[:]), r=["ps_l"], w=["LGt"])
            cx.op("dve", lambda e: e.max(out=mx8[:], in_=LGt[:]), r=["LGt"], w=["mx8"])
            cx.op("dve", lambda e: e.tensor_scalar(out=MKf[:], in0=LGt[:], scalar1=mx8[:, 3:4], scalar2=None,
                                                   op0=ALU.is_ge), r=["LGt", "mx8"], w=["MKf"])
            cx.op("dve", lambda e: e.tensor_scalar(out=sm[:, 0:1], in0=mx8[:, 0:1], scalar1=-1.0, scalar2=None,
                                                   op0=ALU.mult), r=["mx8"], w=["sm0"])
            cx.op("act", lambda e: e.activation(out=EX[:], in_=LGt[:], func=AF.Exp, bias=sm[:, 0:1], scale=1.0),
                  r=["LGt", "sm0"], w=["EX"])
            cx.op("dve", lambda e: e.tensor_tensor(out=EX[:], in0=EX[:], in1=MKf[:], op=ALU.mult),
                  r=["EX", "MKf"], w=["EX"])
            cx.op("dve", lambda e: e.reduce_sum(out=sm[:, 1:2], in_=EX[:], axis=AX.X), r=["EX"], w=["sm1"])
            cx.op("dve", lambda e: e.reciprocal(out=sm[:, 2:3], in_=sm[:, 1:2]), r=["sm1"], w=["sm2"])
            cx.op("dve", lambda e: e.tensor_scalar(out=GT[:], in0=EX[:], scalar1=sm[:, 2:3], scalar2=None,
                                                   op0=ALU.mult), r=["EX", "sm2"], w=["GT"])
            cx.op("pool", lambda e: e.tensor_copy(out=MKb[:], in_=MKf[:]), r=["MKf"], w=["MKb"])
            cx.op("pe", lambda e: e.matmul(ps_p[:], lhsT=cst["trib"][:], rhs=MKb[:], start=True, stop=True),
                  r=["c_trib", "MKb"], w=["ps_p"])
            cx.op("pe", lambda e: e.matmul(ps_c[:], lhsT=cst["onesb"][:], rhs=MKb[:], start=True, stop=True),
                  r=["c_onesb", "MKb"], w=["ps_c"])
            cx.op("dve", lambda e: e.tensor_tensor(out=POS[:], in0=ps_p[:], in1=CNT[:], op=ALU.add),
                  r=["ps_p", "CNT"], w=["POS"])
            cx.op("dve", lambda e: e.tensor_tensor(out=CNT[:], in0=ps_c[:], in1=CNT[:], op=ALU.add),
                  r=["ps_c", "CNT"], w=["CNT"])
            cx.op("dve", lambda e: e.scalar_tensor_tensor(out=TM[:], in0=POS[:], scalar=float(C) - 0.5, in1=MKf[:],
                                                          op0=ALU.is_lt, op1=ALU.mult),
                  r=["POS", "MKf"], w=["TM"])
            cx.op("dve", lambda e: e.tensor_tensor(out=V[:], in0=POS[:], in1=EOFF[:], op=ALU.add),
                  r=["POS", "EOFF"], w=["V"])
            cx.op("dve", lambda e: e.tensor_tensor(out=V[:], in0=V[:], in1=TM[:], op=ALU.mult),
                  r=["V", "TM"], w=["V"])
            cx.op("dve", lambda e: e.max(out=vx8[:], in_=V[:]), r=["V"], w=["vx8"])
            for k in range(4):
                cx.op("dve", lambda e, k=k: e.scalar_tensor_tensor(out=TM[:], in0=V[:], scalar=vx8[:, k:k + 1], in1=GT[:],
                                                                   op0=ALU.is_equal, op1=ALU.mult),
                      r=["V", "vx8", "GT", "TM"], w=["TM"])
                cx.op("dve", lambda e, k=k, i=i: e.reduce_sum(out=GK[:, i, k:k + 1], in_=TM[:], axis=AX.X),
                      r=["TM"], w=[("GK", i)])
            cx.op("dve", lambda e: e.tensor_scalar(out=sm[:, 4:8], in0=vx8[:, 0:4], scalar1=0.5, scalar2=float(NS + 1),
                                                   op0=ALU.is_lt, op1=ALU.mult), r=["vx8"], w=["sm4"])
            cx.op("dve", lambda e: e.tensor_tensor(out=sm[:, 4:8], in0=sm[:, 4:8], in1=vx8[:, 0:4], op=ALU.add),
                  r=["sm4", "vx8"], w=["sm4"])
            cx.op("dve", lambda e, i=i: e.tensor_scalar(out=SL[:, i, :], in0=sm[:, 4:8], scalar1=-1.0, scalar2=None,
                                                        op0=ALU.add), r=["sm4"], w=[("SL", i)])
            for k in range(4):
                cx.dma("pool", lambda e, k=k, i=i, b=b: e.indirect_dma_start(
                    out=Xg[:, :], out_offset=bass.IndirectOffsetOnAxis(ap=SL[:, i, k:k + 1], axis=0),
                    in_=xb[b][:, :], in_offset=None),
                    r=[("SL", i), ("xb", b)] + xg0_keys, w=[])
        cx.barrier()

    with ExitStack() as es:
        W1P = [cx.sb("W1P%d" % i, [128, KD, 2, 256], BF16, es) for i in range(4)]
        W2D = [cx.sb("W2D%d" % i, [128, KF, 512], BF16, es) for i in range(4)]
        B2F = [cx.sb("B2F%d" % i, [1, D], F32, es) for i in range(2)]
        XE = [cx.sb("XE%d" % i, [128, CS, D], BF16, es) for i in range(2)]
        XT = [cx.sb("XT%d" % i, [128, KD, C], BF16, es) for i in range(2)]
        AT = [cx.sb("AT%d" % i, [128, KF, C], BF16, es) for i in range(2)]
        YS = [cx.sb("YS%d" % i, [128, CS, 512], F32, es) for i in range(2)]
        B2B = [cx.sb("B2B%d" % i, [1, D], BF16, es) for i in range(2)]
        g1 = [cx.sb("g1_%d" % i, [128, C], F32, es) for i in range(2)]
        sg = [cx.sb("sg_%d" % i, [128, C], F32, es) for i in range(2)]
        l1 = [cx.sb("l1_%d" % i, [128, C], F32, es) for i in range(2)]
        ps_g = [cx.ps("ps_g%d" % i, [128, 512], F32, es) for i in range(2)]
        ps_h = [cx.ps("ps_h%d" % i, [128, 512], F32, es) for i in range(2)]
        ps_y = [cx.ps("ps_y%d" % i, [128, 512], F32, es) for i in range(2)]
        ps_x = [cx.ps("ps_x%d" % i, [128, 1024], BF16, es) for i in range(2)]
        w1v = w1.rearrange("e (j p) (g f) -> e p j g f", p=128, g=2)
        w2v = w2.rearrange("e (c p) d -> e p c d", p=128)
        Xgv = Xg[0:NS, :].rearrange("(e s p) d -> e p s d", p=128, s=CS)
        Ygv = Yg[0:NS, :].rearrange("(e s p) d -> e p s d", p=128, s=CS)
        NW1, NW2 = len(W1P), len(W2D)
        LA = 3
        seq = []
        for ex in range(E):
            for pbk in range(4):
                seq.append(("w1", ex, pbk))
            for db in range(D // 512):
                seq.append(("w2", ex, db))
        issued = [0]
        cnt = {"w1": 0, "w2": 0}
        slot_of = {}

        def prefetch(upto):
            while issued[0] < min(upto + 1, len(seq)):
                kind, ex, idx = seq[issued[0]]
                issued[0] += 1
                if kind == "w1":
                    wb = cnt["w1"] % NW1
                    cnt["w1"] += 1
                    slot_of[(kind, ex, idx)] = wb
                    for g in range(2):
                        cx.dma("pool", lambda e, ex=ex, idx=idx, wb=wb, g=g: e.dma_start(
                            out=W1P[wb][:, :, g, :], in_=w1v[ex][:, :, g, idx * 256:(idx + 1) * 256]),
                            w=[("W1P", wb, g)])
                else:
                    wb = cnt["w2"] % NW2
                    cnt["w2"] += 1
                    slot_of[(kind, ex, idx)] = wb
                    cx.dma("pool", lambda e, ex=ex, idx=idx, wb=wb: e.dma_start(
                        out=W2D[wb][:], in_=w2v[ex][:, :, idx * 512:(idx + 1) * 512]), w=[("W2D", wb)])

        def cp(en, out, in_, r, w):
            if en == "act":
                cx.op("act", lambda e: e.copy(out=out, in_=in_), r=r, w=w)
            else:
                cx.op(en, lambda e: e.tensor_copy(out=out, in_=in_), r=r, w=w)

        nchunk = 0
        ny = 0
        nxc = [0]
        t = 0

        def tr_burst(ex_, bi):
            eb_ = ex_ % 2
            s_, hh = divmod(bi, KD // 8)
            pb = nxc[0] % 2
            nxc[0] += 1
            for jj in range(8):
                j = hh * 8 + jj
                cx.op("pe", lambda e, j=j, jj=jj: e.transpose(
                    ps_x[pb][:, jj * 128:(jj + 1) * 128], XE[eb_][:, s_, j * 128:(j + 1) * 128], cst["idb"][:]),
                    r=[("XE", eb_), "c_idb"], w=[("ps_x", pb)])
            cp("dve" if (nxc[0] % 2) else "act", XT[eb_][:, hh * 8:(hh + 1) * 8, s_ * 128:(s_ + 1) * 128],
               ps_x[pb][:].rearrange("p (a m) -> p a m", a=8), [("ps_x", pb)], [("XT", eb_, s_, hh)])

        cx.dma("sp", lambda e: e.dma_start(out=XE[0][:], in_=Xgv[0]), w=[("XE", 0)])
        for ex in range(E):
            eb = ex % 2
            if ex + 1 < E:
                cx.dma("sp", lambda e, ex=ex: e.dma_start(out=XE[(ex + 1) % 2][:], in_=Xgv[ex + 1]),
                       w=[("XE", (ex + 1) % 2)])
            cx.dma("sp", lambda e, ex=ex, eb=eb: e.dma_start(out=B2F[eb][:], in_=b2[ex:ex + 1, :]), w=[("B2F", eb)])
            cx.op("act", lambda e, eb=eb: e.copy(out=B2B[eb][:], in_=B2F[eb][:]), r=[("B2F", eb)], w=[("B2B", eb)])
            prefetch(t + LA)
            if ex == 0:
                for bi in range(CS * (KD // 8)):
                    tr_burst(0, bi)
            xt_keys = [("XT", eb, s, hh) for s in range(CS) for hh in range(KD // 8)]
            for pbk in range(4):
                prefetch(t + LA)
                wb = slot_of[("w1", ex, pbk)]
                t += 1
                for cc in range(2):
                    c = pbk * 2 + cc
                    q = nchunk % 2
                    nchunk += 1
                    for j in range(KD):
                        cx.op("pe", lambda e, j=j, cc=cc, wb=wb, q=q, eb=eb: e.matmul(
                            ps_g[q][:, 0:C], lhsT=W1P[wb][:, j, 0, cc * 128:(cc + 1) * 128], rhs=XT[eb][:, j, :],
                            start=(j == 0), stop=(j == KD - 1)),
                            r=[("W1P", wb, 0)] + xt_keys, w=[("ps_g", q)])
                    for j in range(KD):
                        cx.op("pe", lambda e, j=j, cc=cc, wb=wb, q=q, eb=eb: e.matmul(
                            ps_h[q][:, 0:C], lhsT=W1P[wb][:, j, 1, cc * 128:(cc + 1) * 128], rhs=XT[eb][:, j, :],
                            start=(j == 0), stop=(j == KD - 1)),
                            r=[("W1P", wb, 1)] + xt_keys, w=[("ps_h", q)])
                    cx.op("dve", lambda e, c=c, q=q, ex=ex: e.tensor_scalar(
                        out=g1[q][:], in0=ps_g[q][:, 0:C], scalar1=B1T[:, c, ex:ex + 1], scalar2=7.0,
                        op0=ALU.add, op1=ALU.min), r=[("ps_g", q), ("B1T", c)], w=[("g1", q)])
                    cx.op("act", lambda e, q=q: e.activation(out=sg[q][:], in_=g1[q][:], func=AF.Sigmoid, scale=1.702),
                          r=[("g1", q)], w=[("sg", q)])
                    cx.op("dve", lambda e, c=c, q=q, ex=ex: e.tensor_scalar(
                        out=l1[q][:], in0=ps_h[q][:, 0:C], scalar1=B1T[:, KF + c, ex:ex + 1], scalar2=7.0,
                        op0=ALU.add, op1=ALU.min), r=[("ps_h", q), ("B1T", KF + c)], w=[("l1", q)])
                    cx.op("dve", lambda e, q=q: e.tensor_scalar(
                        out=l1[q][:], in0=l1[q][:], scalar1=-7.0, scalar2=1.0, op0=ALU.max, op1=ALU.add),
                        r=[("l1", q)], w=[("l1", q)])
                    cx.op("dve", lambda e, q=q: e.tensor_tensor(out=g1[q][:], in0=g1[q][:], in1=sg[q][:], op=ALU.mult),
                          r=[("g1", q), ("sg", q)], w=[("g1", q)])
                    cx.op("dve", lambda e, q=q, c=c, eb=eb: e.tensor_tensor(
                        out=AT[eb][:, c, :], in0=g1[q][:], in1=l1[q][:], op=ALU.mult),
                        r=[("g1", q), ("l1", q)], w=[("AT", eb, c)])
            at_keys = [("AT", eb, c) for c in range(KF)]
            nb_ = CS * (KD // 8)
            for db in range(D // 512):
                if ex + 1 < E:
                    for bi in range(db * nb_ // 4, (db + 1) * nb_ // 4):
                        tr_burst(ex + 1, bi)
                prefetch(t + LA)
                wb = slot_of[("w2", ex, db)]
                t += 1
                yb = db % 2
                for s in range(CS):
                    q = ny % 2
                    ny += 1
                    for c in range(KF):
                        cx.op("pe", lambda e, c=c, s=s, q=q, wb=wb, eb=eb: e.matmul(
                            ps_y[q][:], lhsT=AT[eb][:, c, s * 128:(s + 1) * 128], rhs=W2D[wb][:, c, :],
                            start=(c == 0), stop=False),
                            r=[("W2D", wb)] + at_keys, w=[("ps_y", q)])
                    cx.op("pe", lambda e, q=q, db=db, eb=eb: e.matmul(
                        ps_y[q][:], lhsT=cst["onesb"][0:1, :], rhs=B2B[eb][0:1, db * 512:(db + 1) * 512],
                        start=False, stop=True), r=[("B2B", eb), "c_onesb"], w=[("ps_y", q)])
                    cx.op("act", lambda e, q=q, s=s, yb=yb: e.copy(out=YS[yb][:, s, :], in_=ps_y[q][:]),
                          r=[("ps_y", q)], w=[("YS", yb, s)])
                cx.dma("sp", lambda e, ex=ex, db=db, yb=yb: e.dma_start(
                    out=Ygv[ex][:, :, db * 512:(db + 1) * 512], in_=YS[yb][:]),
                    r=[("YS", yb, s) for s in range(CS)], w=[])
        cx.barrier()

    with ExitStack() as es:
        G2 = cx.sb("G2", [128, D], F32, es)
        Bt2 = cx.sb("Bt2", [128, D], F32, es)
        YK = [cx.sb("YK%d" % i, [128, 4, D], F32, es) for i in range(2)]
        xt = [cx.sb("cxt%d" % i, [128, D], F32, es) for i in range(2)]
        acc = [cx.sb("acc%d" % i, [128, D], F32, es) for i in range(2)]
        st = cx.sb("lnst", [128, 6 * (D // 512)], F32, es)
        mv = cx.sb("lnmv", [128, 2], F32, es)
        rstd = cx.sb("lnrs", [128, 1], F32, es)
        cx.dma("sp", lambda e: e.dma_start(out=G2[:], in_=lg.partition_broadcast(128)), w=["G2"])
        cx.dma("sp", lambda e: e.dma_start(out=Bt2[:], in_=lb.partition_broadcast(128)), w=["Bt2"])
        for b in range(2):
            cx.op("pool", lambda e, b=b: e.memset(YK[b][:], 0.0), w=[("YK", b, k) for k in range(4)])
        for i in range(NT):
            b = i % 2
            cx.dma("sp", lambda e, i=i, b=b: e.dma_start(out=xt[b][:], in_=h1[i * 128:(i + 1) * 128, :]), w=[("cxt", b)])
            for k in range(4):
                cx.dma("pool", lambda e, k=k, i=i, b=b: e.indirect_dma_start(
                    out=YK[b][:, k, :], out_offset=None, in_=Yg[:, :],
                    in_offset=bass.IndirectOffsetOnAxis(ap=SL[:, i, k:k + 1], axis=0)), r=[("SL", i)], w=[("YK", b, k)])
            cx.op("act", lambda e, b=b: e.activation(out=acc[b][:], in_=xt[b][:], func=AF.Copy, scale=float(alpha)),
                  r=[("cxt", b)], w=[("acc", b)])
            for k in range(4):
                cx.op("dve", lambda e, k=k, i=i, b=b: e.scalar_tensor_tensor(
                    out=acc[b][:], in0=YK[b][:, k, :], scalar=GK[:, i, k:k + 1], in1=acc[b][:],
                    op0=ALU.mult, op1=ALU.add), r=[("YK", b, k), ("GK", i), ("acc", b)], w=[("acc", b)])
            layer_norm_tile(cx, acc[b][:], xt[b][:], G2[:], Bt2[:], (st, mv, rstd), D,
                            ("acc", b), ("cxt", b), rk=["G2", "Bt2"])
            cx.dma("sp", lambda e, i=i, b=b: e.dma_start(out=out[i * 128:(i + 1) * 128, :], in_=xt[b][:]),
                   r=[("cxt", b)], w=[])
            if tail_out is not None and i == NT - 1:
                cx.dma("sp", lambda e, b=b: e.dma_start(out=tail_out, in_=xt[b][:]), r=[("cxt", b)], w=[])
        cx.barrier()
    if ext is not None:
        return nc
    cx.finish()
    print("moe program: ops", cx.nops, "waits", cx.nwaits)
    return nc

import math

NEG = -30000.0
T_LOC = 2048
HALO = 128


def cpy(cx, en, out, in_, r, w):
    if en == "act":
        return cx.op("act", lambda e: e.copy(out=out, in_=in_), r=r, w=w)
    return cx.op(en, lambda e: e.tensor_copy(out=out, in_=in_), r=r, w=w)


class Mix:
    def __init__(self, kind, n_in, lite=False, ext=None):
        self.kind = kind
        self.lite = lite
        self.n_in = n_in
        D = 2048
        self.D = D
        self.KD = 16
        self.NTT = (T_LOC + HALO) // 128
        self.ext = ext
        if ext is not None:
            nc = self.nc = ext["nc"]
            self.cx, self.cst = ext["cx"], ext["cst"]
            for k_ in ("hx", "mem", "w_in", "b_in", "w_kv", "w_out", "b_out", "ln_g", "ln_b", "out", "U"):
                setattr(self, k_, ext[k_])
            self.flg = ext["flg"]
            cx = self.cx
            self.mkT = cx.sb("mkT", [128, 4, 256], BF16, ext["es"])
            self.mv = cx.sb("mv", [128, 2, 512], BF16, ext["es"])
            return
        nc = self.nc = bass.Bass("TRN2", target_bir_lowering=False)
        dt = nc.dram_tensor
        self.hx = dt("hx", [T_LOC + HALO, D], F32, kind="ExternalInput").ap()
        self.flag = dt("flag", [128, 1], F32, kind="ExternalInput").ap()
        if not lite:
            self.mem = dt("mem", [256, D], F32, kind="ExternalInput").ap()
        self.w_in = dt("w_in", [D, n_in], F32, kind="ExternalInput").ap()
        self.b_in = dt("b_in", [1, n_in], F32, kind="ExternalInput").ap()
        if not lite:
            self.w_kv = dt("w_kv", [D, 1024], F32, kind="ExternalInput").ap()
            self.w_out = dt("w_out", [D, D], F32, kind="ExternalInput").ap()
            self.b_out = dt("b_out", [1, D], F32, kind="ExternalInput").ap()
            self.ln_g = dt("ln_g", [1, D], F32, kind="ExternalInput").ap()
            self.ln_b = dt("ln_b", [1, D], F32, kind="ExternalInput").ap()
            self.out = dt("out", [T_LOC, D], F32, kind="ExternalOutput").ap()
        self.U = dt("U", [T_LOC + HALO, n_in], F32, kind="Internal").ap()
        self.cx = Ctx(nc)
        self.cst = make_consts(self.cx)
        cx = self.cx
        self.flg = cx.sb("flg", [128, 1], F32)
        cx.dma("sp", lambda e: e.dma_start(out=self.flg[:], in_=self.flag), w=["flg"])
        self.mkT = cx.sb("mkT", [128, 4, 256], BF16)
        self.mv = cx.sb("mv", [128, 2, 512], BF16)

    def phase_inproj(self, tm_ranges=None, fm_sink=None):
        cx, cst, D, KD, NTT = self.cx, self.cst, self.D, self.KD, self.NTT
        n_in = self.n_in
        with ExitStack() as es:
            hT = cx.sb("hT", [128, KD, NTT * 128], BF16, es)
            memT = cx.sb("memT", [128, KD, 256], BF16, es)
            Wb = [cx.sb("Wb%d" % i, [128, KD, 512], BF16, es) for i in range(2)]
            bbc = cx.sb("bbc", [128, n_in], F32, es)
            ev = [cx.sb("iev%d" % i, [128, 512], F32, es) for i in range(3)]
            ps_o = [cx.ps("ips_o%d" % i, [128, 512], F32, es) for i in range(3)]
            es0 = ExitStack()
            xt = [cx.sb("ixt%d" % i, [128, D], F32, es0) for i in range(2)]
            xb = [cx.sb("ixb%d" % i, [128, D], BF16, es0) for i in range(2)]
            ps_x = [cx.ps("ips_x%d" % i, [128, 1024], BF16, es0) for i in range(2)]
            cx.dma("sp", lambda e: e.dma_start(out=bbc[:], in_=self.b_in.partition_broadcast(128)), w=["bbc"])
            nx = 0
            srcs = [(self.hx[t * 128:(t + 1) * 128, :], hT, t) for t in range(NTT)]
            if not self.lite:
                srcs += [(self.mem[t * 128:(t + 1) * 128, :], memT, t) for t in range(2)]
            for n, (src, dstT, t) in enumerate(srcs):
                b = n % 2
                cx.dma("sp", lambda e, src=src, b=b: e.dma_start(out=xt[b][:], in_=src), w=[("ixt", b)])
                cpy(cx, "act" if n % 2 else "dve", xb[b][:], xt[b][:], [("ixt", b)], [("ixb", b)])
                for hh in range(2):
                    pb = nx % 2
                    nx += 1
                    for jj in range(8):
                        j = hh * 8 + jj
                        cx.op("pe", lambda e, j=j, jj=jj, pb=pb, b=b: e.transpose(
                            ps_x[pb][:, jj * 128:(jj + 1) * 128], xb[b][:, j * 128:(j + 1) * 128], cst["idb"][:]),
                            r=[("ixb", b), "c_idb"], w=[("ips_x", pb)])
                    cpy(cx, "dve" if nx % 2 else "act", dstT[:, hh * 8:(hh + 1) * 8, t * 128:(t + 1) * 128],
                        ps_x[pb][:].rearrange("p (a m) -> p a m", a=8), [("ips_x", pb)],
                        [("T", id(dstT), t, hh)])
            cx.barrier()
            es0.close()
            hT_keys = [("T", id(hT), t, hh) for t in range(NTT) for hh in range(2)]
            memT_keys = [("T", id(memT), t, hh) for t in range(2) for hh in range(2)]
            self.hT_keys = hT_keys
            no = 0
            if not self.lite:
                wkv = self.w_kv.rearrange("(j p) n -> p j n", p=128)
            for blk in range(0 if self.lite else 2):
                wb = blk % 2
                cx.dma("pool", lambda e, blk=blk, wb=wb: e.dma_start(out=Wb[wb][:], in_=wkv[:, :, blk * 512:(blk + 1) * 512]),
                       w=[("Wb", wb)])
                if blk == 0:
                    for h in range(4):
                        q = no % 3
                        no += 1
                        for j in range(KD):
                            cx.op("pe", lambda e, j=j, h=h, q=q, wb=wb: e.matmul(
                                ps_o[q][:, 0:256], lhsT=Wb[wb][:, j, h * 128:(h + 1) * 128], rhs=memT[:, j, :],
                                start=(j == 0), stop=(j == KD - 1)), r=[("Wb", wb)] + memT_keys, w=[("ips_o", q)])
                        cpy(cx, "dve", self.mkT[:, h, :], ps_o[q][:, 0:256], [("ips_o", q)], [("mkT", h)])
                else:
                    for mc in range(2):
                        q = no % 3
                        no += 1
                        for j in range(KD):
                            cx.op("pe", lambda e, j=j, mc=mc, q=q, wb=wb: e.matmul(
                                ps_o[q][:], lhsT=memT[:, j, mc * 128:(mc + 1) * 128], rhs=Wb[wb][:, j, :],
                                start=(j == 0), stop=(j == KD - 1)), r=[("Wb", wb)] + memT_keys, w=[("ips_o", q)])
                        cpy(cx, "dve", self.mv[:, mc, :], ps_o[q][:], [("ips_o", q)], [("mv", mc)])
            win = self.w_in.rearrange("(j p) n -> p j n", p=128)
            if tm_ranges is None:
                tm_ranges = [(0, n_in)]
            blks = []
            for (a0, a1) in tm_ranges:
                c = a0
                while c < a1:
                    blks.append((c, min(512, a1 - c)))
                    c += 512
            nw = 0
            nev = 0
            for (c0, cw) in blks:
                wb = nw % 2
                nw += 1
                cx.dma("pool", lambda e, c0=c0, cw=cw, wb=wb: e.dma_start(out=Wb[wb][:, :, 0:cw], in_=win[:, :, c0:c0 + cw]),
                       w=[("Wb", wb)])
                for t in range(NTT):
                    q = no % 3
                    no += 1
                    for j in range(KD):
                        cx.op("pe", lambda e, j=j, t=t, q=q, wb=wb, cw=cw: e.matmul(
                            ps_o[q][:, 0:cw], lhsT=hT[:, j, t * 128:(t + 1) * 128], rhs=Wb[wb][:, j, 0:cw],
                            start=(j == 0), stop=(j == KD - 1)),
                            r=[("Wb", wb), ("T", id(hT), t, 0), ("T", id(hT), t, 1)], w=[("ips_o", q)])
                    eb = nev % 3
                    nev += 1
                    cx.op("dve", lambda e, q=q, eb=eb, c0=c0, cw=cw: e.tensor_tensor(
                        out=ev[eb][:, 0:cw], in0=ps_o[q][:, 0:cw], in1=bbc[:, c0:c0 + cw], op=ALU.add),
                        r=[("ips_o", q), "bbc"], w=[("iev", eb)])
                    cx.dma("sp", lambda e, t=t, eb=eb, c0=c0, cw=cw: e.dma_start(
                        out=self.U[t * 128:(t + 1) * 128, c0:c0 + cw], in_=ev[eb][:, 0:cw]),
                        r=[("iev", eb)], w=[])
            if fm_sink is not None:
                fm_sink(es, hT, win, Wb, ps_o, ev)
            cx.barrier()

    def attn_setup(self, es):
        cx = self.cx
        a = {}
        a["Sm"] = [cx.sb("aSm%d" % i, [128, 4, 256], F32, es) for i in range(2)]
        a["P"] = [cx.sb("aP%d" % i, [128, 4, 256], BF16, es) for i in range(2)]
        a["PT"] = [cx.sb("aPT%d" % i, [128, 8, 128], BF16, es) for i in range(2)]
        a["st"] = [cx.sb("ast%d" % i, [128, 4, 4], F32, es) for i in range(2)]
        a["ps_s"] = [cx.ps("aps_s%d" % i, [128, 4, 256], F32, es) for i in range(1)]
        a["ps_pt"] = cx.ps("aps_pt", [128, 8, 128], BF16, es)
        a["ps_o"] = cx.ps("aps_o", [128, 512], F32, es)
        a["n"] = 0
        self.att = a
        return a

    def attn_group(self, s_fn, s_keys, scale, mask, mask_keys, sink_ap, sink_keys, v_fn, v_keys, hd, out_ap, out_key):
        cx, cst, a = self.cx, self.cst, self.att
        b = a["n"] % 2
        a["n"] += 1
        ps = a["ps_s"][0]
        Sm, P, PT, st = a["Sm"][b], a["P"][b], a["PT"][b], a["st"][b]
        kS, kSm, kP, kPT, kst = ("aps_s", 0), ("aSm", b), ("aP", b), ("aPT", b), ("ast", b)
        for g in range(4):
            s_fn(ps, g, list(s_keys), [kS])
        if mask is not None:
            cx.op("dve", lambda e: e.tensor_tensor(out=Sm[:], in0=ps[:], in1=mask, op=ALU.add),
                  r=[kS] + list(mask_keys), w=[kSm])
        else:
            cpy(cx, "dve", Sm[:], ps[:], [kS], [kSm])
        cx.op("dve", lambda e: e.tensor_reduce(out=st[:, 0, :], in_=Sm[:], axis=AX.X, op=ALU.max), r=[kSm], w=[(kst, 0)])
        cx.op("dve", lambda e: e.tensor_scalar(out=st[:, 1, :], in0=st[:, 0, :], scalar1=-float(scale), scalar2=None,
                                               op0=ALU.mult), r=[(kst, 0)], w=[(kst, 1)])
        for g in range(4):
            cx.op("act", lambda e, g=g: e.activation(out=P[:, g, :], in_=Sm[:, g, :], func=AF.Exp,
                                                    bias=st[:, 1, g:g + 1], scale=float(scale)),
                  r=[kSm, (kst, 1)], w=[(kP, g)])
        pk = [(kP, g) for g in range(4)]
        cx.op("dve", lambda e: e.tensor_reduce(out=st[:, 2, :], in_=P[:], axis=AX.X, op=ALU.add), r=pk, w=[(kst, 2)])
        if sink_ap is not None:
            cx.op("dve", lambda e: e.tensor_tensor(out=st[:, 3, :], in0=st[:, 1, :], in1=sink_ap, op=ALU.add),
                  r=[(kst, 1)] + list(sink_keys), w=[(kst, 3)])
            cx.op("act", lambda e: e.activation(out=st[:, 3, :], in_=st[:, 3, :], func=AF.Exp), r=[(kst, 3)], w=[(kst, 3)])
            cx.op("dve", lambda e: e.tensor_tensor(out=st[:, 2, :], in0=st[:, 2, :], in1=st[:, 3, :], op=ALU.add),
                  r=[(kst, 2), (kst, 3)], w=[(kst, 2)])
        cx.op("dve", lambda e: e.reciprocal(out=st[:, 3, :], in_=st[:, 2, :]), r=[(kst, 2)], w=[(kst, 3)])
        for g in range(4):
            for kc in range(2):
                cx.op("pe", lambda e, g=g, kc=kc: e.transpose(a["ps_pt"][:, g * 2 + kc, :], P[:, g, kc * 128:(kc + 1) * 128],
                                                              cst["idb"][:]),
                      r=[(kP, g), "c_idb"], w=["aps_pt"])
        cpy(cx, "act", PT[:], a["ps_pt"][:], ["aps_pt"], [kPT])
        for g in range(4):
            for kc in range(2):
                cx.op("pe", lambda e, g=g, kc=kc: e.matmul(a["ps_o"][:, g * hd:(g + 1) * hd], lhsT=PT[:, g * 2 + kc, :],
                                                           rhs=v_fn(g, kc), start=(kc == 0), stop=(kc == 1)),
                      r=[kPT] + list(v_keys), w=["aps_o"])
        cx.op("dve", lambda e: e.tensor_tensor(
            out=out_ap.rearrange("p (g d) -> p g d", g=4),
            in0=a["ps_o"][:, 0:4 * hd].rearrange("p (g d) -> p g d", g=4),
            in1=st[:, 3, :].unsqueeze(2).to_broadcast([128, 4, hd]), op=ALU.mult),
            r=["aps_o", (kst, 3)], w=[out_key])

    def xattn_setup(self, es):
        cx = self.cx
        self.qmb = cx.sb("qmb", [128, 512], BF16, es)
        self.qmT = cx.sb("qmT", [128, 4, 128], BF16, es)
        self.ps_q = cx.ps("ps_q", [128, 4, 128], BF16, es)

    def xattn_tile(self, qm_ap, qm_keys, cat_ap, cat_key):
        cx, cst = self.cx, self.cst
        cpy(cx, "act", self.qmb[:], qm_ap, list(qm_keys), ["qmb"])
        for h in range(4):
            cx.op("pe", lambda e, h=h: e.transpose(self.ps_q[:, h, :], self.qmb[:, h * 128:(h + 1) * 128], cst["idb"][:]),
                  r=["qmb", "c_idb"], w=["ps_q"])
        cpy(cx, "dve", self.qmT[:], self.ps_q[:], ["ps_q"], ["qmT"])

        def s_fn(ps, g, rk, wk):
            cx.op("pe", lambda e: e.matmul(ps[:, g, :], lhsT=self.qmT[:, g, :], rhs=self.mkT[:, g, :], start=True, stop=True),
                  r=["qmT", ("mkT", g)] + rk, w=wk)

        self.attn_group(s_fn, [], 128 ** -0.5, None, [], None, [],
                        lambda g, kc: self.mv[:, kc, g * 128:(g + 1) * 128], [("mv", 0), ("mv", 1)], 128,
                        cat_ap, cat_key)

    def outproj_setup(self, es, nbuf=2):
        cx, D, KD = self.cx, self.D, self.KD
        o = {}
        o["Wo"] = cx.sb("Wo", [128, KD, D], BF16, es)
        wo = self.w_out.rearrange("(j p) n -> p j n", p=128)
        for q4 in range(4):
            cx.dma("pool", lambda e, q4=q4: e.dma_start(out=o["Wo"][:, q4 * 4:(q4 + 1) * 4, :], in_=wo[:, q4 * 4:(q4 + 1) * 4, :]),
                   w=[("Wo", q4)])
        o["G"] = cx.sb("oG", [128, D], F32, es)
        o["B"] = cx.sb("oB", [128, D], F32, es)
        o["bo"] = cx.sb("obo", [128, D], F32, es)
        cx.dma("sp", lambda e: e.dma_start(out=o["G"][:], in_=self.ln_g.partition_broadcast(128)), w=["oG"])
        cx.dma("sp", lambda e: e.dma_start(out=o["B"][:], in_=self.ln_b.partition_broadcast(128)), w=["oB"])
        cx.dma("sp", lambda e: e.dma_start(out=o["bo"][:], in_=self.b_out.partition_broadcast(128)), w=["obo"])
        o["catT"] = [cx.sb("catT%d" % i, [128, KD, 128], BF16, es) for i in range(nbuf)]
        o["nbuf"] = nbuf
        o["ht"] = [cx.sb("oht%d" % i, [128, D], F32, es) for i in range(nbuf)]
        o["acc"] = [cx.sb("oacc%d" % i, [128, D], F32, es) for i in range(nbuf)]
        o["st"] = cx.sb("ost", [128, 6 * (D // 512)], F32, es)
        o["mv"] = cx.sb("omv", [128, 2], F32, es)
        o["rs"] = cx.sb("ors", [128, 1], F32, es)
        o["ps_ct"] = cx.ps("ps_ct", [128, 8, 128], BF16, es)
        o["ps_op"] = [cx.ps("ps_op%d" % i, [128, 512], F32, es) for i in range(2)]
        o["n"] = 0
        o["nq"] = 0
        self.o = o

    def outproj_tile(self, t_own, cat_ap, cat_keys):
        cx, cst, o, D, KD = self.cx, self.cst, self.o, self.D, self.KD
        b = o["n"] % o["nbuf"]
        o["n"] += 1
        catT, ht, acc = o["catT"][b], o["ht"][b], o["acc"][b]
        cx.dma("sp", lambda e: e.dma_start(out=ht[:], in_=self.hx[HALO + t_own * 128: HALO + (t_own + 1) * 128, :]),
               w=[("oht", b)])
        for hh in range(2):
            for jj in range(8):
                j = hh * 8 + jj
                cx.op("pe", lambda e, j=j, jj=jj: e.transpose(o["ps_ct"][:, jj, :], cat_ap[:, j * 128:(j + 1) * 128], cst["idb"][:]),
                      r=list(cat_keys) + ["c_idb"], w=["ps_ct"])
            cpy(cx, "act" if hh else "dve", catT[:, hh * 8:(hh + 1) * 8, :], o["ps_ct"][:], ["ps_ct"], [("catT", b, hh)])
        cx.op("dve", lambda e: e.scalar_tensor_tensor(out=acc[:], in0=ht[:], scalar=float(DN_ALPHA), in1=o["bo"][:],
                                                      op0=ALU.mult, op1=ALU.add), r=[("oht", b), "obo"], w=[("oacc", b)])
        for db in range(4):
            q = o["nq"] % 2
            o["nq"] += 1
            for j in range(KD):
                cx.op("pe", lambda e, j=j, db=db, q=q: e.matmul(
                    o["ps_op"][q][:], lhsT=catT[:, j, :], rhs=o["Wo"][:, j, db * 512:(db + 1) * 512],
                    start=(j == 0), stop=(j == KD - 1)),
                    r=[("catT", b, 0), ("catT", b, 1), ("Wo", j // 4)], w=[("ps_op", q)])
            cx.op("dve", lambda e, db=db, q=q: e.tensor_tensor(
                out=acc[:, db * 512:(db + 1) * 512], in0=o["ps_op"][q][:], in1=acc[:, db * 512:(db + 1) * 512], op=ALU.add),
                r=[("ps_op", q), ("oacc", b)], w=[("oacc", b)])
        layer_norm_tile(cx, acc[:], ht[:], o["G"][:], o["B"][:], (o["st"], o["mv"], o["rs"]), D,
                        ("oacc", b), ("oht", b), rk=["oG", "oB"])
        cx.dma("sp", lambda e: e.dma_start(out=self.out[t_own * 128:(t_own + 1) * 128, :], in_=ht[:]),
               r=[("oht", b)], w=[])

import os
STAGE = int(os.environ.get('STAGE', '9'))

ATTN_IN = 2432


def build_swa(ext=None):
    m = Mix("swa", ATTN_IN, ext=ext)
    nc, cx, cst = m.nc, m.cx, m.cst
    NTT = m.NTT
    if ext is None:
        pos = nc.dram_tensor("pos", [NTT, 128], I32, kind="ExternalInput").ap()
        sinks = nc.dram_tensor("sinks", [1, 24], F32, kind="ExternalInput").ap()
    else:
        pos, sinks = ext["pos"], ext["sinks"]
    m.phase_inproj()
    with ExitStack() as es:
        m.attn_setup(es)
        m.xattn_setup(es)
        m.outproj_setup(es)
        o = m.o
        COS = cx.sb("COS", [128, NTT, 8], F32, es)
        SIN = cx.sb("SIN", [128, NTT, 8], F32, es)
        SINKB = cx.sb("SINKB", [128, 24], F32, es)
        MASK = cx.sb("MASK", [128, 4, 256], F32, es)
        MASK0 = cx.sb("MASK0", [128, 4, 256], F32, es)
        cx.dma("sp", lambda e: e.dma_start(out=SINKB[:], in_=sinks.partition_broadcast(128)), w=["SINKB"])
        with ExitStack() as es2:
            pi_ = cx.sb("pi_", [NTT, 128], I32, es2)
            pf = cx.sb("pf", [NTT, 128], F32, es2)
            POSF = cx.sb("POSF", [128, NTT], F32, es2)
            INVF = cx.sb("INVF", [128, 8], F32, es2)
            ANG = cx.sb("ANG", [128, NTT, 8], F32, es2)
            AR = cx.sb("AR", [128, NTT, 8], F32, es2)
            pst = m.att["ps_o"][:, 0:NTT]
            cx.dma("sp", lambda e: e.dma_start(out=pi_[:], in_=pos), w=["pi_"])
            cx.op("dve", lambda e: e.tensor_copy(out=pf[:], in_=pi_[:]), r=["pi_"], w=["pf"])
            cx.op("pe", lambda e: e.transpose(pst, pf[:], cst["idf"][0:NTT, 0:NTT]), r=["pf", "c_idf"], w=["aps_o"])
            cx.op("dve", lambda e: e.tensor_copy(out=POSF[:], in_=pst), r=["aps_o"], w=["POSF"])
            for j in range(8):
                cx.op("pool", lambda e, j=j: e.memset(INVF[:, j:j + 1], float(500000.0 ** (-j / 8.0))), w=[("INVF", j)])
            cx.op("dve", lambda e: e.tensor_tensor(out=ANG[:], in0=POSF[:].unsqueeze(2).to_broadcast([128, NTT, 8]),
                                                   in1=INVF[:].unsqueeze(1).to_broadcast([128, NTT, 8]), op=ALU.mult),
                  r=["POSF"] + [("INVF", j) for j in range(8)], w=["ANG"])
            NI = cx.sb("NI", [128, NTT, 8], I32, es2)
            NF = cx.sb("NF", [128, NTT, 8], F32, es2)
            TW = cx.sb("TW", [128, NTT, 8], F32, es2)
            C1 = 6.28125
            C2 = 2 * math.pi - C1
            cx.op("dve", lambda e: e.tensor_scalar(out=NI[:], in0=ANG[:], scalar1=1.0 / (2 * math.pi), scalar2=None,
                                                   op0=ALU.mult), r=["ANG"], w=["NI"])
            cx.op("dve", lambda e: e.tensor_copy(out=NF[:], in_=NI[:]), r=["NI"], w=["NF"])
            cx.op("dve", lambda e: e.scalar_tensor_tensor(out=AR[:], in0=NF[:], scalar=-C1, in1=ANG[:],
                                                          op0=ALU.mult, op1=ALU.add), r=["NF", "ANG"], w=["AR"])
            cx.op("dve", lambda e: e.scalar_tensor_tensor(out=AR[:], in0=NF[:], scalar=-C2, in1=AR[:],
                                                          op0=ALU.mult, op1=ALU.add), r=["NF", "AR"], w=["AR"])

            def wrap(dst_key):
                cx.op("dve", lambda e: e.tensor_scalar(out=TW[:], in0=AR[:], scalar1=math.pi, scalar2=2 * math.pi,
                                                       op0=ALU.is_gt, op1=ALU.mult), r=["AR"], w=["TW"])
                cx.op("dve", lambda e: e.tensor_tensor(out=AR[:], in0=AR[:], in1=TW[:], op=ALU.subtract),
                      r=["AR", "TW"], w=["AR"])
                cx.op("dve", lambda e: e.tensor_scalar(out=TW[:], in0=AR[:], scalar1=-math.pi, scalar2=2 * math.pi,
                                                       op0=ALU.is_lt, op1=ALU.mult), r=["AR"], w=["TW"])
                cx.op("dve", lambda e: e.tensor_tensor(out=AR[:], in0=AR[:], in1=TW[:], op=ALU.add),
                      r=["AR", "TW"], w=["AR"])
            wrap(None)
            cx.op("act", lambda e: e.activation(out=SIN[:], in_=AR[:], func=AF.Sin), r=["AR"], w=["SIN"])
            cx.op("dve", lambda e: e.tensor_scalar(out=AR[:], in0=AR[:], scalar1=math.pi / 2, scalar2=None,
                                                   op0=ALU.add), r=["AR"], w=["AR"])
            wrap(None)
            cx.op("act", lambda e: e.activation(out=COS[:], in_=AR[:], func=AF.Sin), r=["AR"], w=["COS"])
            cx.barrier()
        cx.op("pool", lambda e: e.memset(MASK[:], 0.0), w=["MASK"])
        for g in range(4):
            cx.op("pool", lambda e, g=g: e.affine_select(out=MASK[:, g, :], in_=MASK[:, g, :], pattern=[[1, 256]],
                                                        compare_op=ALU.is_ge, fill=NEG, base=-1, channel_multiplier=-1),
                  r=["MASK"], w=["MASK"])
            cx.op("pool", lambda e, g=g: e.affine_select(out=MASK[:, g, :], in_=MASK[:, g, :], pattern=[[-1, 256]],
                                                        compare_op=ALU.is_ge, fill=NEG, base=128, channel_multiplier=1),
                  r=["MASK"], w=["MASK"])
        fm1 = cx.sb("fm1", [128, 1], F32, es)
        cx.op("dve", lambda e: e.tensor_scalar(out=fm1[:], in0=m.flg[:], scalar1=-1.0, scalar2=-NEG, op0=ALU.add, op1=ALU.mult),
              r=["flg"], w=["fm1"])
        cx.op("dve", lambda e: e.tensor_copy(out=MASK0[:], in_=MASK[:]), r=["MASK"], w=["MASK0"])
        cx.op("dve", lambda e: e.tensor_scalar(out=MASK0[:, :, 0:128], in0=MASK0[:, :, 0:128], scalar1=fm1[:, 0:1], scalar2=None,
                                               op0=ALU.add), r=["MASK0", "fm1"], w=["MASK0"])
        Ut = [cx.sb("Ut%d" % i, [128, ATTN_IN], F32, es) for i in range(2)]
        QR = cx.sb("QR", [128, 1536], BF16, es)
        K2 = cx.sb("K2", [128, 3, 64], BF16, es)
        qT = cx.sb("qT", [64, 24, 128], BF16, es)
        kT2 = [cx.sb("kT2_%d" % i, [64, 3, 128], BF16, es) for i in range(3)]
        Vr = [cx.sb("Vr%d" % i, [128, 192], BF16, es) for i in range(3)]
        cat = [cx.sb("cat%d" % i, [128, 2048], BF16, es) for i in range(2)]
        RA = [cx.sb("RA%d" % i, [128, 24, 8], F32, es) for i in range(4)]

        def rope(src3, nh, dst1, dst2, cos_b, sin_b, rk, wk):
            t1 = src3[:, :, 0:8]
            t2 = src3[:, :, 8:16]
            A, B_, C_, D_ = [RA[i][:, 0:nh, :] for i in range(4)]
            cx.op("dve", lambda e: e.tensor_tensor(out=A, in0=t1, in1=cos_b, op=ALU.mult), r=rk + ["COS"], w=[("RA", 0)])
            cx.op("dve", lambda e: e.tensor_tensor(out=B_, in0=t2, in1=sin_b, op=ALU.mult), r=rk + ["SIN"], w=[("RA", 1)])
            cx.op("dve", lambda e: e.tensor_tensor(out=C_, in0=t2, in1=cos_b, op=ALU.mult), r=rk + ["COS"], w=[("RA", 2)])
            cx.op("dve", lambda e: e.tensor_tensor(out=D_, in0=t1, in1=sin_b, op=ALU.mult), r=rk + ["SIN"], w=[("RA", 3)])
            cx.op("dve", lambda e: e.tensor_tensor(out=dst1, in0=A, in1=B_, op=ALU.subtract),
                  r=[("RA", 0), ("RA", 1)] + wk, w=wk)
            cx.op("dve", lambda e: e.tensor_tensor(out=dst2, in0=C_, in1=D_, op=ALU.add),
                  r=[("RA", 2), ("RA", 3)] + wk, w=wk)

        for t in range(NTT):
            ub = t % 2
            U_ = Ut[ub]
            cx.dma("sp", lambda e, t=t, U_=U_: e.dma_start(out=U_[:], in_=m.U[t * 128:(t + 1) * 128, :]), w=[("Ut", ub)])
            ku = [("Ut", ub)]
            kv3 = U_[:, 1536:1728].rearrange("p (h d) -> p h d", h=3)
            cpy(cx, "act", K2[:], kv3, ku, ["K2"])
            rope(kv3, 3, K2[:, :, 0:8], K2[:, :, 8:16],
                 COS[:, t, :].unsqueeze(1).to_broadcast([128, 3, 8]), SIN[:, t, :].unsqueeze(1).to_broadcast([128, 3, 8]),
                 ku, ["K2"])
            for g in range(3):
                cx.op("pe", lambda e, g=g: e.transpose(o["ps_ct"][0:64, g, :], K2[:, g, :], cst["idb"][:]),
                      r=["K2", "c_idb"], w=["ps_ct"])
            cpy(cx, "dve", kT2[t % 3][:], o["ps_ct"][0:64, 0:3, :], ["ps_ct"], [("kT2", t % 3)])
            cpy(cx, "act", Vr[t % 3][:], U_[:, 1728:1920], ku, [("Vr", t % 3)])
            if t == 0 or STAGE < 3:
                continue
            q3 = U_[:, 0:1536].rearrange("p (h d) -> p h d", h=24)
            QR3 = QR[:].rearrange("p (h d) -> p h d", h=24)
            cpy(cx, "act", QR[:], U_[:, 0:1536], ku, ["QR"])
            rope(q3, 24, QR3[:, :, 0:8], QR3[:, :, 8:16],
                 COS[:, t, :].unsqueeze(1).to_broadcast([128, 24, 8]), SIN[:, t, :].unsqueeze(1).to_broadcast([128, 24, 8]),
                 ku, ["QR"])
            for c0 in (0, 8, 16):
                for jj in range(8):
                    cx.op("pe", lambda e, c0=c0, jj=jj: e.transpose(o["ps_ct"][0:64, jj, :], QR[:, (c0 + jj) * 64:(c0 + jj + 1) * 64],
                                                                   cst["idb"][:]), r=["QR", "c_idb"], w=["ps_ct"])
                cpy(cx, "dve" if c0 != 8 else "act", qT[:, c0:c0 + 8, :], o["ps_ct"][0:64, :, :], ["ps_ct"], [("qT", c0)])
            cb = t % 2
            cat_t = cat[cb]
            if STAGE < 4:
                continue
            mask_t = MASK0 if t == 1 else MASK
            for g in range(3):
                for r0 in (0, 4):
                    h0 = g * 8 + r0

                    def s_fn(ps, gi, rk, wk, h0=h0, g=g, t=t):
                        h = h0 + gi
                        qk = [("qT", 0), ("qT", 8), ("qT", 16)]
                        cx.op("pe", lambda e: e.matmul(ps[:, gi, 0:128], lhsT=qT[:, h, :], rhs=kT2[(t - 1) % 3][:, g, :],
                                                       start=True, stop=True),
                              r=qk + [("kT2", (t - 1) % 3)] + rk, w=wk)
                        cx.op("pe", lambda e: e.matmul(ps[:, gi, 128:256], lhsT=qT[:, h, :], rhs=kT2[t % 3][:, g, :],
                                                       start=True, stop=True),
                              r=qk + [("kT2", t % 3)] + rk, w=wk)

                    m.attn_group(s_fn, [], 64 ** -0.5, mask_t[:], ["MASK", "MASK0"], SINKB[:, h0:h0 + 4], ["SINKB"],
                                 lambda gi, kc, g=g, t=t: Vr[(t - 1 + kc) % 3][:, g * 64:(g + 1) * 64],
                                 [("Vr", (t - 1) % 3), ("Vr", t % 3)], 64,
                                 cat_t[:, h0 * 64:(h0 + 4) * 64], ("cat", cb, h0 // 4))
            if STAGE < 5:
                continue
            m.xattn_tile(U_[:, 1920:2432], ku, cat_t[:, 1536:2048], ("cat", cb, 6))
            if STAGE < 6:
                continue
            m.outproj_tile(t - 1, cat_t, [("cat", cb, i) for i in range(7)])
    if ext is not None:
        cx.barrier()
        return nc
    cx.finish()
    print("swa program: ops", cx.nops, "waits", cx.nwaits)
    return nc


CONF_IN = 3584


def build_conf(ext=None):
    m = Mix("conf", CONF_IN, ext=ext)
    nc, cx, cst = m.nc, m.cx, m.cst
    NTT = m.NTT
    TT = NTT * 128
    if ext is None:
        dw_w = nc.dram_tensor("dw_w", [31, 1536], F32, kind="ExternalInput").ap()
        dw_b = nc.dram_tensor("dw_b", [1, 1536], F32, kind="ExternalInput").ap()
        cg = nc.dram_tensor("cln_g", [1, 1536], F32, kind="ExternalInput").ap()
        cb_ = nc.dram_tensor("cln_b", [1, 1536], F32, kind="ExternalInput").ap()
        Ymix = nc.dram_tensor("Ymix", [T_LOC, 1536], F32, kind="Internal").ap()
    else:
        dw_w, dw_b, cg, cb_, Ymix = [ext[k_] for k_ in ("dw_w", "dw_b", "cln_g", "cln_b", "Ymix")]

    def fm_sink(es, hT, win, Wb, ps_o, ev):
        KD = m.KD
        bsrc = cx.sb("bsrc", [28, 128], F32, es)
        BT = cx.sb("BT", [128, 28], F32, es)
        dws = cx.sb("dws", [31, 1536], F32, es)
        DWT = cx.sb("DWT", [128, 12, 32], F32, es)
        dbs = cx.sb("dbs", [12, 128], F32, es)
        DWB = cx.sb("DWB", [128, 12], F32, es)
        HC = [cx.sb("HC%d" % i, [128, TT], F32, es) for i in range(2)]
        Y = [cx.sb("Y%d" % i, [128, T_LOC], F32, es) for i in range(4)]
        sg = [cx.sb("csg%d" % i, [128, 512], F32, es) for i in range(2)]
        ps_tr = [cx.ps("ps_tr%d" % i, [128, 512], F32, es) for i in range(2)]
        cx.dma("sp", lambda e: e.dma_start(out=bsrc[:], in_=m.b_in.rearrange("o (c p) -> (o c) p", p=128)), w=["bsrc"])
        cx.dma("sp", lambda e: e.dma_start(out=dws[:], in_=dw_w), w=["dws"])
        cx.dma("sp", lambda e: e.dma_start(out=dbs[:], in_=dw_b.rearrange("o (c p) -> (o c) p", p=128)), w=["dbs"])
        cx.op("pe", lambda e: e.transpose(ps_tr[0][:, 0:28], bsrc[:], cst["idf"][0:28, 0:28]), r=["bsrc", "c_idf"], w=[("ps_tr", 0)])
        cpy(cx, "dve", BT[:], ps_tr[0][:, 0:28], [("ps_tr", 0)], ["BT"])
        cx.op("pe", lambda e: e.transpose(ps_tr[0][:, 0:12], dbs[:], cst["idf"][0:12, 0:12]), r=["dbs", "c_idf"], w=[("ps_tr", 0)])
        cpy(cx, "dve", DWB[:], ps_tr[0][:, 0:12], [("ps_tr", 0)], ["DWB"])
        for c in range(12):
            cx.op("pe", lambda e, c=c: e.transpose(ps_tr[1][:, c * 32:c * 32 + 31], dws[:, c * 128:(c + 1) * 128],
                                                   cst["idf"][0:31, 0:31]), r=["dws", "c_idf"], w=[("ps_tr", 1)])
        cpy(cx, "dve", DWT[:, :, 0:31], ps_tr[1][:, 0:384].rearrange("p (c k) -> p c k", k=32)[:, :, 0:31], [("ps_tr", 1)], ["DWT"])
        no = 0
        ntr = 0
        nst = 0
        for cb in range(3):
            cx.dma("pool", lambda e, cb=cb: e.dma_start(out=Wb[0][:], in_=win[:, :, cb * 512:(cb + 1) * 512]), w=[("Wb", 0)])
            cx.dma("pool", lambda e, cb=cb: e.dma_start(out=Wb[1][:], in_=win[:, :, 1536 + cb * 512:1536 + (cb + 1) * 512]),
                   w=[("Wb", 1)])
            for cc in range(4):
                c = cb * 4 + cc
                hb = c % 2
                H = HC[hb]
                for tb in range((TT + 511) // 512):
                    t0 = tb * 512
                    tw = min(512, TT - t0)
                    hk = []
                    for tt in range(t0 // 128, (t0 + tw) // 128):
                        hk += [("T", id(hT), tt, 0), ("T", id(hT), tt, 1)]
                    qa = no % 3
                    qb = (no + 1) % 3
                    no += 2
                    for j in range(KD):
                        cx.op("pe", lambda e, j=j, cc=cc, qa=qa, t0=t0, tw=tw: e.matmul(
                            ps_o[qa][:, 0:tw], lhsT=Wb[0][:, j, cc * 128:(cc + 1) * 128], rhs=hT[:, j, t0:t0 + tw],
                            start=(j == 0), stop=(j == KD - 1)), r=[("Wb", 0)] + hk, w=[("ips_o", qa)])
                    for j in range(KD):
                        cx.op("pe", lambda e, j=j, cc=cc, qb=qb, t0=t0, tw=tw: e.matmul(
                            ps_o[qb][:, 0:tw], lhsT=Wb[1][:, j, cc * 128:(cc + 1) * 128], rhs=hT[:, j, t0:t0 + tw],
                            start=(j == 0), stop=(j == KD - 1)), r=[("Wb", 1)] + hk, w=[("ips_o", qb)])
                    sb_ = no % 2
                    cx.op("act", lambda e, qb=qb, tw=tw, c=c, sb_=sb_: e.activation(
                        out=sg[sb_][:, 0:tw], in_=ps_o[qb][:, 0:tw], func=AF.Sigmoid, bias=BT[:, 12 + c:13 + c], scale=1.0),
                        r=[("ips_o", qb), "BT"], w=[("csg", sb_)])
                    cx.op("dve", lambda e, qa=qa, t0=t0, tw=tw, c=c, sb_=sb_, H=H: e.scalar_tensor_tensor(
                        out=H[:, t0:t0 + tw], in0=ps_o[qa][:, 0:tw], scalar=BT[:, c:c + 1], in1=sg[sb_][:, 0:tw],
                        op0=ALU.add, op1=ALU.mult), r=[("ips_o", qa), ("csg", sb_), "BT"], w=[("HC", hb)])
                cx.op("dve", lambda e, H=H: e.tensor_scalar(out=H[:, 0:128], in0=H[:, 0:128], scalar1=m.flg[:, 0:1], scalar2=None,
                                                            op0=ALU.mult), r=[("HC", hb), "flg"], w=[("HC", hb)])
                Yc = Y[cc]
                cx.op("dve", lambda e, H=H, Yc=Yc, c=c: e.tensor_scalar(
                    out=Yc[:], in0=H[:, 98:98 + T_LOC], scalar1=DWT[:, c, 0:1], scalar2=DWB[:, c:c + 1],
                    op0=ALU.mult, op1=ALU.add), r=[("HC", hb), "DWT", "DWB"], w=[("Y", cc)])
                for k in range(1, 31):
                    cx.op("dve", lambda e, H=H, Yc=Yc, c=c, k=k: e.scalar_tensor_tensor(
                        out=Yc[:], in0=H[:, 98 + k:98 + k + T_LOC], scalar=DWT[:, c, k:k + 1], in1=Yc[:],
                        op0=ALU.mult, op1=ALU.add), r=[("HC", hb), "DWT", ("Y", cc)], w=[("Y", cc)])
            for t in range(T_LOC // 128):
                pb = ntr % 2
                ntr += 1
                for cc in range(4):
                    cx.op("pe", lambda e, cc=cc, t=t, pb=pb: e.transpose(
                        ps_tr[pb][:, cc * 128:(cc + 1) * 128], Y[cc][:, t * 128:(t + 1) * 128], cst["idf"][:]),
                        r=[("Y", cc), "c_idf"], w=[("ps_tr", pb)])
                eb = nst % 3
                nst += 1
                cpy(cx, "act", ev[eb][:], ps_tr[pb][:], [("ps_tr", pb)], [("iev", eb)])
                cx.dma("sp", lambda e, t=t, cb=cb, eb=eb: e.dma_start(
                    out=Ymix[t * 128:(t + 1) * 128, cb * 512:(cb + 1) * 512], in_=ev[eb][:]), r=[("iev", eb)], w=[])

    m.phase_inproj(tm_ranges=[(3072, 3584)], fm_sink=fm_sink)
    with ExitStack() as es:
        m.attn_setup(es)
        m.xattn_setup(es)
        m.outproj_setup(es)
        CG = cx.sb("CG", [128, 1536], F32, es)
        CB = cx.sb("CB", [128, 1536], F32, es)
        cx.dma("sp", lambda e: e.dma_start(out=CG[:], in_=cg.partition_broadcast(128)), w=["CG"])
        cx.dma("sp", lambda e: e.dma_start(out=CB[:], in_=cb_.partition_broadcast(128)), w=["CB"])
        yt = [cx.sb("yt%d" % i, [128, 1536], F32, es) for i in range(2)]
        yn = [cx.sb("yn%d" % i, [128, 1536], F32, es) for i in range(2)]
        qm = [cx.sb("qm%d" % i, [128, 512], F32, es) for i in range(2)]
        cat = [cx.sb("cat%d" % i, [128, 2048], BF16, es) for i in range(2)]
        st = cx.sb("cst_", [128, 18], F32, es)
        mv = cx.sb("cmv_", [128, 2], F32, es)
        rs = cx.sb("crs_", [128, 1], F32, es)
        for t in range(T_LOC // 128):
            b = t % 2
            cx.dma("sp", lambda e, t=t, b=b: e.dma_start(out=yt[b][:], in_=Ymix[t * 128:(t + 1) * 128, :]), w=[("yt", b)])
            cx.dma("sp", lambda e, t=t, b=b: e.dma_start(out=qm[b][:], in_=m.U[HALO + t * 128:HALO + (t + 1) * 128, 3072:3584]),
                   w=[("qm", b)])
            layer_norm_tile(cx, yt[b][:], yn[b][:], CG[:], CB[:], (st, mv, rs), 1536, ("yt", b), ("yn", b), rk=["CG", "CB"])
            cx.op("act", lambda e, b=b: e.activation(out=cat[b][:, 0:1536], in_=yn[b][:], func=AF.Silu), r=[("yn", b)],
                  w=[("cat", b, 0)])
            m.xattn_tile(qm[b][:], [("qm", b)], cat[b][:, 1536:2048], ("cat", b, 1))
            m.outproj_tile(t, cat[b], [("cat", b, 0), ("cat", b, 1)])
    if ext is not None:
        cx.barrier()
        return nc
    cx.finish()
    print("conf program: ops", cx.nops, "waits", cx.nwaits)
    return nc


SSD_IN = 4632


def build_ssd(mode="B", ext=None):
    A_ONLY = (mode == "A")
    m = Mix("ssd", SSD_IN, lite=A_ONLY, ext=ext)
    nc, cx, cst = m.nc, m.cx, m.cst
    NTT = m.NTT
    TT = NTT * 128
    NCH = T_LOC // 128
    dt_ = nc.dram_tensor
    if ext is None:
        conv_w = dt_("conv_w", [4, 2560], F32, kind="ExternalInput").ap()
        conv_b = dt_("conv_b", [1, 2560], F32, kind="ExternalInput").ap()
        dt_bias = dt_("dt_bias", [1, 24], F32, kind="ExternalInput").ap()
        a_log = dt_("a_log", [1, 24], F32, kind="ExternalInput").ap()
        if A_ONLY:
            S_end = dt_("S_end", [128, 1536], F32, kind="ExternalOutput").ap()
            a_tot = dt_("a_tot", [128, 24], F32, kind="ExternalOutput").ap()
        else:
            d_skip = dt_("d_skip", [1, 24], F32, kind="ExternalInput").ap()
            norm_g = dt_("norm_g", [1, 1536], F32, kind="ExternalInput").ap()
            Sp = dt_("Sp", [3, 128, 1536], F32, kind="ExternalInput").ap()
            Ap = dt_("Ap", [3, 128, 24], F32, kind="ExternalInput").ap()
        XTOK = dt_("XTOK", [T_LOC, 1536], F32, kind="Internal").ap()
        BTOK = dt_("BTOK", [T_LOC, 512], F32, kind="Internal").ap()
        BCT = dt_("BCT", [1024, T_LOC], BF16, kind="Internal").ap()
    else:
        conv_w, conv_b, dt_bias, a_log, S_end, a_tot, d_skip, norm_g, Sp, Ap, XTOK, BTOK, BCT = [ext[k_] for k_ in (
            "conv_w", "conv_b", "dt_bias", "a_log", "S_end", "a_tot", "d_skip", "norm_g", "Sp", "Ap", "XTOK", "BTOK", "BCT")]

    def fm_sink(es, hT, win, Wb, ps_o, ev):
        KD = m.KD
        bsrc = cx.sb("bsrc", [36, 128], F32, es)
        BT = cx.sb("BT", [128, 36], F32, es)
        cws = cx.sb("cws", [4, 1280], F32, es)
        CWT = cx.sb("CWT", [128, 20, 4], F32, es)
        cbs = cx.sb("cbs", [20, 128], F32, es)
        CBT_ = cx.sb("CBT_", [128, 20], F32, es)
        PRE = [cx.sb("PRE%d" % i, [128, TT], F32, es) for i in range(1)]
        XC = [cx.sb("XC%d" % i, [128, T_LOC], F32, es) for i in range(1)]
        XS = [cx.sb("XS%d" % i, [128, T_LOC], F32, es) for i in range(4)]
        XSb = [cx.sb("XSb%d" % i, [128, T_LOC], BF16, es) for i in range(1)]
        ps_tr = [cx.ps("ps_tr%d" % i, [128, 512], F32, es) for i in range(2)]
        cx.dma("sp", lambda e: e.dma_start(out=bsrc[:], in_=m.b_in[:, 0:4608].rearrange("o (c p) -> (o c) p", p=128)), w=["bsrc"])
        cx.dma("sp", lambda e: e.dma_start(out=cbs[:], in_=conv_b.rearrange("o (c p) -> (o c) p", p=128)), w=["cbs"])
        cx.op("pe", lambda e: e.transpose(ps_tr[0][:, 0:36], bsrc[:], cst["idf"][0:36, 0:36]), r=["bsrc", "c_idf"], w=[("ps_tr", 0)])
        cpy(cx, "dve", BT[:], ps_tr[0][:, 0:36], [("ps_tr", 0)], ["BT"])
        cx.op("pe", lambda e: e.transpose(ps_tr[0][:, 0:20], cbs[:], cst["idf"][0:20, 0:20]), r=["cbs", "c_idf"], w=[("ps_tr", 0)])
        cpy(cx, "dve", CBT_[:], ps_tr[0][:, 0:20], [("ps_tr", 0)], ["CBT_"])
        for half in range(2):
            cx.dma("sp", lambda e, half=half: e.dma_start(out=cws[:], in_=conv_w[:, half * 1280:(half + 1) * 1280]), w=["cws"])
            for c in range(10):
                cx.op("pe", lambda e, c=c, half=half: e.transpose(ps_tr[1][:, (half * 10 + c) * 4:(half * 10 + c) * 4 + 4],
                                                                 cws[:, c * 128:(c + 1) * 128], cst["idf"][0:4, 0:4]),
                      r=["cws", "c_idf"], w=[("ps_tr", 1)])
        cpy(cx, "dve", CWT[:], ps_tr[1][:, 0:80].rearrange("p (c k) -> p c k", k=4), [("ps_tr", 1)], ["CWT"])
        no = 0
        ntr = 0
        nst = 0
        nxb = 0
        for cblk in range(5):
            wb = cblk % 2
            cx.dma("pool", lambda e, cblk=cblk, wb=wb: e.dma_start(
                out=Wb[wb][:], in_=win[:, :, 1536 + cblk * 512:1536 + (cblk + 1) * 512]), w=[("Wb", wb)])
            for cc in range(4):
                c = cblk * 4 + cc
                hb = 0
                P_ = PRE[hb]
                for tb in range((TT + 511) // 512):
                    t0 = tb * 512
                    tw = min(512, TT - t0)
                    hk = []
                    for tt in range(t0 // 128, (t0 + tw) // 128):
                        hk += [("T", id(hT), tt, 0), ("T", id(hT), tt, 1)]
                    qa = no % 3
                    no += 1
                    for j in range(KD):
                        cx.op("pe", lambda e, j=j, cc=cc, qa=qa, t0=t0, tw=tw, wb=wb: e.matmul(
                            ps_o[qa][:, 0:tw], lhsT=Wb[wb][:, j, cc * 128:(cc + 1) * 128], rhs=hT[:, j, t0:t0 + tw],
                            start=(j == 0), stop=(j == KD - 1)), r=[("Wb", wb)] + hk, w=[("ips_o", qa)])
                    cx.op("act", lambda e, qa=qa, t0=t0, tw=tw, c=c, P_=P_: e.activation(
                        out=P_[:, t0:t0 + tw], in_=ps_o[qa][:, 0:tw], func=AF.Identity, bias=BT[:, 12 + c:13 + c], scale=1.0),
                        r=[("ips_o", qa), "BT"], w=[("PRE", hb)])
                cx.op("dve", lambda e, P_=P_: e.tensor_scalar(out=P_[:, 0:128], in0=P_[:, 0:128], scalar1=m.flg[:, 0:1], scalar2=None,
                                                              op0=ALU.mult), r=[("PRE", hb), "flg"], w=[("PRE", hb)])
                X_ = XC[0]
                cx.op("dve", lambda e, P_=P_, X_=X_, c=c: e.tensor_scalar(
                    out=X_[:], in0=P_[:, 125:125 + T_LOC], scalar1=CWT[:, c, 0:1], scalar2=CBT_[:, c:c + 1],
                    op0=ALU.mult, op1=ALU.add), r=[("PRE", hb), "CWT", "CBT_"], w=[("XC", 0)])
                for k in range(1, 4):
                    cx.op("dve", lambda e, P_=P_, X_=X_, c=c, k=k: e.scalar_tensor_tensor(
                        out=X_[:], in0=P_[:, 125 + k:125 + k + T_LOC], scalar=CWT[:, c, k:k + 1], in1=X_[:],
                        op0=ALU.mult, op1=ALU.add), r=[("PRE", hb), "CWT", ("XC", 0)], w=[("XC", 0)])
                if cblk <= 3:
                    cx.op("act", lambda e, X_=X_, cc=cc: e.activation(out=XS[cc][:], in_=X_[:], func=AF.Silu),
                          r=[("XC", 0)], w=[("XS", cc)])
                if cblk >= 3:
                    xb_ = 0
                    nxb += 1
                    cx.op("act", lambda e, X_=X_, xb_=xb_: e.activation(out=XSb[xb_][:], in_=X_[:], func=AF.Silu),
                          r=[("XC", 0)], w=[("XSb", xb_)])
                    cx.dma("sp", lambda e, c=c, xb_=xb_: e.dma_start(out=BCT[(c - 12) * 128:(c - 11) * 128, :], in_=XSb[xb_][:]),
                           r=[("XSb", xb_)], w=[])
            if cblk <= 3:
                for t in range(NCH):
                    pb = ntr % 2
                    ntr += 1
                    for cc in range(4):
                        cx.op("pe", lambda e, cc=cc, t=t, pb=pb: e.transpose(
                            ps_tr[pb][:, cc * 128:(cc + 1) * 128], XS[cc][:, t * 128:(t + 1) * 128], cst["idf"][:]),
                            r=[("XS", cc), "c_idf"], w=[("ps_tr", pb)])
                    eb = nst % 3
                    nst += 1
                    cpy(cx, "dve", ev[eb][:], ps_tr[pb][:], [("ps_tr", pb)], [("iev", eb)])
                    if cblk < 3:
                        dst = XTOK[t * 128:(t + 1) * 128, cblk * 512:(cblk + 1) * 512]
                    else:
                        dst = BTOK[t * 128:(t + 1) * 128, :]
                    cx.dma("sp", lambda e, dst=dst, eb=eb: e.dma_start(out=dst, in_=ev[eb][:]), r=[("iev", eb)], w=[])

    m.phase_inproj(tm_ranges=([(4096, 4632)] if A_ONLY else [(0, 1536), (4096, 4632)]), fm_sink=fm_sink)
    if mode == "F":
        _ssd_loop(m, True, locals())
        ext["exchange"]()
        _ssd_loop(m, False, locals())
        cx.barrier()
        return nc
    _ssd_loop(m, A_ONLY, locals())
    cx.finish()
    print("ssd program", mode, ": ops", cx.nops, "waits", cx.nwaits)
    return nc


def _ssd_loop(m, A_ONLY, env):
    nc, cx, cst = m.nc, m.cx, m.cst
    NCH = T_LOC // 128
    conv_w, conv_b, dt_bias, a_log, XTOK, BTOK, BCT = [env[k_] for k_ in ("conv_w", "conv_b", "dt_bias", "a_log", "XTOK", "BTOK", "BCT")]
    if A_ONLY or env["mode"] == "F":
        S_end, a_tot = env["S_end"], env["a_tot"]
    if (not A_ONLY) or env["mode"] == "F":
        d_skip, norm_g, Sp, Ap = env["d_skip"], env["norm_g"], env["Sp"], env["Ap"]
    with ExitStack() as es:
        m.attn_setup(es)
        m.xattn_setup(es)
        if not A_ONLY:
            m.outproj_setup(es, nbuf=1)
            o = m.o
            ps_ct, ps_op1, k_op1 = o["ps_ct"], o["ps_op"][1], ("ps_op", 1)
        else:
            ps_ct = cx.ps("ps_ct", [128, 8, 128], BF16, es)
            ps_op1 = cx.ps("ps_op1", [128, 512], F32, es)
            k_op1 = ("ps_op", 1)
        a = m.att
        ps_s = a["ps_s"][0]
        P_SNEW = ps_s[:, 0:2, :].rearrange("p a b -> p (a b)")[:, 0:384]
        P_YOFF = ps_s[:, 2:4, :].rearrange("p a b -> p (a b)")[:, 0:384]
        kS = ("aps_s", 0)
        P_CBT = a["ps_pt"][:].rearrange("p a b -> p (a b)").bitcast(F32)[:, 0:128]
        kCBT = "aps_pt"
        P_YDG = a["ps_o"][:, 0:384]
        kYDG = "aps_o"
        segq = m.ps_q[:].rearrange("p a b -> p (a b)").bitcast(F32)
        P_SEG = [segq[:, 0:128], ps_op1[:, 0:128]]
        kSEG = ["ps_q", k_op1]
        smallp = ps_ct[:].rearrange("p a b -> p (a b)").bitcast(F32)
        P_ACS = smallp[:, 0:24]
        P_ATOT = smallp[:, 32:56]
        kSM = "ps_ct"
        DTB = cx.sb("DTB", [128, 24], F32, es)
        ANEG = cx.sb("ANEG", [128, 24], F32, es)
        LT = cx.sb("LT", [128, 128], F32, es)
        NEGM = cx.sb("NEGM", [128, 128], F32, es)
        cx.dma("sp", lambda e: e.dma_start(out=DTB[:], in_=dt_bias.partition_broadcast(128)), w=["DTB"])
        cx.dma("sp", lambda e: e.dma_start(out=ANEG[:], in_=a_log.partition_broadcast(128)), w=["ANEG"])
        cx.op("act", lambda e: e.activation(out=ANEG[:], in_=ANEG[:], func=AF.Exp), r=["ANEG"], w=["ANEG"])
        cx.op("dve", lambda e: e.tensor_scalar(out=ANEG[:], in0=ANEG[:], scalar1=-1.0, scalar2=None, op0=ALU.mult),
              r=["ANEG"], w=["ANEG"])
        cx.op("pool", lambda e: e.affine_select(out=LT[:], in_=cst["onesf"][:], pattern=[[-1, 128]], compare_op=ALU.is_gt,
                                                fill=0.0, base=0, channel_multiplier=1), r=["c_onesf"], w=["LT"])
        cx.op("pool", lambda e: e.tensor_scalar(out=NEGM[:], in0=LT[:], scalar1=NEG, scalar2=None, op0=ALU.mult),
              r=["LT"], w=["NEGM"])
        S = cx.sb("S", [128, 1536], F32, es)
        S3 = S[:].rearrange("p (h d) -> p h d", h=24)
        SM = cx.sb("SM", [128, 8, 24], F32, es)
        xc = [cx.sb("xc%d" % i, [128, 1536], F32, es) for i in range(1)] * 2
        bc = [cx.sb("bc%d" % i, [128, 512], F32, es) for i in range(2)]
        dtr = [cx.sb("dtr%d" % i, [128, 536], F32, es) for i in range(2)]
        bcb = cx.sb("bcb", [128, 512], BF16, es)
        xdt = cx.sb("xdt", [128, 1536], F32, es)
        xwb = cx.sb("xwb", [128, 1536], BF16, es)
        if A_ONLY:
            ATS = cx.sb("ATS", [128, 24], F32, es)
            cx.op("dve", lambda e: e.memset(S[:], 0.0), w=["S"])
            cx.op("dve", lambda e: e.memset(ATS[:], 0.0), w=["ATS"])
        else:
            Sb = cx.sb("Sb", [128, 1536], BF16, es)
            DSK = cx.sb("DSK", [128, 24], F32, es)
            GN = cx.sb("GN", [128, 1536], F32, es)
            cx.dma("sp", lambda e: e.dma_start(out=DSK[:], in_=d_skip.partition_broadcast(128)), w=["DSK"])
            cx.dma("sp", lambda e: e.dma_start(out=GN[:], in_=norm_g.partition_broadcast(128)), w=["GN"])
            zc = [cx.sb("zc%d" % i, [128, 1536], F32, es) for i in range(1)] * 2
            BTc = [cx.sb("BTc%d" % i, [128, 4, 128], BF16, es) for i in range(2)]
            CTc = [cx.sb("CTc%d" % i, [128, 4, 128], BF16, es) for i in range(2)]
            xdb = cx.sb("xdb", [128, 1536], BF16, es)
            M1 = [cx.sb("M1_%d" % i, [128, 128], F32, es) for i in range(2)]
            DEC = [cx.sb("DEC%d" % i, [128, 128], F32, es) for i in range(2)]
            Gt = [cx.sb("Gt%d" % i, [128, 128], BF16, es) for i in range(2)]
            Yt = cx.sb("Yt", [128, 1536], F32, es)
            tmp = cx.sb("ytmp", [128, 1536], F32, es)
            cat = [cx.sb("cat%d" % i, [128, 2048], BF16, es) for i in range(1)] * 2
            ssq = cx.sb("ssq", [128, 2], F32, es)
            cx.dma("sp", lambda e: e.dma_start(out=S[:], in_=Sp[0]), w=["S"])
            for i in (1, 2):
                cx.dma("sp", lambda e, i=i: e.dma_start(out=tmp[:], in_=Sp[i]), w=["ytmp"])
                cx.dma("sp", lambda e, i=i: e.dma_start(out=SM[:, 0, :], in_=Ap[i]), w=[("SM", 0)])
                cx.op("act", lambda e: e.activation(out=SM[:, 0, :], in_=SM[:, 0, :], func=AF.Exp), r=[("SM", 0)], w=[("SM", 0)])
                cx.op("dve", lambda e: e.tensor_tensor(out=S3, in0=S3, in1=SM[:, 0, :].unsqueeze(2).to_broadcast([128, 24, 64]),
                                                       op=ALU.mult), r=["S", ("SM", 0)], w=["S"])
                cx.op("dve", lambda e: e.tensor_tensor(out=S[:], in0=S[:], in1=tmp[:], op=ALU.add), r=["S", "ytmp"], w=["S"])
            cpy(cx, "act", Sb[:], S[:], ["S"], ["Sb"])
        nhead = 0
        for c in range(NCH):
            b = c % 2
            t = c + 1
            cx.dma("sp", lambda e, c=c, b=b: e.dma_start(out=xc[b][:], in_=XTOK[c * 128:(c + 1) * 128, :]), w=[("xc", 0)])
            cx.dma("sp", lambda e, c=c, b=b: e.dma_start(out=bc[b][:], in_=BTOK[c * 128:(c + 1) * 128, :]), w=[("bc", b)])
            cx.dma("sp", lambda e, t=t, b=b: e.dma_start(out=dtr[b][:], in_=m.U[t * 128:(t + 1) * 128, 4096:4632]), w=[("dtr", b)])
            if not A_ONLY:
                cx.dma("sp", lambda e, t=t, b=b: e.dma_start(out=zc[b][:], in_=m.U[t * 128:(t + 1) * 128, 0:1536]), w=[("zc", 0)])
                cx.dma("sp", lambda e, c=c, b=b: e.dma_start(
                    out=BTc[b][:], in_=BCT[0:512, c * 128:(c + 1) * 128].rearrange("(g n) l -> n g l", n=128)), w=[("BTc", b)])
                cx.dma("sp", lambda e, c=c, b=b: e.dma_start(
                    out=CTc[b][:], in_=BCT[512:1024, c * 128:(c + 1) * 128].rearrange("(g n) l -> n g l", n=128)), w=[("CTc", b)])
            sm = lambda i: SM[:, i, :]
            k = lambda i: ("SM", i)
            cx.op("dve", lambda e: e.tensor_tensor(out=sm(0), in0=dtr[b][:, 0:24], in1=DTB[:], op=ALU.add),
                  r=[("dtr", b), "DTB"], w=[k(0)])
            cx.op("act", lambda e: e.activation(out=sm(0), in_=sm(0), func=AF.Exp), r=[k(0)], w=[k(0)])
            cx.op("dve", lambda e: e.tensor_scalar(out=sm(0), in0=sm(0), scalar1=1.0, scalar2=None, op0=ALU.add), r=[k(0)], w=[k(0)])
            cx.op("act", lambda e: e.activation(out=sm(1), in_=sm(0), func=AF.Ln), r=[k(0)], w=[k(1)])
            cx.op("dve", lambda e: e.tensor_tensor(out=sm(2), in0=sm(1), in1=ANEG[:], op=ALU.mult), r=[k(1), "ANEG"], w=[k(2)])
            cx.op("pe", lambda e: e.matmul(P_ACS, lhsT=cst["trif"][:], rhs=sm(2), start=True, stop=True),
                  r=["c_trif", k(2)], w=[kSM])
            cx.op("pe", lambda e: e.matmul(P_ATOT, lhsT=cst["onesf"][:], rhs=sm(2), start=True, stop=True),
                  r=["c_onesf", k(2)], w=[kSM])
            cpy(cx, "dve", sm(3), P_ACS, [kSM], [k(3)])
            cpy(cx, "dve", sm(4), P_ATOT, [kSM], [k(4)])
            cx.op("dve", lambda e: e.tensor_tensor(out=sm(5), in0=sm(4), in1=sm(3), op=ALU.subtract), r=[k(3), k(4)], w=[k(5)])
            cx.op("act", lambda e: e.activation(out=sm(5), in_=sm(5), func=AF.Exp), r=[k(5)], w=[k(5)])
            cx.op("act", lambda e: e.activation(out=sm(6), in_=sm(4), func=AF.Exp), r=[k(4)], w=[k(6)])
            if A_ONLY:
                cx.op("dve", lambda e: e.tensor_tensor(out=ATS[:], in0=ATS[:], in1=sm(4), op=ALU.add), r=["ATS", k(4)], w=["ATS"])
            else:
                cx.op("act", lambda e: e.activation(out=sm(7), in_=sm(3), func=AF.Exp), r=[k(3)], w=[k(7)])
            xc3 = xc[b][:].rearrange("p (h d) -> p h d", h=24)
            xdt3 = xdt[:].rearrange("p (h d) -> p h d", h=24)
            cx.op("dve", lambda e: e.tensor_tensor(out=xdt3, in0=xc3, in1=sm(1).unsqueeze(2).to_broadcast([128, 24, 64]),
                                                   op=ALU.mult), r=[("xc", 0), k(1)], w=["xdt"])
            cx.op("dve", lambda e: e.tensor_tensor(out=xwb[:].rearrange("p (h d) -> p h d", h=24), in0=xdt3,
                                                   in1=sm(5).unsqueeze(2).to_broadcast([128, 24, 64]), op=ALU.mult),
                  r=["xdt", k(5)], w=["xwb"])
            cpy(cx, "act", bcb[:], bc[b][:], [("bc", b)], ["bcb"])
            if not A_ONLY:
                cpy(cx, "act", xdb[:], xdt[:], ["xdt"], ["xdb"])
            for g in range(4):
                gs = slice(g * 384, (g + 1) * 384)
                cx.op("pe", lambda e, g=g, gs=gs: e.matmul(P_SNEW, lhsT=bcb[:, g * 128:(g + 1) * 128], rhs=xwb[:, gs],
                                                           start=True, stop=True), r=["bcb", "xwb"], w=[kS])
                if not A_ONLY:
                    cx.op("pe", lambda e, g=g, gs=gs: e.matmul(P_YOFF, lhsT=CTc[b][:, g, :], rhs=Sb[:, gs], start=True, stop=True),
                          r=[("CTc", b), "Sb"], w=[kS])
                    cx.op("pe", lambda e, g=g: e.matmul(P_CBT, lhsT=BTc[b][:, g, :], rhs=CTc[b][:, g, :], start=True, stop=True),
                          r=[("BTc", b), ("CTc", b)], w=[kCBT])
                    for r_ in range(6):
                        h = g * 6 + r_
                        hb = nhead % 2
                        nhead += 1
                        cx.op("pool", lambda e, h=h, hb=hb: e.tensor_scalar(out=M1[hb][:], in0=LT[:], scalar1=SM[:, 2, h:h + 1],
                                                                           scalar2=None, op0=ALU.mult),
                              r=["LT", k(2)], w=[("M1", hb)])
                        cx.op("pe", lambda e, hb=hb: e.matmul(P_SEG[hb], lhsT=M1[hb][:], rhs=cst["trif"][:], start=True, stop=False),
                              r=[("M1", hb), "c_trif"], w=[kSEG[hb]])
                        cx.op("pe", lambda e, hb=hb: e.matmul(P_SEG[hb], lhsT=cst["idf"][:], rhs=NEGM[:], start=False, stop=True),
                              r=["c_idf", "NEGM"], w=[kSEG[hb]])
                        cx.op("act", lambda e, hb=hb: e.activation(out=DEC[hb][:], in_=P_SEG[hb], func=AF.Exp),
                              r=[kSEG[hb]], w=[("DEC", hb)])
                        cx.op("dve", lambda e, hb=hb: e.tensor_tensor(out=Gt[hb][:], in0=DEC[hb][:], in1=P_CBT, op=ALU.mult),
                              r=[("DEC", hb), kCBT], w=[("Gt", hb)])
                        cx.op("pe", lambda e, hb=hb, h=h, r_=r_: e.matmul(
                            P_YDG[:, r_ * 64:(r_ + 1) * 64], lhsT=Gt[hb][:], rhs=xdb[:, h * 64:(h + 1) * 64], start=True, stop=True),
                            r=[("Gt", hb), "xdb"], w=[kYDG])
                    cx.op("dve", lambda e, g=g, gs=gs: e.tensor_tensor(
                        out=tmp[:, gs].rearrange("p (h d) -> p h d", h=6), in0=P_YOFF.rearrange("p (h d) -> p h d", h=6),
                        in1=SM[:, 7, g * 6:(g + 1) * 6].unsqueeze(2).to_broadcast([128, 6, 64]), op=ALU.mult),
                        r=[kS, k(7)], w=["ytmp"])
                    cx.op("dve", lambda e, gs=gs, g=g: e.tensor_tensor(out=Yt[:, gs], in0=tmp[:, gs], in1=P_YDG, op=ALU.add),
                          r=["ytmp", kYDG], w=["Yt"])
                S3g = S[:, gs].rearrange("p (h d) -> p h d", h=6)
                cx.op("dve", lambda e, g=g, S3g=S3g: e.tensor_tensor(
                    out=S3g, in0=S3g, in1=SM[:, 6, g * 6:(g + 1) * 6].unsqueeze(2).to_broadcast([128, 6, 64]), op=ALU.mult),
                    r=["S", k(6)], w=["S"])
                cx.op("dve", lambda e, gs=gs: e.tensor_tensor(out=S[:, gs], in0=S[:, gs], in1=P_SNEW, op=ALU.add),
                      r=["S", kS], w=["S"])
                if not A_ONLY:
                    cpy(cx, "act", Sb[:, gs], S[:, gs], ["S"], ["Sb"])
            if A_ONLY:
                continue
            ytk = ["Yt"]
            cx.op("dve", lambda e: e.tensor_tensor(out=tmp[:].rearrange("p (h d) -> p h d", h=24), in0=xc3,
                                                   in1=DSK[:].unsqueeze(2).to_broadcast([128, 24, 64]), op=ALU.mult),
                  r=[("xc", 0), "DSK", "ytmp"], w=["ytmp"])
            cx.op("dve", lambda e: e.tensor_tensor(out=Yt[:], in0=Yt[:], in1=tmp[:], op=ALU.add), r=["Yt", "ytmp"], w=["Yt"])
            cx.op("act", lambda e: e.activation(out=tmp[:], in_=zc[b][:], func=AF.Silu), r=[("zc", 0), "ytmp"], w=["ytmp"])
            cx.op("dve", lambda e: e.tensor_tensor(out=Yt[:], in0=Yt[:], in1=tmp[:], op=ALU.mult), r=["Yt", "ytmp"], w=["Yt"])
            cx.op("dve", lambda e: e.tensor_tensor(out=tmp[:], in0=Yt[:], in1=Yt[:], op=ALU.mult), r=["Yt", "ytmp"], w=["ytmp"])
            cx.op("dve", lambda e: e.reduce_sum(out=ssq[:, 0:1], in_=tmp[:], axis=AX.X), r=["ytmp"], w=["ssq"])
            cx.op("dve", lambda e: e.tensor_scalar(out=ssq[:, 0:1], in0=ssq[:, 0:1], scalar1=1.0 / 1536, scalar2=LN_EPS,
                                                   op0=ALU.mult, op1=ALU.add), r=["ssq"], w=["ssq"])
            cx.op("act", lambda e: e.sqrt(out=ssq[:, 0:1], in_=ssq[:, 0:1]), r=["ssq"], w=["ssq"])
            cx.op("dve", lambda e: e.reciprocal(out=ssq[:, 1:2], in_=ssq[:, 0:1]), r=["ssq"], w=["ssq"])
            cx.op("dve", lambda e: e.tensor_scalar(out=Yt[:], in0=Yt[:], scalar1=ssq[:, 1:2], scalar2=None, op0=ALU.mult),
                  r=["Yt", "ssq"], w=["Yt"])
            cx.op("dve", lambda e: e.tensor_tensor(out=cat[b][:, 0:1536], in0=Yt[:], in1=GN[:], op=ALU.mult),
                  r=["Yt", "GN"], w=[("cat", 0, 0)])
            m.xattn_tile(dtr[b][:, 24:536], [("dtr", b)], cat[b][:, 1536:2048], ("cat", 0, 1))
            m.outproj_tile(c, cat[b], [("cat", 0, 0), ("cat", 0, 1)])
        if A_ONLY:
            cx.dma("sp", lambda e: e.dma_start(out=S_end, in_=S[:]), r=["S"], w=[])
            cx.dma("sp", lambda e: e.dma_start(out=a_tot, in_=ATS[:]), r=["ATS"], w=[])
        cx.barrier()

_PROGS = {}


def _prog(name):
    if name not in _PROGS:
        if name == "swa":
            _PROGS[name] = build_swa()
        elif name == "conf":
            _PROGS[name] = build_conf()
        elif name == "ssdA":
            _PROGS[name] = build_ssd("A")
        elif name == "ssdB":
            _PROGS[name] = build_ssd("B")
        elif name == "moe":
            _PROGS[name] = build_moe()
    return _PROGS[name]


NCORES = 8
SEGS = 4


def _run(name, in_maps):
    res = run_bass_kernel_spmd(_prog(name), in_maps, core_ids=list(range(NCORES)))
    return res.results


def _halo(h, positions=None):
    outs = []
    for c in range(NCORES):
        b, s = divmod(c, SEGS)
        s0 = s * T_LOC
        hx = np.zeros((T_LOC + HALO,) + h.shape[2:], h.dtype)
        hx[HALO:] = h[b, s0:s0 + T_LOC]
        if s > 0:
            hx[:HALO] = h[b, s0 - HALO:s0]
        outs.append(hx)
    return outs


def kernel(x, mem, positions,
           attn_w_in, attn_b_in, attn_sinks,
           ssd_w_in, ssd_b_in, ssd_conv_w, ssd_conv_b, ssd_dt_bias, ssd_a_log, ssd_d_skip, ssd_norm_g,
           conf_w_in, conf_b_in, conf_dw_w, conf_dw_b, conf_ln_g, conf_ln_b,
           mem_w_kv, w_out, b_out, ln1_g, ln1_b,
           router_w, router_b, moe_w1, moe_b1, moe_w2, moe_b2, ln2_g, ln2_b):
    f = lambda a: np.ascontiguousarray(np.asarray(a))
    h = f(x).astype(np.float32, copy=False)
    mem = f(mem)
    positions = f(positions).astype(np.int32, copy=False)
    flags = [np.full((128, 1), 1.0 if (c % SEGS) > 0 else 0.0, np.float32) for c in range(NCORES)]
    pos_h = _halo(positions[:, :, None])
    DEPTH = 4
    for i in range(DEPTH):
        kind, j = i % 3, i // 3
        hx = _halo(h)
        common = lambda c: dict(hx=hx[c], flag=flags[c], mem=f(mem[c // SEGS]), w_kv=f(mem_w_kv[i]), w_out=f(w_out[i]),
                                b_out=f(b_out[i])[None], ln_g=f(ln1_g[i])[None], ln_b=f(ln1_b[i])[None])
        if kind == 0:
            ins = [dict(common(c), w_in=f(attn_w_in[j]), b_in=f(attn_b_in[j])[None],
                        pos=np.ascontiguousarray(pos_h[c].reshape(17, 128)), sinks=f(attn_sinks[j])[None])
                   for c in range(NCORES)]
            r = _run("swa", ins)
        elif kind == 1:
            base = lambda c: dict(hx=hx[c], flag=flags[c], w_in=f(ssd_w_in[j]), b_in=f(ssd_b_in[j])[None],
                                  conv_w=f(ssd_conv_w[j]), conv_b=f(ssd_conv_b[j])[None],
                                  dt_bias=f(ssd_dt_bias[j])[None], a_log=f(ssd_a_log[j])[None])
            ra = _run("ssdA", [base(c) for c in range(NCORES)])
            ins = []
            for c in range(NCORES):
                b, s = divmod(c, SEGS)
                Sp = np.zeros((3, 128, 1536), np.float32)
                Ap = np.zeros((3, 128, 24), np.float32)
                for q in range(3):
                    sp = s - 3 + q
                    if sp >= 0:
                        Sp[q] = ra[b * SEGS + sp]["S_end"]
                        Ap[q] = ra[b * SEGS + sp]["a_tot"]
                d = dict(common(c), **base(c))
                d.update(Sp=Sp, Ap=Ap, d_skip=f(ssd_d_skip[j])[None], norm_g=f(ssd_norm_g[j])[None])
                ins.append(d)
            r = _run("ssdB", ins)
        else:
            ins = [dict(common(c), w_in=f(conf_w_in[j]), b_in=f(conf_b_in[j])[None], dw_w=f(conf_dw_w[j]),
                        dw_b=f(conf_dw_b[j])[None], cln_g=f(conf_ln_g[j])[None], cln_b=f(conf_ln_b[j])[None])
                   for c in range(NCORES)]
            r = _run("conf", ins)
        h1 = [r[c]["out"] for c in range(NCORES)]
        ins = [dict(h1=h1[c], router_w=f(router_w[i]), router_b=f(router_b[i])[None], w1=f(moe_w1[i]), b1=f(moe_b1[i]),
                    w2=f(moe_w2[i]), b2=f(moe_b2[i]), ln_g=f(ln2_g[i])[None], ln_b=f(ln2_b[i])[None]) for c in range(NCORES)]
        r = _run("moe", ins)
        h = np.stack([np.concatenate([r[b * SEGS + s]["out"] for s in range(SEGS)], 0) for b in range(2)], 0)
    return h.astype(np.float32, copy=False)
```
